# Optimizing a Trainium2 kernel written in Bass

```python
import math
import jax, jax.numpy as jnp
from jax import lax
import numpy as np

D_MODEL = 2048
BATCH = 4
SEQ = 2048
DEPTH = 2

SSM_WIDTH = D_MODEL // 2
SSM_GROUP = 16
SSM_GROUPS = SSM_WIDTH // SSM_GROUP
SSM_STATE = 64
DT_MIN = 0.001
DT_MAX = 0.1
ATTN_HEADS = 16
HEAD_DIM = 128
ATTN_WIDTH = ATTN_HEADS * HEAD_DIM
MOBA_BLOCK = 256
MOBA_TOPK = 3
Q_CHUNK = 16
ROT_DIM = HEAD_DIM // 4
ROPE_THETA = 500000.0
NORM_EPS = 1e-6
IN_SPLITS = (SSM_WIDTH, SSM_WIDTH, ATTN_WIDTH, ATTN_WIDTH, ATTN_WIDTH, ATTN_WIDTH, D_MODEL, D_MODEL)
IN_WIDTH = sum(IN_SPLITS)

kernel_name = "hybrid_s5_moba_gated_block"


def rms_norm(x, gain):
    xf = x.astype(jnp.float32)
    ms = jnp.mean(xf * xf, axis=-1, keepdims=True)
    return (xf * lax.rsqrt(ms + NORM_EPS) * gain.astype(jnp.float32)).astype(x.dtype)


def _complex_scan_op(e1, e2):
    a1r, a1i, b1r, b1i = e1
    a2r, a2i, b2r, b2i = e2
    return (a2r * a1r - a2i * a1i,
            a2r * a1i + a2i * a1r,
            a2r * b1r - a2i * b1i + b2r,
            a2r * b1i + a2i * b1r + b2i)


def s5_mixer(u, lam_re, lam_im, log_dt, b_re, b_im, c_re, c_im, d_skip):
    bsz, seq, _ = u.shape
    uf = u.astype(jnp.float32).reshape(bsz, seq, SSM_GROUPS, SSM_GROUP)
    lr = lam_re.astype(jnp.float32)
    li = lam_im.astype(jnp.float32)
    dt = jnp.exp(log_dt.astype(jnp.float32))[:, None]
    mag = jnp.exp(lr * dt)
    abar_re = mag * jnp.cos(li * dt)
    abar_im = mag * jnp.sin(li * dt)
    den = lr * lr + li * li
    nr = abar_re - 1.0
    ni = abar_im
    f_re = ((nr * lr + ni * li) / den)[..., None]
    f_im = ((ni * lr - nr * li) / den)[..., None]
    br = b_re.astype(jnp.float32)
    bi = b_im.astype(jnp.float32)
    bb_re = f_re * br - f_im * bi
    bb_im = f_re * bi + f_im * br
    bu_re = jnp.einsum('blgc,gpc->blgp', uf, bb_re)
    bu_im = jnp.einsum('blgc,gpc->blgp', uf, bb_im)
    full = bu_re.shape
    a_re = jnp.broadcast_to(abar_re, full)
    a_im = jnp.broadcast_to(abar_im, full)
    _, _, x_re, x_im = lax.associative_scan(_complex_scan_op, (a_re, a_im, bu_re, bu_im), axis=1)
    y = (jnp.einsum('blgp,gcp->blgc', x_re, c_re.astype(jnp.float32))
         - jnp.einsum('blgp,gcp->blgc', x_im, c_im.astype(jnp.float32))
         + d_skip.astype(jnp.float32).reshape(SSM_GROUPS, SSM_GROUP) * uf)
    return y.reshape(bsz, seq, SSM_WIDTH)


def partial_rope(t, cos, sin):
    half = ROT_DIM // 2
    t1 = t[..., :half]
    t2 = t[..., half:ROT_DIM]
    return jnp.concatenate([t1 * cos - t2 * sin, t2 * cos + t1 * sin, t[..., ROT_DIM:]], axis=-1)


def moba_attention(q, k, v):
    bsz, nh, seq, hd = q.shape
    nblk = -(-seq // MOBA_BLOCK)
    pad = nblk * MOBA_BLOCK - seq
    kp = jnp.pad(k, ((0, 0), (0, 0), (0, pad), (0, 0)))
    vp = jnp.pad(v, ((0, 0), (0, 0), (0, pad), (0, 0)))
    kb = kp.reshape(bsz, nh, nblk, MOBA_BLOCK, hd)
    vb = vp.reshape(bsz, nh, nblk, MOBA_BLOCK, hd)
    kmean = jnp.mean(kb.astype(jnp.float32), axis=3)
    ksel = min(MOBA_TOPK, nblk)
    n_chunks = seq // Q_CHUNK
    qc = q.reshape(bsz, nh, n_chunks, Q_CHUNK, hd).transpose(2, 0, 1, 3, 4)
    scale = 1.0 / math.sqrt(hd)
    b_ix = jnp.arange(bsz)[:, None, None, None]
    h_ix = jnp.arange(nh)[None, :, None, None]
    blk_ids = jnp.arange(nblk)

    def one_chunk(args):
        ci, qi = args
        q0 = ci * Q_CHUNK
        own = q0 // MOBA_BLOCK
        qpos = q0 + jnp.arange(Q_CHUNK)
        k_own = lax.dynamic_index_in_dim(kb, own, axis=2, keepdims=False)
        v_own = lax.dynamic_index_in_dim(vb, own, axis=2, keepdims=False)
        kpos = own * MOBA_BLOCK + jnp.arange(MOBA_BLOCK)
        s_own = jnp.einsum('bhqd,bhkd->bhqk', qi, k_own).astype(jnp.float32) * scale
        s_own = jnp.where(kpos[None, :] <= qpos[:, None], s_own, -jnp.inf)
        gate = jnp.einsum('bhqd,bhnd->bhqn', qi.astype(jnp.float32), kmean)
        gate = jnp.where(blk_ids < own, gate, -jnp.inf)
        _, idx = lax.top_k(gate, ksel)
        valid = idx < own
        k_sel = kb[b_ix, h_ix, idx]
        v_sel = vb[b_ix, h_ix, idx]
        s_sel = jnp.einsum('bhqd,bhqnkd->bhqnk', qi, k_sel).astype(jnp.float32) * scale
        s_sel = jnp.where(valid[..., None], s_sel, -jnp.inf)
        s_sel = s_sel.reshape(bsz, nh, Q_CHUNK, ksel * MOBA_BLOCK)
        p = jax.nn.softmax(jnp.concatenate([s_own, s_sel], axis=-1), axis=-1)
        p_own = p[..., :MOBA_BLOCK].astype(v.dtype)
        p_sel = p[..., MOBA_BLOCK:].reshape(bsz, nh, Q_CHUNK, ksel, MOBA_BLOCK).astype(v.dtype)
        o = (jnp.einsum('bhqk,bhkd->bhqd', p_own, v_own)
             + jnp.einsum('bhqnk,bhqnkd->bhqd', p_sel, v_sel))
        return o.astype(q.dtype)

    out = lax.map(one_chunk, (jnp.arange(n_chunks), qc))
    return out.transpose(1, 2, 0, 3, 4).reshape(bsz, nh, seq, hd)


def setup_inputs(seed: int = 0) -> dict:
    key = jax.random.key(seed)
    ks = jax.random.split(key, 20)
    f32 = jnp.float32
    L, G, P, GC = DEPTH, SSM_GROUPS, SSM_STATE, SSM_GROUP
    x = jax.random.normal(ks[0], (BATCH, SEQ, D_MODEL), f32)
    pre_norm = 1.0 + 0.02 * jax.random.normal(ks[1], (L, D_MODEL), f32)
    post_norm = 1.0 + 0.02 * jax.random.normal(ks[2], (L, D_MODEL), f32)
    w_in = jax.random.normal(ks[3], (L, D_MODEL, IN_WIDTH), f32) * D_MODEL ** -0.5
    n = jnp.arange(P, dtype=f32)
    lam_re = -0.5 + 0.01 * jax.random.normal(ks[4], (L, G, P), f32)
    lam_im = jnp.pi * n[None, None, :] + 0.01 * jax.random.normal(ks[5], (L, G, P), f32)
    log_dt = jax.random.uniform(ks[6], (L, G), f32, math.log(DT_MIN), math.log(DT_MAX))
    b_re = jax.random.normal(ks[7], (L, G, P, GC), f32) * (2.0 * GC) ** -0.5
    b_im = jax.random.normal(ks[8], (L, G, P, GC), f32) * (2.0 * GC) ** -0.5
    c_re = jax.random.normal(ks[9], (L, G, GC, P), f32) * (2.0 * P) ** -0.5
    c_im = jax.random.normal(ks[10], (L, G, GC, P), f32) * (2.0 * P) ** -0.5
    d_skip = jax.random.normal(ks[11], (L, SSM_WIDTH), f32) * 0.5
    w_glu = jax.random.normal(ks[12], (L, SSM_WIDTH, SSM_WIDTH), f32) * SSM_WIDTH ** -0.5
    b_glu = 0.01 * jax.random.normal(ks[13], (L, SSM_WIDTH), f32)
    w_br_ssm = jax.random.normal(ks[14], (L, SSM_WIDTH, D_MODEL), f32) * SSM_WIDTH ** -0.5
    w_br_attn = jax.random.normal(ks[15], (L, ATTN_WIDTH, D_MODEL), f32) * ATTN_WIDTH ** -0.5
    w_out = jax.random.normal(ks[16], (L, D_MODEL, D_MODEL), f32) * D_MODEL ** -0.5
    return {"x": x, "pre_norm": pre_norm, "post_norm": post_norm, "w_in": w_in,
            "lam_re": lam_re, "lam_im": lam_im, "log_dt": log_dt,
            "b_re": b_re, "b_im": b_im, "c_re": c_re, "c_im": c_im, "d_skip": d_skip,
            "w_glu": w_glu, "b_glu": b_glu, "w_br_ssm": w_br_ssm, "w_br_attn": w_br_attn,
            "w_out": w_out}


def reference(x, pre_norm, post_norm, w_in, lam_re, lam_im, log_dt, b_re, b_im, c_re, c_im,
              d_skip, w_glu, b_glu, w_br_ssm, w_br_attn, w_out):
    bsz, seq, _ = x.shape
    pos = jnp.arange(seq, dtype=jnp.float32)
    inv_freq = ROPE_THETA ** (-jnp.arange(0, ROT_DIM, 2, dtype=jnp.float32) / ROT_DIM)
    ang = pos[:, None] * inv_freq[None, :]
    cos = jnp.cos(ang).astype(x.dtype)
    sin = jnp.sin(ang).astype(x.dtype)
    split_at = [int(s) for s in np.cumsum(IN_SPLITS)[:-1]]

    for l in range(DEPTH):
        h = rms_norm(x, pre_norm[l])
        proj = h @ w_in[l]
        u_s, z_s, q, k, v, z_a, g_s, g_a = jnp.split(proj, split_at, axis=-1)

        y_s = s5_mixer(u_s, lam_re[l], lam_im[l], log_dt[l], b_re[l], b_im[l],
                       c_re[l], c_im[l], d_skip[l]).astype(x.dtype)
        y_s = jax.nn.gelu(y_s)
        y_s = y_s * jax.nn.sigmoid(y_s @ w_glu[l] + b_glu[l])
        o_s = y_s * jax.nn.silu(z_s)

        def heads(t):
            return t.reshape(bsz, seq, ATTN_HEADS, HEAD_DIM).transpose(0, 2, 1, 3)
        qh = partial_rope(heads(q), cos, sin)
        kh = partial_rope(heads(k), cos, sin)
        vh = heads(v)
        o_a = moba_attention(qh, kh, vh).transpose(0, 2, 1, 3).reshape(bsz, seq, ATTN_WIDTH)
        o_a = o_a * jax.nn.silu(z_a)

        merged = (jax.nn.sigmoid(g_s) * (o_s @ w_br_ssm[l])
                  + jax.nn.sigmoid(g_a) * (o_a @ w_br_attn[l]))
        out = merged @ w_out[l]
        x = x + rms_norm(out, post_norm[l])
    return x
```

```python
import math
import numpy as np
import ml_dtypes
import concourse.bass as bass
import concourse.mybir as mybir
from concourse.bass_utils import run_bass_kernel_spmd

F32 = mybir.dt.float32
BF16 = mybir.dt.bfloat16
ALU = mybir.AluOpType
AF = mybir.ActivationFunctionType
AX = mybir.AxisListType

D = 2048
T = 1024
TT = 2048
NH = 16
DEPTH = 2
INW = 14336
C_U, C_ZS, C_Q, C_K, C_V, C_ZA, C_GS, C_GA = 0, 1024, 2048, 4096, 6144, 8192, 10240, 12288
MAGIC = 12582912.0
TWO_PI = 2.0 * math.pi
BIG = 30000.0
NEG = -1.0e30


class Plan:
    ENG = ("pe", "act", "dve", "pool", "sp")

    def __init__(self):
        self.ops = {e: [] for e in self.ENG}
        self.cnt = {}
        self.known = {e: {} for e in self.ENG}
        self.res = {}
        self.slots = []

    def _deps(self, reads, writes):
        deps = {}

        def add(d):
            if d is None:
                return
            k, c = d
            if deps.get(k, 0) < c:
                deps[k] = c
        for k in reads:
            r = self.res.get(k)
            if r:
                add(r[0])
        for k in writes:
            r = self.res.get(k)
            if r:
                add(r[0])
                for kk, cc in r[1].items():
                    add((kk, cc))
        return deps

    def _waits(self, eng, deps, skip_self=False):
        for k, c in deps.items():
            if skip_self and k == eng:
                continue
            if self.known[eng].get(k, 0) >= c:
                continue
            self.known[eng][k] = c
            self.ops[eng].append(("wait", k, c))

    def _update(self, reads, writes, prod):
        for k in reads:
            r = self.res.setdefault(k, [None, {}])
            if r[1].get(prod[0], 0) < prod[1]:
                r[1][prod[0]] = prod[1]
        for k in writes:
            self.res[k] = [prod, {}]

    def op(self, eng, fn, reads=(), writes=()):
        deps = self._deps(reads, writes)
        self._waits(eng, deps, skip_self=(eng == "pe"))
        c = self.cnt.get(eng, 0) + 1
        self.cnt[eng] = c
        self.ops[eng].append(("op", fn, eng, 1))
        self._update(reads, writes, (eng, c))

    def dma(self, q, fn, slot, reads=(), writes=(), inc=16):
        deps = self._deps(reads, writes)
        self._waits(q, deps)
        if slot not in self.slots:
            self.slots.append(slot)
        c = self.cnt.get(slot, 0) + inc
        self.cnt[slot] = c
        self.ops[q].append(("op", fn, slot, inc))
        self._update(reads, writes, (slot, c))

    def barrier(self):
        allk = dict(self.cnt)
        for e in self.ENG:
            self._waits(e, allk, skip_self=True)
        self.res = {}


def build_nc(n_cores=8, upto=None):
    nc = bass.Bass("TRN2", target_bir_lowering=False)
    P = Plan()

    def din(name, shape, dt=F32):
        return nc.dram_tensor(name, list(shape), dt, kind="ExternalInput").ap()

    x_in = din("x", [T, D])
    pre_norm = din("pre_norm", [DEPTH, D])
    post_norm = din("post_norm", [DEPTH, D])
    w_in = din("w_in", [DEPTH, D, INW])
    lam_re = din("lam_re", [DEPTH, 64, 64])
    lam_im = din("lam_im", [DEPTH, 64, 64])
    log_dt = din("log_dt", [DEPTH, 64])
    b_re = din("b_re", [DEPTH, 64, 64, 16])
    b_im = din("b_im", [DEPTH, 64, 64, 16])
    c_re = din("c_re", [DEPTH, 64, 16, 64])
    c_im = din("c_im", [DEPTH, 64, 16, 64])
    d_skip = din("d_skip", [DEPTH, 1024])
    w_glu = din("w_glu", [DEPTH, 1024, 1024])
    b_glu = din("b_glu", [DEPTH, 1024])
    w_br_ssm = din("w_br_ssm", [DEPTH, 1024, D])
    w_br_attn = din("w_br_attn", [DEPTH, D, D])
    w_out = din("w_out", [DEPTH, D, D])
    c_identf = din("c_identf", [128, 128])
    c_perm = din("c_perm", [128, 128], BF16)
    c_tri = din("c_tri", [128, 128], BF16)
    c_ones = din("c_ones", [128, 128], BF16)
    c_selm = din("c_selm", [8, 1024], BF16)
    c_tpos = din("c_tpos", [128, TT])
    c_vbl = din("c_vbl", [128, 64])
    c_ropei = din("c_ropei", [128, 1])
    c_flags = din("c_flags", [128, 4])
    y_out = nc.dram_tensor("y", [T, D], F32, kind="ExternalOutput").ap()

    def dint(name, shape, dt):
        return nc.dram_tensor(name, list(shape), dt).ap()
    x_mid = dint("x_mid", [T, D], F32)
    gin_c = [dint(f"gin{i}", [1024, T], BF16) for i in range(5)]
    gout_c = [dint(f"gout{i}", [2048, T], BF16) for i in range(5)]
    gin_u = gin_c[0]
    gout_u = gout_c[0]
    gin_vc = [gin_c[3 + c].rearrange("(t two) c -> t (two c)", two=2) for c in range(2)]
    gout_vc = [gout_c[3 + c][0:1024, :].rearrange("(t two) c -> t (two c)", two=2) for c in range(2)]

    def gk_rows(gl, h):
        return gl[1 + h // 8][(h % 8) * 128:(h % 8 + 1) * 128, :]

    off = [0]

    arena = nc.alloc_sbuf_tensor_at("arena", [128, 103 * 1024], BF16, offset=16640)

    def SB(name, shape, dt, at=None):
        esz = 4 if dt == F32 else 2
        nbytes = int(np.prod(shape[1:])) * esz
        nb = (nbytes + 63) // 64 * 64
        if at is None:
            at = off[0]
            off[0] += nb
        assert at % 4 == 0 and at + nb <= 206 * 1024, (name, at, nb)
        ap = arena[0:shape[0], at // 2:(at + nbytes) // 2]
        if dt == F32:
            ap = ap.bitcast(F32)
        if len(shape) == 3:
            ap = ap.rearrange("p (a b) -> p a b", a=shape[1])
        elif len(shape) == 4:
            ap = ap.rearrange("p (a b c) -> p a b c", a=shape[1], b=shape[2])
        return ap

    hoa = SB("hoa", [128, 2, 16, T], BF16)
    hT = hoa[:, 0]
    oaT = hoa[:, 1]
    OA0 = 32768
    uTa = SB("uTa", [128, 8, TT], BF16)
    szT = SB("szT", [128, 8, T], BF16)
    wbuf = SB("wbuf", [128, 2, 16, 512], BF16)
    identf = SB("identf", [128, 128], F32)
    permb = SB("permb", [128, 128], BF16)
    trib = SB("trib", [128, 128], BF16)
    onesb = SB("onesb", [128, 128], BF16)
    selm = SB("selm", [8, 1024], BF16)
    vbl = SB("vbl", [128, 64], F32)
    flags = SB("flags", [128, 4], F32)
    ropei = SB("ropei", [128, 1], F32)
    ropeT = SB("ropeT", [32, 2, T], F32)
    small = SB("small", [128, 64], F32)
    tpos_sb = SB("tpos_sb", [128, TT], F32)
    W0 = off[0]
    R_HT, R_OA, R_U, R_SZ = 0, 32768, 65536, 98304

    ps = [nc.alloc_psum_tensor(f"ps{i}", [128, 512], F32) for i in range(8)]

    def load(q, out_ap, in_ap, slot, reads=(), writes=()):
        P.dma(q, lambda e: e.dma_start(out=out_ap, in_=in_ap), slot, reads, writes)

    wslot_n = [0]

    def load_w(src_ap, kch, ncols):
        s = wslot_n[0] % 2
        wslot_n[0] += 1
        src = src_ap.rearrange("(k p) c -> p k c", p=128)
        for k0 in range(0, kch, 4):
            k1 = min(kch, k0 + 4)
            P.dma("pool", lambda e, d=wbuf[:, s, k0:k1, 0:ncols], sr=src[:, k0:k1, :]: e.dma_start(out=d, in_=sr),
                  f"w{s}", reads=(), writes=(f"wbuf{s}",))
        return s

    def mm(out_ap, lhsT, rhs, start, stop, reads, writes, tp=None):
        if tp is None:
            P.op("pe", lambda e: e.matmul(out_ap, lhsT, rhs, start=start, stop=stop), reads, writes)
        else:
            P.op("pe", lambda e: e.matmul(out_ap, lhsT, rhs, start=start, stop=stop, tile_position=tp),
                 reads, writes)

    def act(out_ap, in_ap, func, reads, writes, scale=1.0, bias=0.0, accum=None):
        if accum is None:
            P.op("act", lambda e: e.activation(out_ap, in_ap, func, bias=bias, scale=scale), reads, writes)
        else:
            P.op("act", lambda e: e.activation(out_ap, in_ap, func, bias=bias, scale=scale, accum_out=accum),
                 reads, writes)

    def tt(eng, out_ap, a, b, op, reads, writes):
        P.op(eng, lambda e: e.tensor_tensor(out_ap, a, b, op), reads, writes)

    def ts(eng, out_ap, a, s1, s2, op0, op1, reads, writes):
        if op1 is None:
            P.op(eng, lambda e: e.tensor_scalar(out_ap, a, s1, None, op0), reads, writes)
        else:
            P.op(eng, lambda e: e.tensor_scalar(out_ap, a, s1, s2, op0, op1), reads, writes)

    def stt(eng, out_ap, a, s, b, op0, op1, reads, writes):
        P.op(eng, lambda e: e.scalar_tensor_tensor(out_ap, a, s, b, op0, op1), reads, writes)

    def cp(eng, out_ap, in_ap, reads, writes):
        if eng == "act":
            P.op("act", lambda e: e.copy(out_ap, in_ap), reads, writes)
        else:
            P.op(eng, lambda e: e.tensor_copy(out_ap, in_ap), reads, writes)

    def fracs(eng, turns, tmp, keys):
        ts(eng, tmp, turns, MAGIC, -MAGIC, ALU.add, ALU.add, keys, keys)
        tt(eng, turns, turns, tmp, ALU.subtract, keys, keys)

    bankn = [0]
    nbanks = [4]

    def nb4():
        b = bankn[0] % nbanks[0]
        bankn[0] += 1
        return b

    for dst, src, nm in ((identf, c_identf, "identf"), (permb, c_perm, "permb"),
                         (trib, c_tri, "trib"), (onesb, c_ones, "onesb"), (selm, c_selm, "selm"),
                         (vbl, c_vbl, "vbl"), (flags, c_flags, "flags"), (ropei, c_ropei, "ropei"),
                         (tpos_sb, c_tpos, "tpos")):
        load("sp", dst[:], src, "c_" + nm, (), (nm,))
    rt1 = SB("rt1", [32, T], F32, at=R_OA)
    rt2 = SB("rt2", [32, T], F32, at=R_OA + 4096)
    act(small[0:32, 0:1], ropei[0:32, :], AF.Exp, ("ropei",), ("small",),
        scale=-math.log(500000.0) / 16.0, bias=-math.log(TWO_PI))
    ts("dve", rt1[:], tpos_sb[0:32, 0:T], flags[0:32, 1:2], small[0:32, 0:1], ALU.add, ALU.mult,
       ("tpos", "flags", "small"), ("rt",))
    fracs("dve", rt1[:], rt2[:], ("rt",))
    act(ropeT[:, 1, :], rt1[:], AF.Sin, ("rt",), ("ropeT",), scale=TWO_PI)
    act(rt2[:], rt1[:], AF.Abs, ("rt",), ("rt",))
    act(ropeT[:, 0, :], rt2[:], AF.Sin, ("rt",), ("ropeT",), scale=-TWO_PI, bias=math.pi / 2)
    P.barrier()

    def phase_norm(l, x_src):
        xt = [SB(f"n_xt{i}_{l}", [128, D], F32, at=R_U + i * 8192) for i in range(2)]
        hb = SB(f"n_hb_{l}", [128, D], F32, at=R_U + 16384)
        gainb = SB(f"n_gain_{l}", [128, D], F32, at=R_U + 24576)
        junk = SB(f"n_junk_{l}", [128, D], BF16, at=R_OA)
        st = SB(f"n_st_{l}", [128, 8], F32, at=R_OA + 4096)
        load("sp", gainb[:], pre_norm[l:l + 1, :].to_broadcast([128, D]), "gain", (), ("gainb",))
        for i in range(8):
            xb = xt[i % 2]
            xk = f"xt{i % 2}"
            load("sp", xb[:], x_src[i * 128:(i + 1) * 128, :], xk, (), (xk,))
            act(junk[:], xb[:], AF.Square, (xk,), ("junk", "st"), accum=st[:, 0:1])
            ts("dve", st[:, 1:2], st[:, 0:1], 1.0 / D, 1e-6, ALU.mult, ALU.add, ("st",), ("st",))
            act(st[:, 2:3], st[:, 1:2], AF.Sqrt, ("st",), ("st",))
            P.op("dve", lambda e: e.reciprocal(st[:, 3:4], st[:, 2:3]), ("st",), ("st",))
            stt("dve", hb[:], xb[:], st[:, 3:4], gainb[:], ALU.mult, ALU.mult, (xk, "st", "gainb"), ("hb",))
            for g4 in range(4):
                b = nb4()
                for j in range(4):
                    k = g4 * 4 + j
                    P.op("pe", lambda e, o=ps[b][:, j * 128:(j + 1) * 128], k=k: e.transpose(o, hb[:, k * 128:(k + 1) * 128], identf[:]),
                         ("hb", "identf"), (f"ps{b}",))
                cp("act" if g4 % 2 == 0 else "dve", hT[:, g4 * 4:(g4 + 1) * 4, i * 128:(i + 1) * 128],
                   ps[b][:, :].rearrange("p (j t) -> p j t", j=4), (f"ps{b}",), ("hT",))

    def proj_fm(s, ct, kch, rhsT, rkey, evac):
        for th in range(2):
            b = nb4()
            for k in range(kch):
                mm(ps[b][:, :], wbuf[:, s, k, ct * 128:(ct + 1) * 128], rhsT[:, k, th * 512:(th + 1) * 512],
                   k == 0, k == kch - 1, (f"wbuf{s}", rkey), (f"ps{b}",))
            evac(b, th)

    def rope(qf, qb, key):
        r1 = SB("rp_r1" + key, [32, 512], F32, at=W0)
        r2 = SB("rp_r2" + key, [32, 512], F32, at=W0 + 2048)
        cp("act", qb[:], qf[:], (key + "f",), (key + "b",))
        for th in range(2):
            sl = slice(th * 512, (th + 1) * 512)
            mm(ps[7][:, :], permb[:], qb[:, sl], True, True, ("permb", key + "b"), ("ps7",))
            tt("dve", r1[:], qf[0:32, sl], ropeT[:, 0, sl], ALU.mult, (key + "f", "ropeT"), ("rp1",))
            tt("dve", r2[:], ps[7][0:32, :], ropeT[:, 1, sl], ALU.mult, ("ps7", "ropeT"), ("rp2",))
            tt("dve", qf[0:32, sl], r1[:], r2[:], ALU.add, ("rp1", "rp2"), (key + "f",))
        cp("act", qb[0:32, :], qf[0:32, :], (key + "f",), (key + "b",))

    def phase_p1(l, stop=None):
        wl = w_in[l]
        kf = SB(f"p1_kf{l}", [128, T], F32, at=R_OA)
        kb = SB(f"p1_kb{l}", [128, T], BF16, at=R_OA + 4096)
        vst = [SB(f"p1_vst{i}_{l}", [128, 512], BF16, at=R_OA + 6144 + i * 1024) for i in range(2)]
        kml = SB(f"p1_kml{l}", [128, 64], F32, at=R_OA + 8192)
        for g in range(2):
            s = load_w(wl[:, C_U + g * 512:C_U + (g + 1) * 512], 16, 512)
            for ct in range(4):
                kt = g * 4 + ct
                proj_fm(s, ct, 16, hT, "hT",
                        lambda b, th, kt=kt: cp("act", uTa[:, kt, T + th * 512:T + (th + 1) * 512], ps[b][:, :],
                                                (f"ps{b}",), ("uTa",)))
        load("sp", gin_u.rearrange("(k p) t -> p k t", p=128), uTa[:, :, T:TT], "gu", ("uTa",), ("gin_u",))
        if stop == "a":
            return
        for g in range(4):
            s = load_w(wl[:, C_K + g * 512:C_K + (g + 1) * 512], 16, 512)
            for ct in range(4):
                h = g * 4 + ct
                proj_fm(s, ct, 16, hT, "hT",
                        lambda b, th: cp("act", kf[:, th * 512:(th + 1) * 512], ps[b][:, :], (f"ps{b}",), ("kff",)))
                rope(kf, kb, "kf")
                load("sp", gk_rows(gin_c, h), kb[:], "gk", ("kfb",), ("gin_k",))
        if stop == "b":
            return
        for g in range(4):
            s = load_w(wl[:, C_V + g * 512:C_V + (g + 1) * 512], 16, 512)
            for i in range(8):
                b = nb4()
                for k in range(16):
                    mm(ps[b][:, :], hT[:, k, i * 128:(i + 1) * 128], wbuf[:, s, k, 0:512], k == 0, k == 15,
                       (f"wbuf{s}", "hT"), (f"ps{b}",))
                vs = vst[i % 2]
                cp("act", vs[:], ps[b][:, :], (f"ps{b}",), (f"vst{i % 2}",))
                load("sp", gin_vc[i // 4][(i % 4) * 128:(i % 4 + 1) * 128, g * 512:(g + 1) * 512], vs[:], f"gv{i % 2}",
                     (f"vst{i % 2}",), ("gin_v",))
        if stop == "c":
            return
        for ci in range(5):
            P.dma("pool", lambda e, ci=ci: e.collective_compute(
                "AllGather", ALU.bypass, replica_groups=[[2 * i, 2 * i + 1] for i in range(n_cores // 2)],
                ins=[gin_c[ci].opt()], outs=[gout_c[ci].opt()]), f"cc{ci}", reads=("gin_u", "gin_k", "gin_v"),
                writes=("gout_u", "gout_k", "gout_v"), inc=1)

    def phase_ssm(l):
        A = R_OA
        ygT = SB(f"s_yg{l}", [128, 8, T], BF16, at=A)
        cpad = SB(f"s_cpad{l}", [128, 2, 32, 128], BF16, at=A + 16384)
        B = W0
        bbT = SB(f"s_bbT{l}", [128, 2, 32, 128], BF16, at=B + 16384)
        prm = SB(f"s_prm{l}", [128, 16, 32], F32, at=B + 4096)
        P.op("pool", lambda e: e.memset(bbT[:], 0.0), (), ("bbT",))
        wk = 114688
        load("sp", uTa[:, :, 0:T], gout_u[0:1024, :].rearrange("(k p) t -> p k t", p=128), "gul", ("gout_u",), ("uTa",))
        for kt in range(8):
            ts("dve", uTa[:, kt, 0:T], uTa[:, kt, 0:T], flags[:, 0:1], None, ALU.mult, None, ("uTa", "flags"), ("uTa",))
        LR, LI, DT, MAG, FR, AR, AI, DEN, NRM, FRE, FIM, T1, T2, T3, T4, T5 = range(16)
        pv = lambda i: prm[:, i, :]
        for two in range(2):
            load("sp", prm[two * 64:(two + 1) * 64, LR, :], lam_re[l].rearrange("(pi two) p -> two p pi", two=2)[two], "pl", (), ("prm",))
            load("sp", prm[two * 64:(two + 1) * 64, LI, :], lam_im[l].rearrange("(pi two) p -> two p pi", two=2)[two], "pl", (), ("prm",))
            load("sp", prm[two * 64:(two + 1) * 64, DT, :],
                 log_dt[l:l + 1, :].rearrange("o (pi two) -> o two pi", two=2)[:, two, :].to_broadcast([64, 32]),
                 "pl", (), ("prm",))
        K = ("prm",)
        act(pv(DT), pv(DT), AF.Exp, K, K)
        tt("dve", pv(T1), pv(LR), pv(DT), ALU.mult, K, K)
        act(pv(MAG), pv(T1), AF.Exp, K, K)
        tt("dve", pv(T2), pv(LI), pv(DT), ALU.mult, K, K)
        ts("dve", pv(FR), pv(T2), 1.0 / TWO_PI, None, ALU.mult, None, K, K)
        fracs("dve", pv(FR), pv(T3), K)
        act(pv(T4), pv(FR), AF.Sin, K, K, scale=TWO_PI)
        act(pv(T3), pv(FR), AF.Abs, K, K)
        act(pv(T5), pv(T3), AF.Sin, K, K, scale=-TWO_PI, bias=math.pi / 2)
        tt("dve", pv(AR), pv(MAG), pv(T5), ALU.mult, K, K)
        tt("dve", pv(AI), pv(MAG), pv(T4), ALU.mult, K, K)
        tt("dve", pv(T1), pv(LR), pv(LR), ALU.mult, K, K)
        tt("dve", pv(T2), pv(LI), pv(LI), ALU.mult, K, K)
        tt("dve", pv(DEN), pv(T1), pv(T2), ALU.add, K, K)
        P.op("dve", lambda e: e.reciprocal(pv(DEN), pv(DEN)), K, K)
        ts("dve", pv(NRM), pv(AR), -1.0, None, ALU.add, None, K, K)
        tt("dve", pv(T1), pv(NRM), pv(LR), ALU.mult, K, K)
        tt("dve", pv(T2), pv(AI), pv(LI), ALU.mult, K, K)
        tt("dve", pv(T1), pv(T1), pv(T2), ALU.add, K, K)
        tt("dve", pv(FRE), pv(T1), pv(DEN), ALU.mult, K, K)
        tt("dve", pv(T1), pv(AI), pv(LR), ALU.mult, K, K)
        tt("dve", pv(T2), pv(NRM), pv(LI), ALU.mult, K, K)
        tt("dve", pv(T1), pv(T1), pv(T2), ALU.subtract, K, K)
        tt("dve", pv(FIM), pv(T1), pv(DEN), ALU.mult, K, K)
        bS = [SB(f"s_bS{i}_{l}", [128, 32, 32], F32, at=wk + i * 4096) for i in range(4)]
        for i in range(2):
            P.op("dve", lambda e, t=bS[i]: e.memset(t[:], 0.0), (), (f"bS{i}",))
            src = (b_re, b_im)[i][l].rearrange("(pi two) p c -> two p pi c", two=2)
            for two in range(2):
                load("sp", bS[i][two * 64:(two + 1) * 64, :, two * 16:(two + 1) * 16], src[two], "pb", (), (f"bS{i}",))
        fre_b = pv(FRE).unsqueeze(2).to_broadcast([128, 32, 32])
        fim_b = pv(FIM).unsqueeze(2).to_broadcast([128, 32, 32])
        KB = ("bS0", "bS1", "bS2", "bS3", "prm")
        bbS = SB(f"s_bbS{l}", [128, 2, 32, 32], F32, at=wk + 16384)
        tt("dve", bS[2][:], bS[0][:], fre_b, ALU.mult, KB, KB)
        tt("dve", bS[3][:], bS[1][:], fim_b, ALU.mult, KB, KB)
        tt("dve", bbS[:, 0], bS[2][:], bS[3][:], ALU.subtract, KB, ("bbS",))
        tt("dve", bS[2][:], bS[1][:], fre_b, ALU.mult, KB, KB)
        tt("dve", bS[3][:], bS[0][:], fim_b, ALU.mult, KB, KB)
        tt("dve", bbS[:, 1], bS[2][:], bS[3][:], ALU.add, KB + ("bbS",), ("bbS",))
        for ri in range(2):
            for kt in range(8):
                P.op("pe", lambda e, ri=ri, kt=kt: e.transpose(ps[4][:, 0:128], bbS[:, ri, kt * 4:(kt + 1) * 4, :].rearrange("p a b -> p (a b)"), identf[:]),
                     ("bbS", "identf"), ("ps4",))
                for a4 in range(4):
                    cp("act", bbT[32 * a4:32 * a4 + 32, ri, kt * 4 + a4, :], ps[4][32 * a4:32 * a4 + 32, 0:128], ("ps4",), ("bbT",))
        P.op("pool", lambda e: e.memset(cpad[:], 0.0), (), ("cpad",))
        cS = [SB(f"s_cS{i}_{l}", [32, 32, 128], F32, at=R_SZ) for i in range(1)]
        for ri in range(2):
            P.op("dve", lambda e: e.memset(cS[0][:], 0.0), ("ps5",), ("cS",))
            src = (c_re, c_im)[ri][l].rearrange("(pi two) c p -> two c pi p", two=2)
            for two in range(2):
                load("sp", cS[0][two * 16:(two + 1) * 16, :, two * 64:(two + 1) * 64], src[two], "pc", (), ("cS",))
            for pi in range(32):
                P.op("pe", lambda e, pi=pi: e.transpose(ps[5][:, 0:32], cS[0][:, pi, :], identf[0:32, 0:32]),
                     ("cS", "identf"), ("ps5",))
                j = pi % 4
                act(cpad[:, ri, pi, j * 32:(j + 1) * 32], ps[5][:, 0:32], AF.Copy, ("ps5",), ("cpad",),
                    scale=(1.0 if ri == 0 else -1.0))
        dsk = SB(f"s_dsk{l}", [128, 8], F32, at=B + 6144)
        load("sp", dsk[:], d_skip[l].rearrange("(k p) -> p k", p=128), "pd", (), ("dsk",))
        P.barrier()
        m0 = wk
        def F(name, n, dt, o):
            return SB(f"s_{name}{l}", [128, n], dt, at=m0 + o)
        trn = [F(f"trn{i}", 512, F32, i * 2048) for i in range(2)]
        ttmp = F("ttmp", 512, F32, 4096)
        sn = F("sn", 512, BF16, 6144); cs = F("cs", 512, BF16, 7168); af = F("af", 512, F32, 8192)
        bre = F("bre", 512, BF16, 10240); bim = F("bim", 512, BF16, 11264)
        q1 = F("q1", 512, BF16, 12288); q2 = F("q2", 512, BF16, 13312)
        vre = F("vre", 512, BF16, 14336); vim = F("vim", 512, BF16, 15360)
        xsr = [F(f"xsr{i}", 512, F32, 16384 + i * 2048) for i in range(2)]
        xsi = [F(f"xsi{i}", 512, F32, 20480 + i * 2048) for i in range(2)]
        return ygT, cpad, bbT, prm, dsk, (trn, ttmp, sn, cs, af, bre, bim, q1, q2, vre, vim, xsr, xsi), (FR, MAG)

    def phase_ssm_main(l, st):
        ygT, cpad, bbT, prm, dsk, tl, (FR, MAG) = st
        trn, ttmp, sn, cs, af, bre, bim, q1, q2, vre, vim, xsr, xsi = tl
        yt = SB(f"s_yt{l}", [128, 512], F32, at=114688 + 24576)
        y2 = SB(f"s_y2{l}", [128, 512], F32, at=114688 + 26624)
        y3 = SB(f"s_y3{l}", [128, 512], F32, at=114688 + 28672)
        lnm = SB(f"s_lnm{l}", [128, 32], F32, at=W0 + 8192)
        act(lnm[:], prm[:, MAG, :], AF.Ln, ("prm",), ("lnm",))
        dec = SB(f"s_dec{l}", [128, 32], F32, at=W0 + 8192 + 128)
        cp("dve", dec[:], prm[:, MAG, :], ("prm",), ("dec",))
        n = 0
        for pi in range(32):
            kt, j = pi // 4, pi % 4
            rows = slice(32 * j, 32 * j + 32)
            for Q in range(4):
                sl = slice(Q * 512, (Q + 1) * 512)
                pr, pm = ps[(n % 2) * 2], ps[(n % 2) * 2 + 1]
                kr, km = f"ps{(n % 2) * 2}", f"ps{(n % 2) * 2 + 1}"
                mm(pr[:, :], bbT[:, 0, pi, :], uTa[:, kt, sl], True, True, ("bbT", "uTa"), (kr,))
                mm(pm[:, :], bbT[:, 1, pi, :], uTa[:, kt, sl], True, True, ("bbT", "uTa"), (km,))
                tr = trn[n % 2]
                tk = f"trn{n % 2}"
                ts("pool", tr[:], tpos_sb[:, sl], prm[:, FR, pi:pi + 1], None, ALU.mult, None, ("tpos", "prm"), (tk,))
                ts("pool", ttmp[:], tr[:], MAGIC, -MAGIC, ALU.add, ALU.add, (tk,), ("ttmp",))
                tt("pool", tr[:], tr[:], ttmp[:], ALU.subtract, (tk, "ttmp"), (tk,))
                act(sn[:], tr[:], AF.Sin, (tk,), ("sn",), scale=TWO_PI)
                act(af[:], tr[:], AF.Abs, (tk,), ("af",))
                act(cs[:], af[:], AF.Sin, ("af",), ("cs",), scale=-TWO_PI, bias=math.pi / 2)
                cp("act", bre[:], pr[:, :], (kr,), ("bre",))
                cp("act", bim[:], pm[:, :], (km,), ("bim",))
                tt("dve", q1[:], bre[:], cs[:], ALU.mult, ("bre", "cs"), ("q1",))
                tt("dve", q2[:], bim[:], sn[:], ALU.mult, ("bim", "sn"), ("q2",))
                tt("dve", vre[:], q1[:], q2[:], ALU.add, ("q1", "q2"), ("vre",))
                tt("dve", q1[:], bim[:], cs[:], ALU.mult, ("bim", "cs"), ("q1",))
                tt("dve", q2[:], bre[:], sn[:], ALU.mult, ("bre", "sn"), ("q2",))
                tt("dve", vim[:], q1[:], q2[:], ALU.subtract, ("q1", "q2"), ("vim",))
                xr, xi = xsr[Q % 2], xsi[Q % 2]
                xrp, xip = xsr[(Q + 1) % 2], xsi[(Q + 1) % 2]
                dcy = dec[:, pi:pi + 1].to_broadcast([128, 512])
                if Q == 0:
                    P.op("dve", lambda e, xr=xr, dcy=dcy: e.tensor_tensor_scan(xr[:], dcy, vre[:], 0.0, ALU.mult, ALU.add),
                         ("vre", "dec"), (f"xr{Q % 2}",))
                    P.op("dve", lambda e, xi=xi, dcy=dcy: e.tensor_tensor_scan(xi[:], dcy, vim[:], 0.0, ALU.mult, ALU.add),
                         ("vim", "dec"), (f"xi{Q % 2}",))
                else:
                    P.op("dve", lambda e, xr=xr, xrp=xrp, dcy=dcy: e.tensor_tensor_scan(xr[:], dcy, vre[:], xrp[:, 511:512], ALU.mult, ALU.add),
                         ("vre", "dec", f"xr{(Q + 1) % 2}"), (f"xr{Q % 2}",))
                    P.op("dve", lambda e, xi=xi, xip=xip, dcy=dcy: e.tensor_tensor_scan(xi[:], dcy, vim[:], xip[:, 511:512], ALU.mult, ALU.add),
                         ("vim", "dec", f"xi{(Q + 1) % 2}"), (f"xi{Q % 2}",))
                if Q >= 2:
                    tt("dve", q1[:], xr[:], cs[:], ALU.mult, (f"xr{Q % 2}", "cs"), ("q1",))
                    tt("dve", q2[:], xi[:], sn[:], ALU.mult, (f"xi{Q % 2}", "sn"), ("q2",))
                    tt("dve", vre[:], q1[:], q2[:], ALU.subtract, ("q1", "q2"), ("vre",))
                    tt("dve", q1[:], xr[:], sn[:], ALU.mult, (f"xr{Q % 2}", "sn"), ("q1",))
                    tt("dve", q2[:], xi[:], cs[:], ALU.mult, (f"xi{Q % 2}", "cs"), ("q2",))
                    tt("dve", vim[:], q1[:], q2[:], ALU.add, ("q1", "q2"), ("vim",))
                    yb = ps[4 + (Q - 2)]
                    yk = f"ps{4 + (Q - 2)}"
                    mm(yb[:, :], cpad[:, 0, pi, :], vre[:], j == 0, False, ("cpad", "vre"), (yk,))
                    mm(yb[:, :], cpad[:, 1, pi, :], vim[:], False, j == 3, ("cpad", "vim"), (yk,))
                    if j == 3:
                        tok = slice((Q - 2) * 512, (Q - 1) * 512)
                        stt("dve", yt[:], uTa[:, kt, T + (Q - 2) * 512:T + (Q - 1) * 512], dsk[:, kt:kt + 1], yb[:, :],
                            ALU.mult, ALU.add, ("uTa", "dsk", yk), ("yt",))
                        act(y2[:], yt[:], AF.Square, ("yt",), ("y2",))
                        ts("dve", y2[:], y2[:], 0.044715, 1.0, ALU.mult, ALU.add, ("y2",), ("y2",))
                        tt("dve", y2[:], y2[:], yt[:], ALU.mult, ("y2", "yt"), ("y2",))
                        act(y3[:], y2[:], AF.Sigmoid, ("y2",), ("y3",), scale=2.0 * math.sqrt(2.0 / math.pi))
                        tt("dve", ygT[:, kt, tok], yt[:], y3[:], ALU.mult, ("yt", "y3"), ("ygT",))
                n += 1
        P.barrier()
        wl = w_in[l]
        for g in range(2):
            s = load_w(wl[:, C_ZS + g * 512:C_ZS + (g + 1) * 512], 16, 512)
            for ct in range(4):
                kt = g * 4 + ct
                proj_fm(s, ct, 16, hT, "hT",
                        lambda b, th, kt=kt: act(szT[:, kt, th * 512:(th + 1) * 512], ps[b][:, :], AF.Silu,
                                                 (f"ps{b}",), ("szT",)))
        bg = SB(f"s_bg{l}", [128, 8], F32, at=W0 + 8192 + 256)
        load("sp", bg[:], b_glu[l].rearrange("(k p) -> p k", p=128), "pbg", (), ("bg",))
        gl = SB(f"s_gl{l}", [128, 512], BF16, at=W0 + 8192 + 512)
        for g in range(2):
            s = load_w(w_glu[l][:, g * 512:(g + 1) * 512], 8, 512)
            for ct in range(4):
                kt = g * 4 + ct
                def ev(b, th, kt=kt):
                    act(gl[:], ps[b][:, :], AF.Sigmoid, (f"ps{b}", "bg"), ("gl",), bias=bg[:, kt:kt + 1])
                    tt("dve", gl[:], gl[:], ygT[:, kt, th * 512:(th + 1) * 512], ALU.mult, ("gl", "ygT"), ("gl",))
                    tt("dve", szT[:, kt, th * 512:(th + 1) * 512], szT[:, kt, th * 512:(th + 1) * 512], gl[:], ALU.mult,
                       ("gl", "szT"), ("szT",))
                proj_fm(s, ct, 8, ygT, "ygT", ev)
        P.barrier()

    def phase_attn(l):
        wl = w_in[l]
        U = R_U
        kTh = [SB(f"a_kT{i}_{l}", [128, TT], BF16, at=U + i * 4096) for i in range(2)]
        vh = [SB(f"a_vh{i}_{l}", [128, 16, 128], BF16, at=U + 8192 + i * 4096) for i in range(2)]
        qf = SB(f"a_qf{l}", [128, T], F32, at=U + 16384)
        qb = SB(f"a_qb{l}", [128, T], BF16, at=U + 20480)
        sza = SB(f"a_sza{l}", [128, T], BF16, at=U + 22528)
        pT = [SB(f"a_pT{i}_{l}", [128, 256], BF16, at=U + 24576 + i * 512) for i in range(4)]
        mbT = SB(f"a_mbT{l}", [8, T], BF16, at=U + 26624)
        kmT = SB(f"a_kmT{l}", [128, 16, 8], F32, at=U + 28672)
        gsb = SB(f"a_g{l}", [128, 8, 8], F32, at=U + 29184)
        vb = SB(f"a_vb{l}", [128, 8, 8], F32, at=U + 29440)
        top8 = SB(f"a_top{l}", [128, 8, 8], F32, at=U + 29696)
        thr = SB(f"a_thr{l}", [128, 8], F32, at=U + 29952)
        mb = SB(f"a_mb{l}", [128, 8, 8], F32, at=U + 30016)
        rden = SB(f"a_rden{l}", [128, 256], F32, at=U + 30272)
        otmp = SB(f"a_otmp{l}", [128, 256], F32, at=U + 31296)
        cp("dve", vb[:], vbl[:, :].rearrange("p (i n) -> p i n", n=8), ("vbl",), ("vb",))
        ts("dve", vb[:, :, 0:4], vb[:, :, 0:4], flags[:, 2:3], None, ALU.add, None, ("vb", "flags"), ("vb",))
        scale = 1.0 / math.sqrt(128.0)
        pn = [0]
        nbanks[0] = 2
        for h in range(NH):
            g, ct = h // 4, h % 4
            if ct == 0:
                sq = load_w(wl[:, C_Q + g * 512:C_Q + (g + 1) * 512], 16, 512)
                sz = load_w(wl[:, C_ZA + g * 512:C_ZA + (g + 1) * 512], 16, 512)
            kt_, vt_ = kTh[h % 2], vh[h % 2]
            kk, vk = f"kTh{h % 2}", f"vh{h % 2}"
            load("sp", kt_[:, 0:T], gk_rows(gout_c, h), kk + "d", ("gout_k",), (kk,))
            load("sp", kt_[:, T:TT], gk_rows(gin_c, h), kk + "d", ("gin_k",), (kk,))
            for c2 in range(2):
                load("sp", vt_[:, c2 * 4:c2 * 4 + 4, :], gout_vc[c2][:, h * 128:(h + 1) * 128].rearrange("(i p) c -> p i c", p=128),
                     vk + "d", ("gout_v",), (vk,))
                load("sp", vt_[:, 8 + c2 * 4:12 + c2 * 4, :], gin_vc[c2][:, h * 128:(h + 1) * 128].rearrange("(i p) c -> p i c", p=128),
                     vk + "d", ("gin_v",), (vk,))
            proj_fm(sq, ct, 16, hT, "hT",
                    lambda b, th: cp("act", qf[:, th * 512:(th + 1) * 512], ps[b][:, :], (f"ps{b}",), ("qff",)))
            rope(qf, qb, "qf")
            proj_fm(sz, ct, 16, hT, "hT",
                    lambda b, th: act(sza[:, th * 512:(th + 1) * 512], ps[b][:, :], AF.Silu, (f"ps{b}",), ("sza",)))
            P.op("dve", lambda e, kt_=kt_, h=h: e.tensor_reduce(kmT[:, h, :], kt_[:, :].rearrange("p (n j) -> p n j", n=8),
                                                               AX.X, ALU.add), (kk,), ("kmT",))
            for i in range(8):
                mm(ps[7][:, i * 8:(i + 1) * 8], qf[:, i * 128:(i + 1) * 128], kmT[:, h, :], True, True, ("qff", "kmT"), ("ps7",))
            tt("dve", gsb[:], ps[7][:, 0:64].rearrange("p (i n) -> p i n", n=8), vb[:], ALU.add, ("ps7", "vb"), ("gsb",))
            for i in range(8):
                P.op("dve", lambda e, i=i: e.max(top8[:, i, :], gsb[:, i, :]), ("gsb",), ("top8",))
            ts("dve", thr[:], top8[:, :, 2], -1.0e29, None, ALU.max, None, ("top8",), ("thr",))
            tt("dve", mb[:], gsb[:], thr[:].unsqueeze(2).to_broadcast([128, 8, 8]), ALU.is_ge, ("gsb", "thr"), ("mb",))
            ts("dve", mb[:], mb[:], -1.0, BIG / scale, ALU.add, ALU.mult, ("mb",), ("mb",))
            for hf in range(2):
                for ii in range(4):
                    i = hf * 4 + ii
                    P.op("pe", lambda e, i=i, ii=ii: e.transpose(ps[6][0:8, ii * 128:(ii + 1) * 128], mb[:, i, :], identf[:]),
                         ("mb", "identf"), ("ps6",))
                cp("act", mbT[:, hf * 512:(hf + 1) * 512], ps[6][0:8, :], ("ps6",), ("mbT",))
            for jb in range(4):
                qs = slice(jb * 256, (jb + 1) * 256)
                oT = ps[2][:, 0:256]
                dn = ps[3][:, 0:256]
                ok_, dk_ = "ps2", "ps3"
                tiles = [(n, a) for n in range(4 + jb + 1) for a in range(2)]
                for idx, (n, a) in enumerate(tiles):
                    kt = n * 2 + a
                    own = (n == 4 + jb)
                    first, last = idx == 0, idx == len(tiles) - 1
                    q0 = 128 if (own and a == 1) else 0
                    nq = 256 - q0
                    sp_ = ps[4 + pn[0] % 2][:, 0:nq]
                    sk = f"ps{4 + pn[0] % 2}"
                    pt = pT[pn[0] % 4]
                    pk = f"pT{pn[0] % 4}"
                    pn[0] += 1
                    mm(sp_, kt_[:, kt * 128:(kt + 1) * 128], qb[:, jb * 256 + q0:(jb + 1) * 256], True, own, (kk, "qfb"), (sk,))
                    if not own:
                        mm(sp_, selm[0:8, n * 128:(n + 1) * 128], mbT[0:8, jb * 256 + q0:(jb + 1) * 256], False, True,
                           ("selm", "mbT"), (sk,))
                    act(pt[:, 0:nq], sp_, AF.Exp, (sk,), (pk,), scale=scale)
                    if own:
                        tt("pool", pt[:, 0:128], pt[:, 0:128], trib[:], ALU.mult, (pk, "trib"), (pk,))
                    mm(oT[:, q0:256], vt_[:, kt, :], pt[:, 0:nq], first, last, (vk, pk), (ok_,))
                    mm(dn[:, q0:256], onesb[:], pt[:, 0:nq], first, last, ("onesb", pk), (dk_,))
                P.op("dve", lambda e, dn=dn: e.reciprocal(rden[:], dn), (dk_,), ("rden",))
                tt("dve", otmp[:], oT, rden[:], ALU.mult, (ok_, "rden"), ("otmp",))
                tt("dve", oaT[:, h, qs], otmp[:], sza[:, qs], ALU.mult, ("otmp", "sza"), ("oaT",))
        nbanks[0] = 4
        P.barrier()

    def phase_merge(l):
        wl = w_in[l]
        mgT = uTa[:, :, :].rearrange("p k (a t) -> p (k a) t", a=2)
        sg = [SB(f"m_sg{i}_{l}", [128, 4, T], BF16, at=W0 + i * 8192) for i in range(2)]
        m1 = SB(f"m_m1{l}", [128, 512], F32, at=W0 + 16384)
        for g in range(4):
            for which, c0 in ((0, C_GS), (1, C_GA)):
                s = load_w(wl[:, c0 + g * 512:c0 + (g + 1) * 512], 16, 512)
                for ct in range(4):
                    proj_fm(s, ct, 16, hT, "hT",
                            lambda b, th, ct=ct, which=which: act(sg[which][:, ct, th * 512:(th + 1) * 512], ps[b][:, :], AF.Sigmoid,
                                                                  (f"ps{b}",), (f"sg{which}",)))
            s = load_w(w_br_ssm[l][:, g * 512:(g + 1) * 512], 8, 512)
            for ct in range(4):
                proj_fm(s, ct, 8, szT, "szT",
                        lambda b, th, ct=ct: tt("dve", mgT[:, g * 4 + ct, th * 512:(th + 1) * 512], ps[b][:, :],
                                                sg[0][:, ct, th * 512:(th + 1) * 512], ALU.mult, (f"ps{b}", "sg0"), ("mgT",)))
            s = load_w(w_br_attn[l][:, g * 512:(g + 1) * 512], 16, 512)
            for ct in range(4):
                def ev(b, th, ct=ct):
                    dst = mgT[:, g * 4 + ct, th * 512:(th + 1) * 512]
                    tt("dve", m1[:], ps[b][:, :], sg[1][:, ct, th * 512:(th + 1) * 512], ALU.mult, (f"ps{b}", "sg1"), ("m1",))
                    tt("dve", dst, dst, m1[:], ALU.add, ("m1", "mgT"), ("mgT",))
                proj_fm(s, ct, 16, oaT, "oaT", ev)
        P.barrier()
        return mgT

    def phase_out(l, mgT, x_src, x_dst):
        wo = hoa[:, :, :, :].rearrange("p a k (n c) -> p (a k n) c", c=512)
        wsrc = w_out[l].rearrange("(k p) (n c) -> p k n c", p=128, c=512)
        for k in range(16):
            P.dma("pool", lambda e, k=k: e.dma_start(out=wo[:, k * 4:(k + 1) * 4, :], in_=wsrc[:, k, :, :]), "wo", (), ("wo",))
        gainb = SB(f"o_gain{l}", [128, D], F32, at=R_SZ)
        xt = SB(f"o_xt{l}", [128, D], F32, at=R_SZ + 8192)
        junk = SB(f"o_junk{l}", [128, 512], BF16, at=W0)
        st = SB(f"o_st{l}", [128, 8], F32, at=W0 + 1024)
        tmp = SB(f"o_tmp{l}", [128, 512], F32, at=W0 + 2048)
        load("sp", gainb[:], post_norm[l:l + 1, :].to_broadcast([128, D]), "ogain", (), ("gainb",))
        for i in range(8):
            load("sp", xt[:], x_src[i * 128:(i + 1) * 128, :], "oxt", (), ("xt",))
            for n4 in range(4):
                for k in range(16):
                    mm(ps[n4][:, :], mgT[:, k, i * 128:(i + 1) * 128], wo[:, k * 4 + n4, :], k == 0, k == 15, ("mgT", "wo"), (f"ps{n4}",))
                act(junk[:], ps[n4][:, :], AF.Square, (f"ps{n4}",), ("ojunk", "ost"), accum=st[:, n4:n4 + 1])
            P.op("dve", lambda e: e.tensor_reduce(st[:, 4:5], st[:, 0:4], AX.X, ALU.add), ("ost",), ("ost",))
            ts("dve", st[:, 5:6], st[:, 4:5], 1.0 / D, 1e-6, ALU.mult, ALU.add, ("ost",), ("ost",))
            act(st[:, 6:7], st[:, 5:6], AF.Sqrt, ("ost",), ("ost",))
            P.op("dve", lambda e: e.reciprocal(st[:, 7:8], st[:, 6:7]), ("ost",), ("ost",))
            for n4 in range(4):
                cs_ = slice(n4 * 512, (n4 + 1) * 512)
                stt("dve", tmp[:], ps[n4][:, :], st[:, 7:8], gainb[:, cs_], ALU.mult, ALU.mult, (f"ps{n4}", "ost", "gainb"), ("otmp2",))
                tt("dve", xt[:, cs_], xt[:, cs_], tmp[:], ALU.add, ("otmp2", "xt"), ("xt",))
            load("sp", x_dst[i * 128:(i + 1) * 128, :], xt[:], "oxo", ("xt",), ("xdst",))
        P.barrier()

    def dump(name, ap, shape, dt):
        o = nc.dram_tensor("d_" + name, list(shape), dt, kind="ExternalOutput").ap()
        load("sp", o, ap, "dump", (), ("dump_" + name,))

    def program():
        for l in range(DEPTH):
            x_src = x_in if l == 0 else x_mid
            x_dst = x_mid if l == 0 else y_out
            phase_norm(l, x_src)
            P.barrier()
            if upto == f"norm{l}":
                dump("hT", hT, [128, 16, T], BF16)
                dump("ropeT", ropeT[:], [32, 2, T], F32)
                return
            pstop = upto[3:] if (upto or "").startswith(f"p1{l}") and len(upto) > 3 else None
            phase_p1(l, pstop)
            P.barrier()
            if (upto or "").startswith(f"p1{l}"):
                dump("uTa", uTa[:, :, T:TT], [128, 8, T], BF16)
                if pstop != "a":
                    dump("gin_k", gin_c[1], [1024, T], BF16)
                if pstop not in ("a", "b"):
                    dump("gin_v", gin_vc[0], [512, D], BF16)
                if pstop is None:
                    dump("gout_k", gout_c[2], [2048, T], BF16)
                return
            st = phase_ssm(l)
            if upto == f"ssmprep{l}":
                dump("prm", st[3][:], [128, 16, 32], F32)
                dump("bbT", st[2][:], [128, 2, 32, 128], BF16)
                dump("cpad", st[1][:], [128, 2, 32, 128], BF16)
                dump("uTa", uTa[:], [128, 8, TT], BF16)
                return
            phase_ssm_main(l, st)
            if upto == f"ssm{l}":
                dump("ygT", st[0][:], [128, 8, T], BF16)
                dump("osT", szT[:], [128, 8, T], BF16)
                return
            phase_attn(l)
            if upto == f"attn{l}":
                dump("oaT", oaT, [128, 16, T], BF16)
                dump("osT", szT[:], [128, 8, T], BF16)
                return
            mgT = phase_merge(l)
            if upto == f"merge{l}":
                dump("mgT", mgT, [128, 16, T], BF16)
                return
            phase_out(l, mgT, x_src, x_dst)
            if upto == f"out{l}":
                dump("x_mid", x_mid, [T, D], F32)
                return
    program()
    P.barrier()

    import contextlib
    with contextlib.ExitStack() as es:
        sems = {}
        for k in list(Plan.ENG) + P.slots:
            sems[k] = es.enter_context(nc.semaphore("s_" + k))
        es.enter_context(nc.allow_non_contiguous_dma(reason="small strided parameter loads"))
        block = es.enter_context(nc.Block())

        refd = {k: set() for k in Plan.ENG}
        for e in Plan.ENG:
            for o in P.ops[e]:
                if o[0] == "wait" and o[1] in refd:
                    refd[o[1]].add(o[2])
        for k in Plan.ENG:
            if P.cnt.get(k, 0) > 0:
                refd[k].add(P.cnt[k])
        newc = {k: {c: i + 1 for i, c in enumerate(sorted(v))} for k, v in refd.items()}

        def emit(eng_name):
            def f(eng):
                n = 0
                for o in P.ops[eng_name]:
                    if o[0] == "wait":
                        v = newc[o[1]][o[2]] if o[1] in newc else o[2]
                        eng.wait_ge(sems[o[1]], v)
                    elif o[2] in newc:
                        n += 1
                        ins = o[1](eng)
                        if n in refd[o[2]]:
                            ins.then_inc(sems[o[2]], 1)
                    else:
                        o[1](eng).then_inc(sems[o[2]], o[3])
                if eng_name == "sp":
                    for k, c in P.cnt.items():
                        eng.wait_ge(sems[k], newc[k][c] if k in newc else c)
            return f
        block.tensor(emit("pe"))
        block.scalar(emit("act"))
        block.vector(emit("dve"))
        block.gpsimd(emit("pool"))
        block.sync(emit("sp"))
    return nc


_NC = {}


def run(inputs, n_cores=8, upto=None):
    bf = ml_dtypes.bfloat16
    x = np.ascontiguousarray(inputs["x"], dtype=np.float32)
    identf = np.eye(128, dtype=np.float32)
    perm = np.zeros((128, 128), np.float32)
    for m in range(16):
        perm[m + 16, m] = -1.0
        perm[m, m + 16] = 1.0
    tri = np.triu(np.ones((128, 128), np.float32))
    ones = np.ones((128, 128), np.float32)
    selm = np.zeros((8, 8, 128), np.float32)
    for n in range(8):
        selm[n, n, :] = 1.0
    tpos = np.tile(np.arange(TT, dtype=np.float32)[None, :], (128, 1))
    vbl = np.zeros((128, 8, 8), np.float32)
    for i in range(8):
        for n in range(4, 8):
            if (n - 4) >= i // 2:
                vbl[:, i, n] = NEG
    ropei = np.zeros((128, 1), np.float32)
    ropei[:32, 0] = np.arange(32) % 16
    consts = {"c_identf": identf, "c_perm": perm.astype(bf), "c_tri": tri.astype(bf), "c_ones": ones.astype(bf),
              "c_selm": selm.reshape(8, 1024).astype(bf), "c_tpos": tpos, "c_vbl": vbl.reshape(128, 64), "c_ropei": ropei}
    wnames = ["pre_norm", "post_norm", "w_in", "lam_re", "lam_im", "log_dt", "b_re", "b_im", "c_re", "c_im",
              "d_skip", "w_glu", "b_glu", "w_br_ssm", "w_br_attn", "w_out"]
    shared = {k: np.ascontiguousarray(inputs[k], dtype=np.float32) for k in wnames}
    shared.update(consts)
    in_maps = []
    for c in range(n_cores):
        b, s = c // 2, c % 2
        fl = np.zeros((128, 4), np.float32)
        fl[:, 0] = float(s)
        fl[:, 1] = float(s * T)
        fl[:, 2] = (float(s) - 1.0) * 1.0e30
        m = dict(shared)
        m["x"] = np.ascontiguousarray(x[b, s * T:(s + 1) * T, :])
        m["c_flags"] = fl
        in_maps.append(m)
    key = (n_cores, upto)
    if key not in _NC:
        _NC[key] = build_nc(n_cores, upto)
    res = run_bass_kernel_spmd(_NC[key], in_maps, core_ids=list(range(n_cores)))
    return res.results


def kernel(**inputs):
    results = run(inputs, 8, None)
    out = np.zeros((4, 2048, D), np.float32)
    for c in range(8):
        b, s = c // 2, c % 2
        out[b, s * T:(s + 1) * T, :] = results[c]["y"]
    return out
```

```python
import math
import numpy as np
import ml_dtypes
import concourse.bass as bass
import concourse.mybir as mybir
from concourse.bass_utils import run_bass_kernel_spmd

F32 = mybir.dt.float32
BF16 = mybir.dt.bfloat16
ALU = mybir.AluOpType
AF = mybir.ActivationFunctionType
AX = mybir.AxisListType

D = 2048
T = 1024
TT = 2048
NH = 16
DEPTH = 2
INW = 14336
C_U, C_ZS, C_Q, C_K, C_V, C_ZA, C_GS, C_GA = 0, 1024, 2048, 4096, 6144, 8192, 10240, 12288
MAGIC = 12582912.0
TWO_PI = 2.0 * math.pi
BIG = 30000.0
NEG = -1.0e30


class Plan:
    ENG = ("pe", "act", "dve", "pool", "sp")

    def __init__(self):
        self.ops = {e: [] for e in self.ENG}
        self.cnt = {}
        self.known = {e: {} for e in self.ENG}
        self.res = {}
        self.slots = []

    def _deps(self, reads, writes):
        deps = {}

        def add(d):
            if d is None:
                return
            k, c = d
            if deps.get(k, 0) < c:
                deps[k] = c
        for k in reads:
            r = self.res.get(k)
            if r:
                add(r[0])
        for k in writes:
            r = self.res.get(k)
            if r:
                add(r[0])
                for kk, cc in r[1].items():
                    add((kk, cc))
        return deps

    def _waits(self, eng, deps, skip_self=False):
        for k, c in deps.items():
            if skip_self and k == eng:
                continue
            if self.known[eng].get(k, 0) >= c:
                continue
            self.known[eng][k] = c
            self.ops[eng].append(("wait", k, c))

    def _update(self, reads, writes, prod):
        for k in reads:
            r = self.res.setdefault(k, [None, {}])
            if r[1].get(prod[0], 0) < prod[1]:
                r[1][prod[0]] = prod[1]
        for k in writes:
            self.res[k] = [prod, {}]

    def op(self, eng, fn, reads=(), writes=()):
        deps = self._deps(reads, writes)
        self._waits(eng, deps, skip_self=(eng == "pe"))
        c = self.cnt.get(eng, 0) + 1
        self.cnt[eng] = c
        self.ops[eng].append(("op", fn, eng, 1))
        self._update(reads, writes, (eng, c))

    def dma(self, q, fn, slot, reads=(), writes=(), inc=16):
        deps = self._deps(reads, writes)
        self._waits(q, deps)
        if slot not in self.slots:
            self.slots.append(slot)
        c = self.cnt.get(slot, 0) + inc
        self.cnt[slot] = c
        self.ops[q].append(("op", fn, slot, inc))
        self._update(reads, writes, (slot, c))

    def barrier(self):
        allk = dict(self.cnt)
        for e in self.ENG:
            self._waits(e, allk, skip_self=True)
        self.res = {}


def build_nc(n_cores=8, upto=None):
    nc = bass.Bass("TRN2", target_bir_lowering=False)
    P = Plan()

    def din(name, shape, dt=F32):
        return nc.dram_tensor(name, list(shape), dt, kind="ExternalInput").ap()

    x_in = din("x", [T, D])
    pre_norm = din("pre_norm", [DEPTH, D])
    post_norm = din("post_norm", [DEPTH, D])
    w_in = din("w_in", [DEPTH, D, INW])
    lam_re = din("lam_re", [DEPTH, 64, 64])
    lam_im = din("lam_im", [DEPTH, 64, 64])
    log_dt = din("log_dt", [DEPTH, 64])
    b_re = din("b_re", [DEPTH, 64, 64, 16])
    b_im = din("b_im", [DEPTH, 64, 64, 16])
    c_re = din("c_re", [DEPTH, 64, 16, 64])
    c_im = din("c_im", [DEPTH, 64, 16, 64])
    d_skip = din("d_skip", [DEPTH, 1024])
    w_glu = din("w_glu", [DEPTH, 1024, 1024])
    b_glu = din("b_glu", [DEPTH, 1024])
    w_br_ssm = din("w_br_ssm", [DEPTH, 1024, D])
    w_br_attn = din("w_br_attn", [DEPTH, D, D])
    w_out = din("w_out", [DEPTH, D, D])
    c_identf = din("c_identf", [128, 128])
    c_perm = din("c_perm", [128, 128], BF16)
    c_tri = din("c_tri", [128, 128], BF16)
    c_ones = din("c_ones", [128, 128], BF16)
    c_selm = din("c_selm", [8, 1024], BF16)
    c_tpos = din("c_tpos", [128, TT])
    c_vbl = din("c_vbl", [128, 64])
    c_ropei = din("c_ropei", [128, 1])
    c_flags = din("c_flags", [128, 4])
    y_out = nc.dram_tensor("y", [T, D], F32, kind="ExternalOutput").ap()

    def dint(name, shape, dt):
        return nc.dram_tensor(name, list(shape), dt).ap()
    x_mid = dint("x_mid", [T, D], F32)
    gin_c = [dint(f"gin{i}", [1024, T], BF16) for i in range(5)]
    gout_c = [dint(f"gout{i}", [2048, T], BF16) for i in range(5)]
    gin_u = gin_c[0]
    gout_u = gout_c[0]
    gin_vc = [gin_c[3 + c].rearrange("(t two) c -> t (two c)", two=2) for c in range(2)]
    gout_vc = [gout_c[3 + c][0:1024, :].rearrange("(t two) c -> t (two c)", two=2) for c in range(2)]

    def gk_rows(gl, h):
        return gl[1 + h // 8][(h % 8) * 128:(h % 8 + 1) * 128, :]

    off = [0]

    arena = nc.alloc_sbuf_tensor_at("arena", [128, 103 * 1024], BF16, offset=16640)

    def SB(name, shape, dt, at=None):
        esz = 4 if dt == F32 else 2
        nbytes = int(np.prod(shape[1:])) * esz
        nb = (nbytes + 63) // 64 * 64
        if at is None:
            at = off[0]
            off[0] += nb
        assert at % 4 == 0 and at + nb <= 206 * 1024, (name, at, nb)
        ap = arena[0:shape[0], at // 2:(at + nbytes) // 2]
        if dt == F32:
            ap = ap.bitcast(F32)
        if len(shape) == 3:
            ap = ap.rearrange("p (a b) -> p a b", a=shape[1])
        elif len(shape) == 4:
            ap = ap.rearrange("p (a b c) -> p a b c", a=shape[1], b=shape[2])
        return ap

    hoa = SB("hoa", [128, 2, 16, T], BF16)
    hT = hoa[:, 0]
    oaT = hoa[:, 1]
    OA0 = 32768
    uTa = SB("uTa", [128, 8, TT], BF16)
    szT = SB("szT", [128, 8, T], BF16)
    wbuf = SB("wbuf", [128, 2, 16, 512], BF16)
    identf = SB("identf", [128, 128], F32)
    permb = SB("permb", [128, 128], BF16)
    trib = SB("trib", [128, 128], BF16)
    onesb = SB("onesb", [128, 128], BF16)
    selm = SB("selm", [8, 1024], BF16)
    vbl = SB("vbl", [128, 64], F32)
    flags = SB("flags", [128, 4], F32)
    ropei = SB("ropei", [128, 1], F32)
    ropeT = SB("ropeT", [32, 2, T], F32)
    small = SB("small", [128, 64], F32)
    tpos_sb = SB("tpos_sb", [128, TT], F32)
    W0 = off[0]
    R_HT, R_OA, R_U, R_SZ = 0, 32768, 65536, 98304

    ps = [nc.alloc_psum_tensor(f"ps{i}", [128, 512], F32) for i in range(8)]

    def load(q, out_ap, in_ap, slot, reads=(), writes=()):
        P.dma(q, lambda e: e.dma_start(out=out_ap, in_=in_ap), slot, reads, writes)

    wslot_n = [0]

    def load_w(src_ap, kch, ncols):
        s = wslot_n[0] % 2
        wslot_n[0] += 1
        src = src_ap.rearrange("(k p) c -> p k c", p=128)
        for k0 in range(0, kch, 4):
            k1 = min(kch, k0 + 4)
            P.dma("pool", lambda e, d=wbuf[:, s, k0:k1, 0:ncols], sr=src[:, k0:k1, :]: e.dma_start(out=d, in_=sr),
                  f"w{s}", reads=(), writes=(f"wbuf{s}",))
        return s

    def mm(out_ap, lhsT, rhs, start, stop, reads, writes, tp=None):
        if tp is None:
            P.op("pe", lambda e: e.matmul(out_ap, lhsT, rhs, start=start, stop=stop), reads, writes)
        else:
            P.op("pe", lambda e: e.matmul(out_ap, lhsT, rhs, start=start, stop=stop, tile_position=tp),
                 reads, writes)

    def act(out_ap, in_ap, func, reads, writes, scale=1.0, bias=0.0, accum=None):
        if accum is None:
            P.op("act", lambda e: e.activation(out_ap, in_ap, func, bias=bias, scale=scale), reads, writes)
        else:
            P.op("act", lambda e: e.activation(out_ap, in_ap, func, bias=bias, scale=scale, accum_out=accum),
                 reads, writes)

    def tt(eng, out_ap, a, b, op, reads, writes):
        P.op(eng, lambda e: e.tensor_tensor(out_ap, a, b, op), reads, writes)

    def ts(eng, out_ap, a, s1, s2, op0, op1, reads, writes):
        if op1 is None:
            P.op(eng, lambda e: e.tensor_scalar(out_ap, a, s1, None, op0), reads, writes)
        else:
            P.op(eng, lambda e: e.tensor_scalar(out_ap, a, s1, s2, op0, op1), reads, writes)

    def stt(eng, out_ap, a, s, b, op0, op1, reads, writes):
        P.op(eng, lambda e: e.scalar_tensor_tensor(out_ap, a, s, b, op0, op1), reads, writes)

    def cp(eng, out_ap, in_ap, reads, writes):
        if eng == "act":
            P.op("act", lambda e: e.copy(out_ap, in_ap), reads, writes)
        else:
            P.op(eng, lambda e: e.tensor_copy(out_ap, in_ap), reads, writes)

    def fracs(eng, turns, tmp, keys):
        ts(eng, tmp, turns, MAGIC, -MAGIC, ALU.add, ALU.add, keys, keys)
        tt(eng, turns, turns, tmp, ALU.subtract, keys, keys)

    bankn = [0]
    nbanks = [4]

    def nb4():
        b = bankn[0] % nbanks[0]
        bankn[0] += 1
        return b

    for dst, src, nm in ((identf, c_identf, "identf"), (permb, c_perm, "permb"),
                         (trib, c_tri, "trib"), (onesb, c_ones, "onesb"), (selm, c_selm, "selm"),
                         (vbl, c_vbl, "vbl"), (flags, c_flags, "flags"), (ropei, c_ropei, "ropei"),
                         (tpos_sb, c_tpos, "tpos")):
        load("sp", dst[:], src, "c_" + nm, (), (nm,))
    rt1 = SB("rt1", [32, T], F32, at=R_OA)
    rt2 = SB("rt2", [32, T], F32, at=R_OA + 4096)
    act(small[0:32, 0:1], ropei[0:32, :], AF.Exp, ("ropei",), ("small",),
        scale=-math.log(500000.0) / 16.0, bias=-math.log(TWO_PI))
    ts("dve", rt1[:], tpos_sb[0:32, 0:T], flags[0:32, 1:2], small[0:32, 0:1], ALU.add, ALU.mult,
       ("tpos", "flags", "small"), ("rt",))
    fracs("dve", rt1[:], rt2[:], ("rt",))
    act(ropeT[:, 1, :], rt1[:], AF.Sin, ("rt",), ("ropeT",), scale=TWO_PI)
    act(rt2[:], rt1[:], AF.Abs, ("rt",), ("rt",))
    act(ropeT[:, 0, :], rt2[:], AF.Sin, ("rt",), ("ropeT",), scale=-TWO_PI, bias=math.pi / 2)
    P.barrier()

    def phase_norm(l, x_src):
        xt = [SB(f"n_xt{i}_{l}", [128, D], F32, at=R_U + i * 8192) for i in range(2)]
        hb = SB(f"n_hb_{l}", [128, D], F32, at=R_U + 16384)
        gainb = SB(f"n_gain_{l}", [128, D], F32, at=R_U + 24576)
        junk = SB(f"n_junk_{l}", [128, D], BF16, at=R_OA)
        st = SB(f"n_st_{l}", [128, 8], F32, at=R_OA + 4096)
        load("sp", gainb[:], pre_norm[l:l + 1, :].to_broadcast([128, D]), "gain", (), ("gainb",))
        for i in range(8):
            xb = xt[i % 2]
            xk = f"xt{i % 2}"
            load("sp", xb[:], x_src[i * 128:(i + 1) * 128, :], xk, (), (xk,))
            act(junk[:], xb[:], AF.Square, (xk,), ("junk", "st"), accum=st[:, 0:1])
            ts("dve", st[:, 1:2], st[:, 0:1], 1.0 / D, 1e-6, ALU.mult, ALU.add, ("st",), ("st",))
            act(st[:, 2:3], st[:, 1:2], AF.Sqrt, ("st",), ("st",))
            P.op("dve", lambda e: e.reciprocal(st[:, 3:4], st[:, 2:3]), ("st",), ("st",))
            stt("dve", hb[:], xb[:], st[:, 3:4], gainb[:], ALU.mult, ALU.mult, (xk, "st", "gainb"), ("hb",))
            for g4 in range(4):
                b = nb4()
                for j in range(4):
                    k = g4 * 4 + j
                    P.op("pe", lambda e, o=ps[b][:, j * 128:(j + 1) * 128], k=k: e.transpose(o, hb[:, k * 128:(k + 1) * 128], identf[:]),
                         ("hb", "identf"), (f"ps{b}",))
                cp("act" if g4 % 2 == 0 else "dve", hT[:, g4 * 4:(g4 + 1) * 4, i * 128:(i + 1) * 128],
                   ps[b][:, :].rearrange("p (j t) -> p j t", j=4), (f"ps{b}",), ("hT",))

    def proj_fm(s, ct, kch, rhsT, rkey, evac):
        for th in range(2):
            b = nb4()
            for k in range(kch):
                mm(ps[b][:, :], wbuf[:, s, k, ct * 128:(ct + 1) * 128], rhsT[:, k, th * 512:(th + 1) * 512],
                   k == 0, k == kch - 1, (f"wbuf{s}", rkey), (f"ps{b}",))
            evac(b, th)

    def rope(qf, qb, key):
        r1 = SB("rp_r1" + key, [32, 512], F32, at=W0)
        r2 = SB("rp_r2" + key, [32, 512], F32, at=W0 + 2048)
        cp("act", qb[:], qf[:], (key + "f",), (key + "b",))
        for th in range(2):
            sl = slice(th * 512, (th + 1) * 512)
            mm(ps[7][:, :], permb[:], qb[:, sl], True, True, ("permb", key + "b"), ("ps7",))
            tt("dve", r1[:], qf[0:32, sl], ropeT[:, 0, sl], ALU.mult, (key + "f", "ropeT"), ("rp1",))
            tt("dve", r2[:], ps[7][0:32, :], ropeT[:, 1, sl], ALU.mult, ("ps7", "ropeT"), ("rp2",))
            tt("dve", qf[0:32, sl], r1[:], r2[:], ALU.add, ("rp1", "rp2"), (key + "f",))
        cp("act", qb[0:32, :], qf[0:32, :], (key + "f",), (key + "b",))

    def phase_p1(l, stop=None):
        wl = w_in[l]
        kf = SB(f"p1_kf{l}", [128, T], F32, at=R_OA)
        kb = SB(f"p1_kb{l}", [128, T], BF16, at=R_OA + 4096)
        vst = [SB(f"p1_vst{i}_{l}", [128, 512], BF16, at=R_OA + 6144 + i * 1024) for i in range(2)]
        kml = SB(f"p1_kml{l}", [128, 64], F32, at=R_OA + 8192)
        for g in range(2):
            s = load_w(wl[:, C_U + g * 512:C_U + (g + 1) * 512], 16, 512)
            for ct in range(4):
                kt = g * 4 + ct
                proj_fm(s, ct, 16, hT, "hT",
                        lambda b, th, kt=kt: cp("act", uTa[:, kt, T + th * 512:T + (th + 1) * 512], ps[b][:, :],
                                                (f"ps{b}",), ("uTa",)))
        load("sp", gin_u.rearrange("(k p) t -> p k t", p=128), uTa[:, :, T:TT], "gu", ("uTa",), ("gin_u",))
        if stop == "a":
            return
        for g in range(4):
            s = load_w(wl[:, C_K + g * 512:C_K + (g + 1) * 512], 16, 512)
            for ct in range(4):
                h = g * 4 + ct
                proj_fm(s, ct, 16, hT, "hT",
                        lambda b, th: cp("act", kf[:, th * 512:(th + 1) * 512], ps[b][:, :], (f"ps{b}",), ("kff",)))
                rope(kf, kb, "kf")
                load("sp", gk_rows(gin_c, h), kb[:], "gk", ("kfb",), ("gin_k",))
        if stop == "b":
            return
        for g in range(4):
            s = load_w(wl[:, C_V + g * 512:C_V + (g + 1) * 512], 16, 512)
            for i in range(8):
                b = nb4()
                for k in range(16):
                    mm(ps[b][:, :], hT[:, k, i * 128:(i + 1) * 128], wbuf[:, s, k, 0:512], k == 0, k == 15,
                       (f"wbuf{s}", "hT"), (f"ps{b}",))
                vs = vst[i % 2]
                cp("act", vs[:], ps[b][:, :], (f"ps{b}",), (f"vst{i % 2}",))
                load("sp", gin_vc[i // 4][(i % 4) * 128:(i % 4 + 1) * 128, g * 512:(g + 1) * 512], vs[:], f"gv{i % 2}",
                     (f"vst{i % 2}",), ("gin_v",))
        if stop == "c":
            return
        for ci in range(5):
            P.dma("pool", lambda e, ci=ci: e.collective_compute(
                "AllGather", ALU.bypass, replica_groups=[[2 * i, 2 * i + 1] for i in range(n_cores // 2)],
                ins=[gin_c[ci].opt()], outs=[gout_c[ci].opt()]), f"cc{ci}", reads=("gin_u", "gin_k", "gin_v"),
                writes=("gout_u", "gout_k", "gout_v"), inc=1)

    def phase_ssm(l):
        A = R_OA
        ygT = SB(f"s_yg{l}", [128, 8, T], BF16, at=A)
        cpad = SB(f"s_cpad{l}", [128, 2, 32, 128], BF16, at=A + 16384)
        B = W0
        bbT = SB(f"s_bbT{l}", [128, 2, 32, 128], BF16, at=B + 16384)
        prm = SB(f"s_prm{l}", [128, 16, 32], F32, at=B + 4096)
        P.op("pool", lambda e: e.memset(bbT[:], 0.0), (), ("bbT",))
        wk = 114688
        load("sp", uTa[:, :, 0:T], gout_u[0:1024, :].rearrange("(k p) t -> p k t", p=128), "gul", ("gout_u",), ("uTa",))
        for kt in range(8):
            ts("dve", uTa[:, kt, 0:T], uTa[:, kt, 0:T], flags[:, 0:1], None, ALU.mult, None, ("uTa", "flags"), ("uTa",))
        LR, LI, DT, MAG, FR, AR, AI, DEN, NRM, FRE, FIM, T1, T2, T3, T4, T5 = range(16)
        pv = lambda i: prm[:, i, :]
        for two in range(2):
            load("sp", prm[two * 64:(two + 1) * 64, LR, :], lam_re[l].rearrange("(pi two) p -> two p pi", two=2)[two], "pl", (), ("prm",))
            load("sp", prm[two * 64:(two + 1) * 64, LI, :], lam_im[l].rearrange("(pi two) p -> two p pi", two=2)[two], "pl", (), ("prm",))
            load("sp", prm[two * 64:(two + 1) * 64, DT, :],
                 log_dt[l:l + 1, :].rearrange("o (pi two) -> o two pi", two=2)[:, two, :].to_broadcast([64, 32]),
                 "pl", (), ("prm",))
        K = ("prm",)
        act(pv(DT), pv(DT), AF.Exp, K, K)
        tt("dve", pv(T1), pv(LR), pv(DT), ALU.mult, K, K)
        act(pv(MAG), pv(T1), AF.Exp, K, K)
        tt("dve", pv(T2), pv(LI), pv(DT), ALU.mult, K, K)
        ts("dve", pv(FR), pv(T2), 1.0 / TWO_PI, None, ALU.mult, None, K, K)
        fracs("dve", pv(FR), pv(T3), K)
        act(pv(T4), pv(FR), AF.Sin, K, K, scale=TWO_PI)
        act(pv(T3), pv(FR), AF.Abs, K, K)
        act(pv(T5), pv(T3), AF.Sin, K, K, scale=-TWO_PI, bias=math.pi / 2)
        tt("dve", pv(AR), pv(MAG), pv(T5), ALU.mult, K, K)
        tt("dve", pv(AI), pv(MAG), pv(T4), ALU.mult, K, K)
        tt("dve", pv(T1), pv(LR), pv(LR), ALU.mult, K, K)
        tt("dve", pv(T2), pv(LI), pv(LI), ALU.mult, K, K)
        tt("dve", pv(DEN), pv(T1), pv(T2), ALU.add, K, K)
        P.op("dve", lambda e: e.reciprocal(pv(DEN), pv(DEN)), K, K)
        ts("dve", pv(NRM), pv(AR), -1.0, None, ALU.add, None, K, K)
        tt("dve", pv(T1), pv(NRM), pv(LR), ALU.mult, K, K)
        tt("dve", pv(T2), pv(AI), pv(LI), ALU.mult, K, K)
        tt("dve", pv(T1), pv(T1), pv(T2), ALU.add, K, K)
        tt("dve", pv(FRE), pv(T1), pv(DEN), ALU.mult, K, K)
        tt("dve", pv(T1), pv(AI), pv(LR), ALU.mult, K, K)
        tt("dve", pv(T2), pv(NRM), pv(LI), ALU.mult, K, K)
        tt("dve", pv(T1), pv(T1), pv(T2), ALU.subtract, K, K)
        tt("dve", pv(FIM), pv(T1), pv(DEN), ALU.mult, K, K)
        bS = [SB(f"s_bS{i}_{l}", [128, 32, 32], F32, at=wk + i * 4096) for i in range(4)]
        for i in range(2):
            P.op("dve", lambda e, t=bS[i]: e.memset(t[:], 0.0), (), (f"bS{i}",))
            src = (b_re, b_im)[i][l].rearrange("(pi two) p c -> two p pi c", two=2)
            for two in range(2):
                load("sp", bS[i][two * 64:(two + 1) * 64, :, two * 16:(two + 1) * 16], src[two], "pb", (), (f"bS{i}",))
        fre_b = pv(FRE).unsqueeze(2).to_broadcast([128, 32, 32])
        fim_b = pv(FIM).unsqueeze(2).to_broadcast([128, 32, 32])
        KB = ("bS0", "bS1", "bS2", "bS3", "prm")
        bbS = SB(f"s_bbS{l}", [128, 2, 32, 32], F32, at=wk + 16384)
        tt("dve", bS[2][:], bS[0][:], fre_b, ALU.mult, KB, KB)
        tt("dve", bS[3][:], bS[1][:], fim_b, ALU.mult, KB, KB)
        tt("dve", bbS[:, 0], bS[2][:], bS[3][:], ALU.subtract, KB, ("bbS",))
        tt("dve", bS[2][:], bS[1][:], fre_b, ALU.mult, KB, KB)
        tt("dve", bS[3][:], bS[0][:], fim_b, ALU.mult, KB, KB)
        tt("dve", bbS[:, 1], bS[2][:], bS[3][:], ALU.add, KB + ("bbS",), ("bbS",))
        for ri in range(2):
            for kt in range(8):
                P.op("pe", lambda e, ri=ri, kt=kt: e.transpose(ps[4][:, 0:128], bbS[:, ri, kt * 4:(kt + 1) * 4, :].rearrange("p a b -> p (a b)"), identf[:]),
                     ("bbS", "identf"), ("ps4",))
                for a4 in range(4):
                    cp("act", bbT[32 * a4:32 * a4 + 32, ri, kt * 4 + a4, :], ps[4][32 * a4:32 * a4 + 32, 0:128], ("ps4",), ("bbT",))
        P.op("pool", lambda e: e.memset(cpad[:], 0.0), (), ("cpad",))
        cS = [SB(f"s_cS{i}_{l}", [32, 32, 128], F32, at=R_SZ) for i in range(1)]
        for ri in range(2):
            P.op("dve", lambda e: e.memset(cS[0][:], 0.0), ("ps5",), ("cS",))
            src = (c_re, c_im)[ri][l].rearrange("(pi two) c p -> two c pi p", two=2)
            for two in range(2):
                load("sp", cS[0][two * 16:(two + 1) * 16, :, two * 64:(two + 1) * 64], src[two], "pc", (), ("cS",))
            for pi in range(32):
                P.op("pe", lambda e, pi=pi: e.transpose(ps[5][:, 0:32], cS[0][:, pi, :], identf[0:32, 0:32]),
                     ("cS", "identf"), ("ps5",))
                j = pi % 4
                act(cpad[:, ri, pi, j * 32:(j + 1) * 32], ps[5][:, 0:32], AF.Copy, ("ps5",), ("cpad",),
                    scale=(1.0 if ri == 0 else -1.0))
        dsk = SB(f"s_dsk{l}", [128, 8], F32, at=B + 6144)
        load("sp", dsk[:], d_skip[l].rearrange("(k p) -> p k", p=128), "pd", (), ("dsk",))
        P.barrier()
        return ygT, cpad, bbT, prm, dsk, None, (FR, MAG)

    def phase_ssm_main(l, st):
        ygT, cpad, bbT, prm, dsk, tl, (FR, MAG) = st
        Mb = R_SZ
        HT = 1024

        def F(name, dt, o):
            return SB(f"s_{name}{l}", [128, HT], dt, at=Mb + o)
        trn = [F("trn0", F32, 0), F("trn1", F32, 4096)]
        ttmp = F("ttmp", F32, 8192)
        af = F("af", F32, 12288)
        sn = [F("sn0", BF16, 16384), F("sn1", BF16, 18432)]
        cs = [F("cs0", BF16, 20480), F("cs1", BF16, 22528)]
        bre = [F("bre0", BF16, 24576), F("bre1", BF16, 26624)]
        bim = [F("bim0", BF16, 28672), F("bim1", BF16, 30720)]
        q1 = F("q1", BF16, 32768); q2 = F("q2", BF16, 34816); q3 = F("q3", BF16, 36864)
        xr = [F("xr0", BF16, 38912), F("xr1", BF16, 40960)]
        xi = [F("xi0", BF16, 43008), F("xi1", BF16, 45056)]
        dim = F("dim", BF16, 47104)
        yt = SB(f"s_yt{l}", [128, 512], F32, at=W0 + 32768)
        y2 = SB(f"s_y2{l}", [128, 512], F32, at=W0 + 32768 + 2048)
        y3 = SB(f"s_y3{l}", [128, 512], F32, at=W0 + 32768 + 4096)
        dec = SB(f"s_dec{l}", [128, 32], F32, at=W0 + 8192 + 128)
        cp("dve", dec[:], prm[:, MAG, :], ("prm",), ("dec",))
        its = [(pi, Hh) for pi in range(32) for Hh in range(2)]
        qn = [0]

        def frac_of(n):
            pi, Hh = its[n]
            tr, tk = trn[n % 2], f"trn{n % 2}"
            ts("dve", tr[:], tpos_sb[:, Hh * HT:(Hh + 1) * HT], prm[:, FR, pi:pi + 1], None, ALU.mult, None, ("tpos", "prm"), (tk,))
            ts("dve", ttmp[:], tr[:], MAGIC, -MAGIC, ALU.add, ALU.add, (tk,), ("ttmp",))
            tt("dve", tr[:], tr[:], ttmp[:], ALU.subtract, (tk, "ttmp"), (tk,))

        frac_of(0)
        for n, (pi, Hh) in enumerate(its):
            kt, j = pi // 4, pi % 4
            b2 = n % 2
            tr, tk = trn[b2], f"trn{b2}"
            snb, csb, breb, bimb = sn[b2], cs[b2], bre[b2], bim[b2]
            ks, kc, kbr, kbi = f"sn{b2}", f"cs{b2}", f"bre{b2}", f"bim{b2}"
            for qq in range(2):
                sl = slice(Hh * HT + qq * 512, Hh * HT + (qq + 1) * 512)
                pb = (qn[0] % 2) * 2
                qn[0] += 1
                mm(ps[pb][:, :], bbT[:, 0, pi, :], uTa[:, kt, sl], True, True, ("bbT", "uTa"), (f"ps{pb}",))
                mm(ps[pb + 1][:, :], bbT[:, 1, pi, :], uTa[:, kt, sl], True, True, ("bbT", "uTa"), (f"ps{pb + 1}",))
                cp("act", breb[:, qq * 512:(qq + 1) * 512], ps[pb][:, :], (f"ps{pb}",), (kbr,))
                cp("act", bimb[:, qq * 512:(qq + 1) * 512], ps[pb + 1][:, :], (f"ps{pb + 1}",), (kbi,))
            act(snb[:], tr[:], AF.Sin, (tk,), (ks,), scale=TWO_PI)
            act(af[:], tr[:], AF.Abs, (tk,), ("af",))
            act(csb[:], af[:], AF.Sin, ("af",), (kc,), scale=-TWO_PI, bias=math.pi / 2)
            if n + 1 < len(its):
                frac_of(n + 1)
            tt("dve", q1[:], breb[:], csb[:], ALU.mult, (kbr, kc), ("q1",))
            tt("dve", q2[:], bimb[:], snb[:], ALU.mult, (kbi, ks), ("q2",))
            tt("dve", q1[:], q1[:], q2[:], ALU.add, ("q1", "q2"), ("q1",))
            tt("dve", q3[:], bimb[:], csb[:], ALU.mult, (kbi, kc), ("q3",))
            tt("dve", q2[:], breb[:], snb[:], ALU.mult, (kbr, ks), ("q2",))
            tt("dve", q3[:], q3[:], q2[:], ALU.subtract, ("q3", "q2"), ("q3",))
            dcy = dec[:, pi:pi + 1].to_broadcast([128, HT])
            xrc, xic = xr[Hh], xi[Hh]
            if Hh == 0:
                P.op("dve", lambda e, xrc=xrc, dcy=dcy: e.tensor_tensor_scan(xrc[:], dcy, q1[:], 0.0, ALU.mult, ALU.add),
                     ("q1", "dec"), ("xr0",))
                P.op("dve", lambda e, xic=xic, dcy=dcy: e.tensor_tensor_scan(xic[:], dcy, q3[:], 0.0, ALU.mult, ALU.add),
                     ("q3", "dec"), ("xi0",))
            else:
                P.op("dve", lambda e, xrc=xrc, dcy=dcy: e.tensor_tensor_scan(xrc[:], dcy, q1[:], xr[0][:, HT - 1:HT], ALU.mult, ALU.add),
                     ("q1", "dec", "xr0"), ("xr1",))
                P.op("dve", lambda e, xic=xic, dcy=dcy: e.tensor_tensor_scan(xic[:], dcy, q3[:], xi[0][:, HT - 1:HT], ALU.mult, ALU.add),
                     ("q3", "dec", "xi0"), ("xi1",))
                tt("dve", q1[:], xrc[:], csb[:], ALU.mult, ("xr1", kc), ("q1",))
                tt("dve", q3[:], xic[:], snb[:], ALU.mult, ("xi1", ks), ("q3",))
                tt("dve", q2[:], q1[:], q3[:], ALU.subtract, ("q1", "q3"), ("q2",))
                tt("dve", q1[:], xrc[:], snb[:], ALU.mult, ("xr1", ks), ("q1",))
                tt("dve", q3[:], xic[:], csb[:], ALU.mult, ("xi1", kc), ("q3",))
                tt("dve", dim[:], q1[:], q3[:], ALU.add, ("q1", "q3"), ("dim",))
                for qq in range(2):
                    yb, yk = ps[4 + qq], f"ps{4 + qq}"
                    csl = slice(qq * 512, (qq + 1) * 512)
                    mm(yb[:, :], cpad[:, 0, pi, :], q2[:, csl], j == 0, False, ("cpad", "q2"), (yk,))
                    mm(yb[:, :], cpad[:, 1, pi, :], dim[:, csl], False, j == 3, ("cpad", "dim"), (yk,))
                    if j == 3:
                        tok = slice(qq * 512, (qq + 1) * 512)
                        stt("dve", yt[:], uTa[:, kt, T + qq * 512:T + (qq + 1) * 512], dsk[:, kt:kt + 1], yb[:, :],
                            ALU.mult, ALU.add, ("uTa", "dsk", yk), ("yt",))
                        act(y2[:], yt[:], AF.Square, ("yt",), ("y2",))
                        ts("dve", y2[:], y2[:], 0.044715, 1.0, ALU.mult, ALU.add, ("y2",), ("y2",))
                        tt("dve", y2[:], y2[:], yt[:], ALU.mult, ("y2", "yt"), ("y2",))
                        act(y3[:], y2[:], AF.Sigmoid, ("y2",), ("y3",), scale=2.0 * math.sqrt(2.0 / math.pi))
                        tt("dve", ygT[:, kt, tok], yt[:], y3[:], ALU.mult, ("yt", "y3"), ("ygT",))
        P.barrier()
        wl = w_in[l]
        for g in range(2):
            s = load_w(wl[:, C_ZS + g * 512:C_ZS + (g + 1) * 512], 16, 512)
            for ct in range(4):
                kt = g * 4 + ct
                proj_fm(s, ct, 16, hT, "hT",
                        lambda b, th, kt=kt: act(szT[:, kt, th * 512:(th + 1) * 512], ps[b][:, :], AF.Silu,
                                                 (f"ps{b}",), ("szT",)))
        bg = SB(f"s_bg{l}", [128, 8], F32, at=W0 + 8192 + 256)
        load("sp", bg[:], b_glu[l].rearrange("(k p) -> p k", p=128), "pbg", (), ("bg",))
        gl = SB(f"s_gl{l}", [128, 512], BF16, at=W0 + 8192 + 512)
        for g in range(2):
            s = load_w(w_glu[l][:, g * 512:(g + 1) * 512], 8, 512)
            for ct in range(4):
                kt = g * 4 + ct
                def ev(b, th, kt=kt):
                    act(gl[:], ps[b][:, :], AF.Sigmoid, (f"ps{b}", "bg"), ("gl",), bias=bg[:, kt:kt + 1])
                    tt("dve", gl[:], gl[:], ygT[:, kt, th * 512:(th + 1) * 512], ALU.mult, ("gl", "ygT"), ("gl",))
                    tt("dve", szT[:, kt, th * 512:(th + 1) * 512], szT[:, kt, th * 512:(th + 1) * 512], gl[:], ALU.mult,
                       ("gl", "szT"), ("szT",))
                proj_fm(s, ct, 8, ygT, "ygT", ev)
        P.barrier()

    def phase_attn(l):
        wl = w_in[l]
        U = R_U
        kTh = [SB(f"a_kT{i}_{l}", [128, TT], BF16, at=U + i * 4096) for i in range(2)]
        vh = [SB(f"a_vh{i}_{l}", [128, 16, 128], BF16, at=U + 8192 + i * 4096) for i in range(2)]
        qf = SB(f"a_qf{l}", [128, T], F32, at=U + 16384)
        qb = SB(f"a_qb{l}", [128, T], BF16, at=U + 20480)
        sza = SB(f"a_sza{l}", [128, T], BF16, at=U + 22528)
        pT = [SB(f"a_pT{i}_{l}", [128, 256], BF16, at=U + 24576 + i * 512) for i in range(4)]
        mbT = SB(f"a_mbT{l}", [8, T], BF16, at=U + 26624)
        kmT = SB(f"a_kmT{l}", [128, 16, 8], F32, at=U + 28672)
        gsb = SB(f"a_g{l}", [128, 8, 8], F32, at=U + 29184)
        vb = SB(f"a_vb{l}", [128, 8, 8], F32, at=U + 29440)
        top8 = SB(f"a_top{l}", [128, 8, 8], F32, at=U + 29696)
        thr = SB(f"a_thr{l}", [128, 8], F32, at=U + 29952)
        mb = SB(f"a_mb{l}", [128, 8, 8], F32, at=U + 30016)
        rden = SB(f"a_rden{l}", [128, 256], F32, at=U + 30272)
        otmp = SB(f"a_otmp{l}", [128, 256], F32, at=U + 31296)
        cp("dve", vb[:], vbl[:, :].rearrange("p (i n) -> p i n", n=8), ("vbl",), ("vb",))
        ts("dve", vb[:, :, 0:4], vb[:, :, 0:4], flags[:, 2:3], None, ALU.add, None, ("vb", "flags"), ("vb",))
        scale = 1.0 / math.sqrt(128.0)
        pn = [0]
        nbanks[0] = 2
        for h in range(NH):
            g, ct = h // 4, h % 4
            if ct == 0:
                sq = load_w(wl[:, C_Q + g * 512:C_Q + (g + 1) * 512], 16, 512)
                sz = load_w(wl[:, C_ZA + g * 512:C_ZA + (g + 1) * 512], 16, 512)
            kt_, vt_ = kTh[h % 2], vh[h % 2]
            kk, vk = f"kTh{h % 2}", f"vh{h % 2}"
            load("sp", kt_[:, 0:T], gk_rows(gout_c, h), kk + "d", ("gout_k",), (kk,))
            load("sp", kt_[:, T:TT], gk_rows(gin_c, h), kk + "d", ("gin_k",), (kk,))
            for c2 in range(2):
                load("sp", vt_[:, c2 * 4:c2 * 4 + 4, :], gout_vc[c2][:, h * 128:(h + 1) * 128].rearrange("(i p) c -> p i c", p=128),
                     vk + "d", ("gout_v",), (vk,))
                load("sp", vt_[:, 8 + c2 * 4:12 + c2 * 4, :], gin_vc[c2][:, h * 128:(h + 1) * 128].rearrange("(i p) c -> p i c", p=128),
                     vk + "d", ("gin_v",), (vk,))
            proj_fm(sq, ct, 16, hT, "hT",
                    lambda b, th: cp("act", qf[:, th * 512:(th + 1) * 512], ps[b][:, :], (f"ps{b}",), ("qff",)))
            rope(qf, qb, "qf")
            proj_fm(sz, ct, 16, hT, "hT",
                    lambda b, th: act(sza[:, th * 512:(th + 1) * 512], ps[b][:, :], AF.Silu, (f"ps{b}",), ("sza",)))
            P.op("dve", lambda e, kt_=kt_, h=h: e.tensor_reduce(kmT[:, h, :], kt_[:, :].rearrange("p (n j) -> p n j", n=8),
                                                               AX.X, ALU.add), (kk,), ("kmT",))
            for i in range(8):
                mm(ps[7][:, i * 8:(i + 1) * 8], qf[:, i * 128:(i + 1) * 128], kmT[:, h, :], True, True, ("qff", "kmT"), ("ps7",))
            tt("dve", gsb[:], ps[7][:, 0:64].rearrange("p (i n) -> p i n", n=8), vb[:], ALU.add, ("ps7", "vb"), ("gsb",))
            for i in range(8):
                P.op("dve", lambda e, i=i: e.max(top8[:, i, :], gsb[:, i, :]), ("gsb",), ("top8",))
            ts("dve", thr[:], top8[:, :, 2], -1.0e29, None, ALU.max, None, ("top8",), ("thr",))
            tt("dve", mb[:], gsb[:], thr[:].unsqueeze(2).to_broadcast([128, 8, 8]), ALU.is_ge, ("gsb", "thr"), ("mb",))
            ts("dve", mb[:], mb[:], -1.0, BIG / scale, ALU.add, ALU.mult, ("mb",), ("mb",))
            for hf in range(2):
                for ii in range(4):
                    i = hf * 4 + ii
                    P.op("pe", lambda e, i=i, ii=ii: e.transpose(ps[6][0:8, ii * 128:(ii + 1) * 128], mb[:, i, :], identf[:]),
                         ("mb", "identf"), ("ps6",))
                cp("act", mbT[:, hf * 512:(hf + 1) * 512], ps[6][0:8, :], ("ps6",), ("mbT",))
            for jb in range(4):
                qs = slice(jb * 256, (jb + 1) * 256)
                oT = ps[2][:, 0:256]
                dn = ps[3][:, 0:256]
                ok_, dk_ = "ps2", "ps3"
                tiles = [(n, a) for n in range(4 + jb + 1) for a in range(2)]
                for idx, (n, a) in enumerate(tiles):
                    kt = n * 2 + a
                    own = (n == 4 + jb)
                    first, last = idx == 0, idx == len(tiles) - 1
                    q0 = 128 if (own and a == 1) else 0
                    nq = 256 - q0
                    sp_ = ps[4 + pn[0] % 2][:, 0:nq]
                    sk = f"ps{4 + pn[0] % 2}"
                    pt = pT[pn[0] % 4]
                    pk = f"pT{pn[0] % 4}"
                    pn[0] += 1
                    mm(sp_, kt_[:, kt * 128:(kt + 1) * 128], qb[:, jb * 256 + q0:(jb + 1) * 256], True, own, (kk, "qfb"), (sk,))
                    if not own:
                        mm(sp_, selm[0:8, n * 128:(n + 1) * 128], mbT[0:8, jb * 256 + q0:(jb + 1) * 256], False, True,
                           ("selm", "mbT"), (sk,))
                    act(pt[:, 0:nq], sp_, AF.Exp, (sk,), (pk,), scale=scale)
                    if own:
                        tt("pool", pt[:, 0:128], pt[:, 0:128], trib[:], ALU.mult, (pk, "trib"), (pk,))
                    mm(oT[:, q0:256], vt_[:, kt, :], pt[:, 0:nq], first, last, (vk, pk), (ok_,))
                    mm(dn[:, q0:256], onesb[:], pt[:, 0:nq], first, last, ("onesb", pk), (dk_,))
                P.op("dve", lambda e, dn=dn: e.reciprocal(rden[:], dn), (dk_,), ("rden",))
                tt("dve", otmp[:], oT, rden[:], ALU.mult, (ok_, "rden"), ("otmp",))
                tt("dve", oaT[:, h, qs], otmp[:], sza[:, qs], ALU.mult, ("otmp", "sza"), ("oaT",))
        nbanks[0] = 4
        P.barrier()

    def phase_merge(l):
        wl = w_in[l]
        mgT = uTa[:, :, :].rearrange("p k (a t) -> p (k a) t", a=2)
        sg = [SB(f"m_sg{i}_{l}", [128, 4, T], BF16, at=W0 + i * 8192) for i in range(2)]
        m1 = SB(f"m_m1{l}", [128, 512], F32, at=W0 + 16384)
        for g in range(4):
            for which, c0 in ((0, C_GS), (1, C_GA)):
                s = load_w(wl[:, c0 + g * 512:c0 + (g + 1) * 512], 16, 512)
                for ct in range(4):
                    proj_fm(s, ct, 16, hT, "hT",
                            lambda b, th, ct=ct, which=which: act(sg[which][:, ct, th * 512:(th + 1) * 512], ps[b][:, :], AF.Sigmoid,
                                                                  (f"ps{b}",), (f"sg{which}",)))
            s = load_w(w_br_ssm[l][:, g * 512:(g + 1) * 512], 8, 512)
            for ct in range(4):
                proj_fm(s, ct, 8, szT, "szT",
                        lambda b, th, ct=ct: tt("dve", mgT[:, g * 4 + ct, th * 512:(th + 1) * 512], ps[b][:, :],
                                                sg[0][:, ct, th * 512:(th + 1) * 512], ALU.mult, (f"ps{b}", "sg0"), ("mgT",)))
            s = load_w(w_br_attn[l][:, g * 512:(g + 1) * 512], 16, 512)
            for ct in range(4):
                def ev(b, th, ct=ct):
                    dst = mgT[:, g * 4 + ct, th * 512:(th + 1) * 512]
                    tt("dve", m1[:], ps[b][:, :], sg[1][:, ct, th * 512:(th + 1) * 512], ALU.mult, (f"ps{b}", "sg1"), ("m1",))
                    tt("dve", dst, dst, m1[:], ALU.add, ("m1", "mgT"), ("mgT",))
                proj_fm(s, ct, 16, oaT, "oaT", ev)
        P.barrier()
        return mgT

    def phase_out(l, mgT, x_src, x_dst):
        wo = hoa[:, :, :, :].rearrange("p a k (n c) -> p (a k n) c", c=512)
        wsrc = w_out[l].rearrange("(k p) (n c) -> p k n c", p=128, c=512)
        for k in range(16):
            P.dma("pool", lambda e, k=k: e.dma_start(out=wo[:, k * 4:(k + 1) * 4, :], in_=wsrc[:, k, :, :]), "wo", (), ("wo",))
        gainb = SB(f"o_gain{l}", [128, D], F32, at=R_SZ)
        xt = SB(f"o_xt{l}", [128, D], F32, at=R_SZ + 8192)
        junk = SB(f"o_junk{l}", [128, 512], BF16, at=W0)
        st = SB(f"o_st{l}", [128, 8], F32, at=W0 + 1024)
        tmp = SB(f"o_tmp{l}", [128, 512], F32, at=W0 + 2048)
        load("sp", gainb[:], post_norm[l:l + 1, :].to_broadcast([128, D]), "ogain", (), ("gainb",))
        for i in range(8):
            load("sp", xt[:], x_src[i * 128:(i + 1) * 128, :], "oxt", (), ("xt",))
            for n4 in range(4):
                for k in range(16):
                    mm(ps[n4][:, :], mgT[:, k, i * 128:(i + 1) * 128], wo[:, k * 4 + n4, :], k == 0, k == 15, ("mgT", "wo"), (f"ps{n4}",))
                act(junk[:], ps[n4][:, :], AF.Square, (f"ps{n4}",), ("ojunk", "ost"), accum=st[:, n4:n4 + 1])
            P.op("dve", lambda e: e.tensor_reduce(st[:, 4:5], st[:, 0:4], AX.X, ALU.add), ("ost",), ("ost",))
            ts("dve", st[:, 5:6], st[:, 4:5], 1.0 / D, 1e-6, ALU.mult, ALU.add, ("ost",), ("ost",))
            act(st[:, 6:7], st[:, 5:6], AF.Sqrt, ("ost",), ("ost",))
            P.op("dve", lambda e: e.reciprocal(st[:, 7:8], st[:, 6:7]), ("ost",), ("ost",))
            for n4 in range(4):
                cs_ = slice(n4 * 512, (n4 + 1) * 512)
                stt("dve", tmp[:], ps[n4][:, :], st[:, 7:8], gainb[:, cs_], ALU.mult, ALU.mult, (f"ps{n4}", "ost", "gainb"), ("otmp2",))
                tt("dve", xt[:, cs_], xt[:, cs_], tmp[:], ALU.add, ("otmp2", "xt"), ("xt",))
            load("sp", x_dst[i * 128:(i + 1) * 128, :], xt[:], "oxo", ("xt",), ("xdst",))
        P.barrier()

    def dump(name, ap, shape, dt):
        o = nc.dram_tensor("d_" + name, list(shape), dt, kind="ExternalOutput").ap()
        load("sp", o, ap, "dump", (), ("dump_" + name,))

    def program():
        for l in range(DEPTH):
            x_src = x_in if l == 0 else x_mid
            x_dst = x_mid if l == 0 else y_out
            phase_norm(l, x_src)
            P.barrier()
            if upto == f"norm{l}":
                dump("hT", hT, [128, 16, T], BF16)
                dump("ropeT", ropeT[:], [32, 2, T], F32)
                return
            pstop = upto[3:] if (upto or "").startswith(f"p1{l}") and len(upto) > 3 else None
            phase_p1(l, pstop)
            P.barrier()
            if (upto or "").startswith(f"p1{l}"):
                dump("uTa", uTa[:, :, T:TT], [128, 8, T], BF16)
                if pstop != "a":
                    dump("gin_k", gin_c[1], [1024, T], BF16)
                if pstop not in ("a", "b"):
                    dump("gin_v", gin_vc[0], [512, D], BF16)
                if pstop is None:
                    dump("gout_k", gout_c[2], [2048, T], BF16)
                return
            st = phase_ssm(l)
            if upto == f"ssmprep{l}":
                dump("prm", st[3][:], [128, 16, 32], F32)
                dump("bbT", st[2][:], [128, 2, 32, 128], BF16)
                dump("cpad", st[1][:], [128, 2, 32, 128], BF16)
                dump("uTa", uTa[:], [128, 8, TT], BF16)
                return
            phase_ssm_main(l, st)
            if upto == f"ssm{l}":
                dump("ygT", st[0][:], [128, 8, T], BF16)
                dump("osT", szT[:], [128, 8, T], BF16)
                return
            phase_attn(l)
            if upto == f"attn{l}":
                dump("oaT", oaT, [128, 16, T], BF16)
                dump("osT", szT[:], [128, 8, T], BF16)
                return
            mgT = phase_merge(l)
            if upto == f"merge{l}":
                dump("mgT", mgT, [128, 16, T], BF16)
                return
            phase_out(l, mgT, x_src, x_dst)
            if upto == f"out{l}":
                dump("x_mid", x_mid, [T, D], F32)
                return
    program()
    P.barrier()

    import contextlib
    with contextlib.ExitStack() as es:
        sems = {}
        for k in list(Plan.ENG) + P.slots:
            sems[k] = es.enter_context(nc.semaphore("s_" + k))
        es.enter_context(nc.allow_non_contiguous_dma(reason="small strided parameter loads"))
        block = es.enter_context(nc.Block())

        refd = {k: set() for k in Plan.ENG}
        for e in Plan.ENG:
            for o in P.ops[e]:
                if o[0] == "wait" and o[1] in refd:
                    refd[o[1]].add(o[2])
        for k in Plan.ENG:
            if P.cnt.get(k, 0) > 0:
                refd[k].add(P.cnt[k])
        newc = {k: {c: i + 1 for i, c in enumerate(sorted(v))} for k, v in refd.items()}

        def emit(eng_name):
            def f(eng):
                n = 0
                for o in P.ops[eng_name]:
                    if o[0] == "wait":
                        v = newc[o[1]][o[2]] if o[1] in newc else o[2]
                        eng.wait_ge(sems[o[1]], v)
                    elif o[2] in newc:
                        n += 1
                        ins = o[1](eng)
                        if n in refd[o[2]]:
                            ins.then_inc(sems[o[2]], 1)
                    else:
                        o[1](eng).then_inc(sems[o[2]], o[3])
                if eng_name == "sp":
                    for k, c in P.cnt.items():
                        eng.wait_ge(sems[k], newc[k][c] if k in newc else c)
            return f
        block.tensor(emit("pe"))
        block.scalar(emit("act"))
        block.vector(emit("dve"))
        block.gpsimd(emit("pool"))
        block.sync(emit("sp"))
    return nc


_NC = {}


def run(inputs, n_cores=8, upto=None):
    bf = ml_dtypes.bfloat16
    x = np.ascontiguousarray(inputs["x"], dtype=np.float32)
    identf = np.eye(128, dtype=np.float32)
    perm = np.zeros((128, 128), np.float32)
    for m in range(16):
        perm[m + 16, m] = -1.0
        perm[m, m + 16] = 1.0
    tri = np.triu(np.ones((128, 128), np.float32))
    ones = np.ones((128, 128), np.float32)
    selm = np.zeros((8, 8, 128), np.float32)
    for n in range(8):
        selm[n, n, :] = 1.0
    tpos = np.tile(np.arange(TT, dtype=np.float32)[None, :], (128, 1))
    vbl = np.zeros((128, 8, 8), np.float32)
    for i in range(8):
        for n in range(4, 8):
            if (n - 4) >= i // 2:
                vbl[:, i, n] = NEG
    ropei = np.zeros((128, 1), np.float32)
    ropei[:32, 0] = np.arange(32) % 16
    consts = {"c_identf": identf, "c_perm": perm.astype(bf), "c_tri": tri.astype(bf), "c_ones": ones.astype(bf),
              "c_selm": selm.reshape(8, 1024).astype(bf), "c_tpos": tpos, "c_vbl": vbl.reshape(128, 64), "c_ropei": ropei}
    wnames = ["pre_norm", "post_norm", "w_in", "lam_re", "lam_im", "log_dt", "b_re", "b_im", "c_re", "c_im",
              "d_skip", "w_glu", "b_glu", "w_br_ssm", "w_br_attn", "w_out"]
    shared = {k: np.ascontiguousarray(inputs[k], dtype=np.float32) for k in wnames}
    shared.update(consts)
    in_maps = []
    for c in range(n_cores):
        b, s = c // 2, c % 2
        fl = np.zeros((128, 4), np.float32)
        fl[:, 0] = float(s)
        fl[:, 1] = float(s * T)
        fl[:, 2] = (float(s) - 1.0) * 1.0e30
        m = dict(shared)
        m["x"] = np.ascontiguousarray(x[b, s * T:(s + 1) * T, :])
        m["c_flags"] = fl
        in_maps.append(m)
    key = (n_cores, upto)
    if key not in _NC:
        _NC[key] = build_nc(n_cores, upto)
    res = run_bass_kernel_spmd(_NC[key], in_maps, core_ids=list(range(n_cores)))
    return res.results


def kernel(**inputs):
    results = run(inputs, 8, None)
    out = np.zeros((4, 2048, D), np.float32)
    for c in range(8):
        b, s = c // 2, c % 2
        out[b, s * T:(s + 1) * T, :] = results[c]["y"]
    return out
```

```python
import math
import numpy as np
import ml_dtypes
import concourse.bass as bass
import concourse.mybir as mybir
from concourse.bass_utils import run_bass_kernel_spmd

F32 = mybir.dt.float32
BF16 = mybir.dt.bfloat16
ALU = mybir.AluOpType
AF = mybir.ActivationFunctionType
AX = mybir.AxisListType

D = 2048
T = 1024
TT = 2048
NH = 16
DEPTH = 2
INW = 14336
C_U, C_ZS, C_Q, C_K, C_V, C_ZA, C_GS, C_GA = 0, 1024, 2048, 4096, 6144, 8192, 10240, 12288
MAGIC = 12582912.0
TWO_PI = 2.0 * math.pi
BIG = 30000.0
NEG = -1.0e30


class Plan:
    ENG = ("pe", "act", "dve", "pool", "sp")

    def __init__(self):
        self.ops = {e: [] for e in self.ENG}
        self.cnt = {}
        self.known = {e: {} for e in self.ENG}
        self.res = {}
        self.slots = []

    def _deps(self, reads, writes):
        deps = {}

        def add(d):
            if d is None:
                return
            k, c = d
            if deps.get(k, 0) < c:
                deps[k] = c
        for k in reads:
            r = self.res.get(k)
            if r:
                add(r[0])
        for k in writes:
            r = self.res.get(k)
            if r:
                add(r[0])
                for kk, cc in r[1].items():
                    add((kk, cc))
        return deps

    def _waits(self, eng, deps, skip_self=False):
        for k, c in deps.items():
            if skip_self and k == eng:
                continue
            if self.known[eng].get(k, 0) >= c:
                continue
            self.known[eng][k] = c
            self.ops[eng].append(("wait", k, c))

    def _update(self, reads, writes, prod):
        for k in reads:
            r = self.res.setdefault(k, [None, {}])
            if r[1].get(prod[0], 0) < prod[1]:
                r[1][prod[0]] = prod[1]
        for k in writes:
            self.res[k] = [prod, {}]

    def op(self, eng, fn, reads=(), writes=()):
        deps = self._deps(reads, writes)
        self._waits(eng, deps, skip_self=(eng == "pe"))
        c = self.cnt.get(eng, 0) + 1
        self.cnt[eng] = c
        self.ops[eng].append(("op", fn, eng, 1))
        self._update(reads, writes, (eng, c))

    def dma(self, q, fn, slot, reads=(), writes=(), inc=16):
        deps = self._deps(reads, writes)
        self._waits(q, deps)
        if slot not in self.slots:
            self.slots.append(slot)
        c = self.cnt.get(slot, 0) + inc
        self.cnt[slot] = c
        self.ops[q].append(("op", fn, slot, inc))
        self._update(reads, writes, (slot, c))

    def barrier(self):
        allk = dict(self.cnt)
        for e in self.ENG:
            self._waits(e, allk, skip_self=True)
        self.res = {}


def build_nc(n_cores=8, upto=None):
    nc = bass.Bass("TRN2", target_bir_lowering=False)
    P = Plan()

    def din(name, shape, dt=F32):
        return nc.dram_tensor(name, list(shape), dt, kind="ExternalInput").ap()

    x_in = din("x", [T, D])
    pre_norm = din("pre_norm", [DEPTH, D])
    post_norm = din("post_norm", [DEPTH, D])
    w_in = din("w_in", [DEPTH, D, INW])
    lam_re = din("lam_re", [DEPTH, 64, 64])
    lam_im = din("lam_im", [DEPTH, 64, 64])
    log_dt = din("log_dt", [DEPTH, 64])
    b_re = din("b_re", [DEPTH, 64, 64, 16])
    b_im = din("b_im", [DEPTH, 64, 64, 16])
    c_re = din("c_re", [DEPTH, 64, 16, 64])
    c_im = din("c_im", [DEPTH, 64, 16, 64])
    d_skip = din("d_skip", [DEPTH, 1024])
    w_glu = din("w_glu", [DEPTH, 1024, 1024])
    b_glu = din("b_glu", [DEPTH, 1024])
    w_br_ssm = din("w_br_ssm", [DEPTH, 1024, D])
    w_br_attn = din("w_br_attn", [DEPTH, D, D])
    w_out = din("w_out", [DEPTH, D, D])
    c_identf = din("c_identf", [128, 128])
    c_perm = din("c_perm", [128, 128], BF16)
    c_tri = din("c_tri", [128, 128], BF16)
    c_ones = din("c_ones", [128, 128], BF16)
    c_selm = din("c_selm", [8, 1024], BF16)
    c_tpos = din("c_tpos", [128, TT])
    c_vbl = din("c_vbl", [128, 64])
    c_ropei = din("c_ropei", [128, 1])
    c_flags = din("c_flags", [128, 4])
    y_out = nc.dram_tensor("y", [T, D], F32, kind="ExternalOutput").ap()

    def dint(name, shape, dt):
        return nc.dram_tensor(name, list(shape), dt).ap()
    x_mid = dint("x_mid", [T, D], F32)
    gin_c = [dint(f"gin{i}", [1024, T], BF16) for i in range(5)]
    gout_c = [dint(f"gout{i}", [2048, T], BF16) for i in range(5)]
    gin_u = gin_c[0]
    gout_u = gout_c[0]
    gin_vc = [gin_c[3 + c].rearrange("(t two) c -> t (two c)", two=2) for c in range(2)]
    gout_vc = [gout_c[3 + c][0:1024, :].rearrange("(t two) c -> t (two c)", two=2) for c in range(2)]

    def gk_rows(gl, h):
        return gl[1 + h // 8][(h % 8) * 128:(h % 8 + 1) * 128, :]

    off = [0]

    arena = nc.alloc_sbuf_tensor_at("arena", [128, 103 * 1024], BF16, offset=16640)

    def SB(name, shape, dt, at=None):
        esz = 4 if dt == F32 else 2
        nbytes = int(np.prod(shape[1:])) * esz
        nb = (nbytes + 63) // 64 * 64
        if at is None:
            at = off[0]
            off[0] += nb
        assert at % 4 == 0 and at + nb <= 206 * 1024, (name, at, nb)
        ap = arena[0:shape[0], at // 2:(at + nbytes) // 2]
        if dt == F32:
            ap = ap.bitcast(F32)
        if len(shape) == 3:
            ap = ap.rearrange("p (a b) -> p a b", a=shape[1])
        elif len(shape) == 4:
            ap = ap.rearrange("p (a b c) -> p a b c", a=shape[1], b=shape[2])
        return ap

    hoa = SB("hoa", [128, 2, 16, T], BF16)
    hT = hoa[:, 0]
    oaT = hoa[:, 1]
    OA0 = 32768
    uTa = SB("uTa", [128, 8, TT], BF16)
    szT = SB("szT", [128, 8, T], BF16)
    wbuf = SB("wbuf", [128, 2, 16, 512], BF16)
    identf = SB("identf", [128, 128], F32)
    permb = SB("permb", [128, 128], BF16)
    trib = SB("trib", [128, 128], BF16)
    onesb = SB("onesb", [128, 128], BF16)
    selm = SB("selm", [8, 1024], BF16)
    vbl = SB("vbl", [128, 64], F32)
    flags = SB("flags", [128, 4], F32)
    ropei = SB("ropei", [128, 1], F32)
    ropeT = SB("ropeT", [32, 2, T], F32)
    small = SB("small", [128, 64], F32)
    tpos_sb = SB("tpos_sb", [128, TT], F32)
    W0 = off[0]
    R_HT, R_OA, R_U, R_SZ = 0, 32768, 65536, 98304

    ps = [nc.alloc_psum_tensor(f"ps{i}", [128, 512], F32) for i in range(8)]

    def load(q, out_ap, in_ap, slot, reads=(), writes=()):
        P.dma(q, lambda e: e.dma_start(out=out_ap, in_=in_ap), slot, reads, writes)

    wslot_n = [0]

    def load_w(src_ap, kch, ncols):
        s = wslot_n[0] % 2
        wslot_n[0] += 1
        src = src_ap.rearrange("(k p) c -> p k c", p=128)
        for k0 in range(0, kch, 4):
            k1 = min(kch, k0 + 4)
            P.dma("pool", lambda e, d=wbuf[:, s, k0:k1, 0:ncols], sr=src[:, k0:k1, :]: e.dma_start(out=d, in_=sr),
                  f"w{s}", reads=(), writes=(f"wbuf{s}",))
        return s

    def mm(out_ap, lhsT, rhs, start, stop, reads, writes, tp=None):
        if tp is None:
            P.op("pe", lambda e: e.matmul(out_ap, lhsT, rhs, start=start, stop=stop), reads, writes)
        else:
            P.op("pe", lambda e: e.matmul(out_ap, lhsT, rhs, start=start, stop=stop, tile_position=tp),
                 reads, writes)

    def act(out_ap, in_ap, func, reads, writes, scale=1.0, bias=0.0, accum=None):
        if accum is None:
            P.op("act", lambda e: e.activation(out_ap, in_ap, func, bias=bias, scale=scale), reads, writes)
        else:
            P.op("act", lambda e: e.activation(out_ap, in_ap, func, bias=bias, scale=scale, accum_out=accum),
                 reads, writes)

    def tt(eng, out_ap, a, b, op, reads, writes):
        P.op(eng, lambda e: e.tensor_tensor(out_ap, a, b, op), reads, writes)

    def ts(eng, out_ap, a, s1, s2, op0, op1, reads, writes):
        if op1 is None:
            P.op(eng, lambda e: e.tensor_scalar(out_ap, a, s1, None, op0), reads, writes)
        else:
            P.op(eng, lambda e: e.tensor_scalar(out_ap, a, s1, s2, op0, op1), reads, writes)

    def stt(eng, out_ap, a, s, b, op0, op1, reads, writes):
        P.op(eng, lambda e: e.scalar_tensor_tensor(out_ap, a, s, b, op0, op1), reads, writes)

    def cp(eng, out_ap, in_ap, reads, writes):
        if eng == "act":
            P.op("act", lambda e: e.copy(out_ap, in_ap), reads, writes)
        else:
            P.op(eng, lambda e: e.tensor_copy(out_ap, in_ap), reads, writes)

    def fracs(eng, turns, tmp, keys):
        ts(eng, tmp, turns, MAGIC, -MAGIC, ALU.add, ALU.add, keys, keys)
        tt(eng, turns, turns, tmp, ALU.subtract, keys, keys)

    bankn = [0]
    nbanks = [4]

    def nb4():
        b = bankn[0] % nbanks[0]
        bankn[0] += 1
        return b

    for dst, src, nm in ((identf, c_identf, "identf"), (permb, c_perm, "permb"),
                         (trib, c_tri, "trib"), (onesb, c_ones, "onesb"), (selm, c_selm, "selm"),
                         (vbl, c_vbl, "vbl"), (flags, c_flags, "flags"), (ropei, c_ropei, "ropei"),
                         (tpos_sb, c_tpos, "tpos")):
        load("sp", dst[:], src, "c_" + nm, (), (nm,))
    rt1 = SB("rt1", [32, T], F32, at=R_OA)
    rt2 = SB("rt2", [32, T], F32, at=R_OA + 4096)
    act(small[0:32, 0:1], ropei[0:32, :], AF.Exp, ("ropei",), ("small",),
        scale=-math.log(500000.0) / 16.0, bias=-math.log(TWO_PI))
    ts("dve", rt1[:], tpos_sb[0:32, 0:T], flags[0:32, 1:2], small[0:32, 0:1], ALU.add, ALU.mult,
       ("tpos", "flags", "small"), ("rt",))
    fracs("dve", rt1[:], rt2[:], ("rt",))
    act(ropeT[:, 1, :], rt1[:], AF.Sin, ("rt",), ("ropeT",), scale=TWO_PI)
    act(rt2[:], rt1[:], AF.Abs, ("rt",), ("rt",))
    act(ropeT[:, 0, :], rt2[:], AF.Sin, ("rt",), ("ropeT",), scale=-TWO_PI, bias=math.pi / 2)
    P.barrier()

    def phase_norm(l, x_src):
        xt = [SB(f"n_xt{i}_{l}", [128, D], F32, at=R_U + i * 8192) for i in range(2)]
        hb = SB(f"n_hb_{l}", [128, D], F32, at=R_U + 16384)
        gainb = SB(f"n_gain_{l}", [128, D], F32, at=R_U + 24576)
        junk = SB(f"n_junk_{l}", [128, D], BF16, at=R_OA)
        st = SB(f"n_st_{l}", [128, 8], F32, at=R_OA + 4096)
        load("sp", gainb[:], pre_norm[l:l + 1, :].to_broadcast([128, D]), "gain", (), ("gainb",))
        for i in range(8):
            xb = xt[i % 2]
            xk = f"xt{i % 2}"
            load("sp", xb[:], x_src[i * 128:(i + 1) * 128, :], xk, (), (xk,))
            act(junk[:], xb[:], AF.Square, (xk,), ("junk", "st"), accum=st[:, 0:1])
            ts("dve", st[:, 1:2], st[:, 0:1], 1.0 / D, 1e-6, ALU.mult, ALU.add, ("st",), ("st",))
            act(st[:, 2:3], st[:, 1:2], AF.Sqrt, ("st",), ("st",))
            P.op("dve", lambda e: e.reciprocal(st[:, 3:4], st[:, 2:3]), ("st",), ("st",))
            stt("dve", hb[:], xb[:], st[:, 3:4], gainb[:], ALU.mult, ALU.mult, (xk, "st", "gainb"), ("hb",))
            for g4 in range(4):
                b = nb4()
                for j in range(4):
                    k = g4 * 4 + j
                    P.op("pe", lambda e, o=ps[b][:, j * 128:(j + 1) * 128], k=k: e.transpose(o, hb[:, k * 128:(k + 1) * 128], identf[:]),
                         ("hb", "identf"), (f"ps{b}",))
                cp("act" if g4 % 2 == 0 else "dve", hT[:, g4 * 4:(g4 + 1) * 4, i * 128:(i + 1) * 128],
                   ps[b][:, :].rearrange("p (j t) -> p j t", j=4), (f"ps{b}",), ("hT",))

    def proj_fm(s, ct, kch, rhsT, rkey, evac):
        for th in range(2):
            b = nb4()
            for k in range(kch):
                mm(ps[b][:, :], wbuf[:, s, k, ct * 128:(ct + 1) * 128], rhsT[:, k, th * 512:(th + 1) * 512],
                   k == 0, k == kch - 1, (f"wbuf{s}", rkey), (f"ps{b}",))
            evac(b, th)

    def rope(qf, qb, key):
        r1 = SB("rp_r1" + key, [32, 512], F32, at=W0)
        r2 = SB("rp_r2" + key, [32, 512], F32, at=W0 + 2048)
        cp("act", qb[:], qf[:], (key + "f",), (key + "b",))
        for th in range(2):
            sl = slice(th * 512, (th + 1) * 512)
            mm(ps[7][:, :], permb[:], qb[:, sl], True, True, ("permb", key + "b"), ("ps7",))
            tt("dve", r1[:], qf[0:32, sl], ropeT[:, 0, sl], ALU.mult, (key + "f", "ropeT"), ("rp1",))
            tt("dve", r2[:], ps[7][0:32, :], ropeT[:, 1, sl], ALU.mult, ("ps7", "ropeT"), ("rp2",))
            tt("dve", qf[0:32, sl], r1[:], r2[:], ALU.add, ("rp1", "rp2"), (key + "f",))
        cp("act", qb[0:32, :], qf[0:32, :], (key + "f",), (key + "b",))

    def phase_p1(l, stop=None):
        wl = w_in[l]
        kf = SB(f"p1_kf{l}", [128, T], F32, at=R_OA)
        kb = SB(f"p1_kb{l}", [128, T], BF16, at=R_OA + 4096)
        vst = [SB(f"p1_vst{i}_{l}", [128, 512], BF16, at=R_OA + 6144 + i * 1024) for i in range(2)]
        kml = SB(f"p1_kml{l}", [128, 64], F32, at=R_OA + 8192)
        for g in range(2):
            s = load_w(wl[:, C_U + g * 512:C_U + (g + 1) * 512], 16, 512)
            for ct in range(4):
                kt = g * 4 + ct
                proj_fm(s, ct, 16, hT, "hT",
                        lambda b, th, kt=kt: cp("act", uTa[:, kt, T + th * 512:T + (th + 1) * 512], ps[b][:, :],
                                                (f"ps{b}",), ("uTa",)))
        load("sp", gin_u.rearrange("(k p) t -> p k t", p=128), uTa[:, :, T:TT], "gu", ("uTa",), ("gin_u",))
        if stop == "a":
            return
        for g in range(4):
            s = load_w(wl[:, C_K + g * 512:C_K + (g + 1) * 512], 16, 512)
            for ct in range(4):
                h = g * 4 + ct
                proj_fm(s, ct, 16, hT, "hT",
                        lambda b, th: cp("act", kf[:, th * 512:(th + 1) * 512], ps[b][:, :], (f"ps{b}",), ("kff",)))
                rope(kf, kb, "kf")
                load("sp", gk_rows(gin_c, h), kb[:], "gk", ("kfb",), ("gin_k",))
        if stop == "b":
            return
        for g in range(4):
            s = load_w(wl[:, C_V + g * 512:C_V + (g + 1) * 512], 16, 512)
            for i in range(8):
                b = nb4()
                for k in range(16):
                    mm(ps[b][:, :], hT[:, k, i * 128:(i + 1) * 128], wbuf[:, s, k, 0:512], k == 0, k == 15,
                       (f"wbuf{s}", "hT"), (f"ps{b}",))
                vs = vst[i % 2]
                cp("act", vs[:], ps[b][:, :], (f"ps{b}",), (f"vst{i % 2}",))
                load("sp", gin_vc[i // 4][(i % 4) * 128:(i % 4 + 1) * 128, g * 512:(g + 1) * 512], vs[:], f"gv{i % 2}",
                     (f"vst{i % 2}",), ("gin_v",))
        if stop == "c":
            return
        for ci in range(5):
            P.dma("pool", lambda e, ci=ci: e.collective_compute(
                "AllGather", ALU.bypass, replica_groups=[[2 * i, 2 * i + 1] for i in range(n_cores // 2)],
                ins=[gin_c[ci].opt()], outs=[gout_c[ci].opt()]), f"cc{ci}", reads=("gin_u", "gin_k", "gin_v"),
                writes=("gout_u", "gout_k", "gout_v"), inc=1)

    def phase_ssm(l):
        A = R_OA
        ygT = SB(f"s_yg{l}", [128, 8, T], BF16, at=A)
        cpad = SB(f"s_cpad{l}", [128, 2, 32, 128], BF16, at=A + 16384)
        B = W0
        bbT = SB(f"s_bbT{l}", [128, 2, 32, 128], BF16, at=B + 16384)
        prm = SB(f"s_prm{l}", [128, 16, 32], F32, at=B + 4096)
        P.op("pool", lambda e: e.memset(bbT[:], 0.0), (), ("bbT",))
        wk = 114688
        load("sp", uTa[:, :, 0:T], gout_u[0:1024, :].rearrange("(k p) t -> p k t", p=128), "gul", ("gout_u",), ("uTa",))
        for kt in range(8):
            ts("dve", uTa[:, kt, 0:T], uTa[:, kt, 0:T], flags[:, 0:1], None, ALU.mult, None, ("uTa", "flags"), ("uTa",))
        LR, LI, DT, MAG, FR, AR, AI, DEN, NRM, FRE, FIM, T1, T2, T3, T4, T5 = range(16)
        pv = lambda i: prm[:, i, :]
        for two in range(2):
            load("sp", prm[two * 64:(two + 1) * 64, LR, :], lam_re[l].rearrange("(pi two) p -> two p pi", two=2)[two], "pl", (), ("prm",))
            load("sp", prm[two * 64:(two + 1) * 64, LI, :], lam_im[l].rearrange("(pi two) p -> two p pi", two=2)[two], "pl", (), ("prm",))
            load("sp", prm[two * 64:(two + 1) * 64, DT, :],
                 log_dt[l:l + 1, :].rearrange("o (pi two) -> o two pi", two=2)[:, two, :].to_broadcast([64, 32]),
                 "pl", (), ("prm",))
        K = ("prm",)
        act(pv(DT), pv(DT), AF.Exp, K, K)
        tt("dve", pv(T1), pv(LR), pv(DT), ALU.mult, K, K)
        act(pv(MAG), pv(T1), AF.Exp, K, K)
        tt("dve", pv(T2), pv(LI), pv(DT), ALU.mult, K, K)
        ts("dve", pv(FR), pv(T2), 1.0 / TWO_PI, None, ALU.mult, None, K, K)
        fracs("dve", pv(FR), pv(T3), K)
        act(pv(T4), pv(FR), AF.Sin, K, K, scale=TWO_PI)
        act(pv(T3), pv(FR), AF.Abs, K, K)
        act(pv(T5), pv(T3), AF.Sin, K, K, scale=-TWO_PI, bias=math.pi / 2)
        tt("dve", pv(AR), pv(MAG), pv(T5), ALU.mult, K, K)
        tt("dve", pv(AI), pv(MAG), pv(T4), ALU.mult, K, K)
        tt("dve", pv(T1), pv(LR), pv(LR), ALU.mult, K, K)
        tt("dve", pv(T2), pv(LI), pv(LI), ALU.mult, K, K)
        tt("dve", pv(DEN), pv(T1), pv(T2), ALU.add, K, K)
        P.op("dve", lambda e: e.reciprocal(pv(DEN), pv(DEN)), K, K)
        ts("dve", pv(NRM), pv(AR), -1.0, None, ALU.add, None, K, K)
        tt("dve", pv(T1), pv(NRM), pv(LR), ALU.mult, K, K)
        tt("dve", pv(T2), pv(AI), pv(LI), ALU.mult, K, K)
        tt("dve", pv(T1), pv(T1), pv(T2), ALU.add, K, K)
        tt("dve", pv(FRE), pv(T1), pv(DEN), ALU.mult, K, K)
        tt("dve", pv(T1), pv(AI), pv(LR), ALU.mult, K, K)
        tt("dve", pv(T2), pv(NRM), pv(LI), ALU.mult, K, K)
        tt("dve", pv(T1), pv(T1), pv(T2), ALU.subtract, K, K)
        tt("dve", pv(FIM), pv(T1), pv(DEN), ALU.mult, K, K)
        bS = [SB(f"s_bS{i}_{l}", [128, 32, 32], F32, at=wk + i * 4096) for i in range(4)]
        for i in range(2):
            P.op("dve", lambda e, t=bS[i]: e.memset(t[:], 0.0), (), (f"bS{i}",))
            src = (b_re, b_im)[i][l].rearrange("(pi two) p c -> two p pi c", two=2)
            for two in range(2):
                load("sp", bS[i][two * 64:(two + 1) * 64, :, two * 16:(two + 1) * 16], src[two], "pb", (), (f"bS{i}",))
        fre_b = pv(FRE).unsqueeze(2).to_broadcast([128, 32, 32])
        fim_b = pv(FIM).unsqueeze(2).to_broadcast([128, 32, 32])
        KB = ("bS0", "bS1", "bS2", "bS3", "prm")
        bbS = SB(f"s_bbS{l}", [128, 2, 32, 32], F32, at=wk + 16384)
        tt("dve", bS[2][:], bS[0][:], fre_b, ALU.mult, KB, KB)
        tt("dve", bS[3][:], bS[1][:], fim_b, ALU.mult, KB, KB)
        tt("dve", bbS[:, 0], bS[2][:], bS[3][:], ALU.subtract, KB, ("bbS",))
        tt("dve", bS[2][:], bS[1][:], fre_b, ALU.mult, KB, KB)
        tt("dve", bS[3][:], bS[0][:], fim_b, ALU.mult, KB, KB)
        tt("dve", bbS[:, 1], bS[2][:], bS[3][:], ALU.add, KB + ("bbS",), ("bbS",))
        for ri in range(2):
            for kt in range(8):
                P.op("pe", lambda e, ri=ri, kt=kt: e.transpose(ps[4][:, 0:128], bbS[:, ri, kt * 4:(kt + 1) * 4, :].rearrange("p a b -> p (a b)"), identf[:]),
                     ("bbS", "identf"), ("ps4",))
                for a4 in range(4):
                    cp("act", bbT[32 * a4:32 * a4 + 32, ri, kt * 4 + a4, :], ps[4][32 * a4:32 * a4 + 32, 0:128], ("ps4",), ("bbT",))
        P.op("pool", lambda e: e.memset(cpad[:], 0.0), (), ("cpad",))
        cS = [SB(f"s_cS{i}_{l}", [32, 32, 128], F32, at=R_SZ) for i in range(1)]
        for ri in range(2):
            P.op("dve", lambda e: e.memset(cS[0][:], 0.0), ("ps5",), ("cS",))
            src = (c_re, c_im)[ri][l].rearrange("(pi two) c p -> two c pi p", two=2)
            for two in range(2):
                load("sp", cS[0][two * 16:(two + 1) * 16, :, two * 64:(two + 1) * 64], src[two], "pc", (), ("cS",))
            for pi in range(32):
                P.op("pe", lambda e, pi=pi: e.transpose(ps[5][:, 0:32], cS[0][:, pi, :], identf[0:32, 0:32]),
                     ("cS", "identf"), ("ps5",))
                j = pi % 4
                act(cpad[:, ri, pi, j * 32:(j + 1) * 32], ps[5][:, 0:32], AF.Copy, ("ps5",), ("cpad",),
                    scale=(1.0 if ri == 0 else -1.0))
        dsk = SB(f"s_dsk{l}", [128, 8], F32, at=B + 6144)
        load("sp", dsk[:], d_skip[l].rearrange("(k p) -> p k", p=128), "pd", (), ("dsk",))
        P.barrier()
        return ygT, cpad, bbT, prm, dsk, None, (FR, MAG)

    def phase_ssm_main(l, st):
        ygT, cpad, bbT, prm, dsk, tl, (FR, MAG) = st
        Mb = R_SZ
        HT = 1024

        def F(name, dt, o):
            return SB(f"s_{name}{l}", [128, HT], dt, at=Mb + o)
        trn = [F("trn0", F32, 0), F("trn1", F32, 4096)]
        ttmp = F("ttmp", F32, 8192)
        af = F("af", F32, 12288)
        sn = [F("sn0", BF16, 16384), F("sn1", BF16, 18432)]
        cs = [F("cs0", BF16, 20480), F("cs1", BF16, 22528)]
        bre = [F("bre0", BF16, 24576), F("bre1", BF16, 26624)]
        bim = [F("bim0", BF16, 28672), F("bim1", BF16, 30720)]
        q1 = F("q1", BF16, 32768); q2 = F("q2", BF16, 34816); q3 = F("q3", BF16, 36864)
        xr = [F("xr0", BF16, 38912), F("xr1", BF16, 40960)]
        xi = [F("xi0", BF16, 43008), F("xi1", BF16, 45056)]
        dim = F("dim", BF16, 47104)
        yt = SB(f"s_yt{l}", [128, 512], F32, at=W0 + 32768)
        y2 = SB(f"s_y2{l}", [128, 512], F32, at=W0 + 32768 + 2048)
        y3 = SB(f"s_y3{l}", [128, 512], F32, at=W0 + 32768 + 4096)
        dec = SB(f"s_dec{l}", [128, 32], F32, at=W0 + 8192 + 128)
        cp("dve", dec[:], prm[:, MAG, :], ("prm",), ("dec",))
        its = [(pi, Hh) for pi in range(32) for Hh in range(2)]
        qn = [0]

        def frac_of(n):
            pi, Hh = its[n]
            tr, tk = trn[n % 2], f"trn{n % 2}"
            ts("dve", tr[:], tpos_sb[:, Hh * HT:(Hh + 1) * HT], prm[:, FR, pi:pi + 1], None, ALU.mult, None, ("tpos", "prm"), (tk,))
            ts("dve", ttmp[:], tr[:], MAGIC, -MAGIC, ALU.add, ALU.add, (tk,), ("ttmp",))
            tt("dve", tr[:], tr[:], ttmp[:], ALU.subtract, (tk, "ttmp"), (tk,))

        frac_of(0)
        for n, (pi, Hh) in enumerate(its):
            kt, j = pi // 4, pi % 4
            b2 = n % 2
            tr, tk = trn[b2], f"trn{b2}"
            snb, csb, breb, bimb = sn[b2], cs[b2], bre[b2], bim[b2]
            ks, kc, kbr, kbi = f"sn{b2}", f"cs{b2}", f"bre{b2}", f"bim{b2}"
            for qq in range(2):
                sl = slice(Hh * HT + qq * 512, Hh * HT + (qq + 1) * 512)
                pb = (qn[0] % 2) * 2
                qn[0] += 1
                mm(ps[pb][:, :], bbT[:, 0, pi, :], uTa[:, kt, sl], True, True, ("bbT", "uTa"), (f"ps{pb}",))
                mm(ps[pb + 1][:, :], bbT[:, 1, pi, :], uTa[:, kt, sl], True, True, ("bbT", "uTa"), (f"ps{pb + 1}",))
                cp("act", breb[:, qq * 512:(qq + 1) * 512], ps[pb][:, :], (f"ps{pb}",), (kbr,))
                cp("act", bimb[:, qq * 512:(qq + 1) * 512], ps[pb + 1][:, :], (f"ps{pb + 1}",), (kbi,))
            act(snb[:], tr[:], AF.Sin, (tk,), (ks,), scale=TWO_PI)
            act(af[:], tr[:], AF.Abs, (tk,), ("af",))
            act(csb[:], af[:], AF.Sin, ("af",), (kc,), scale=-TWO_PI, bias=math.pi / 2)
            if n + 1 < len(its):
                frac_of(n + 1)
            tt("dve", q1[:], breb[:], csb[:], ALU.mult, (kbr, kc), ("q1",))
            tt("dve", q2[:], bimb[:], snb[:], ALU.mult, (kbi, ks), ("q2",))
            tt("dve", q1[:], q1[:], q2[:], ALU.add, ("q1", "q2"), ("q1",))
            tt("dve", q3[:], bimb[:], csb[:], ALU.mult, (kbi, kc), ("q3",))
            tt("dve", q2[:], breb[:], snb[:], ALU.mult, (kbr, ks), ("q2",))
            tt("dve", q3[:], q3[:], q2[:], ALU.subtract, ("q3", "q2"), ("q3",))
            dcy = dec[:, pi:pi + 1].to_broadcast([128, HT])
            xrc, xic = xr[Hh], xi[Hh]
            if Hh == 0:
                P.op("dve", lambda e, xrc=xrc, dcy=dcy: e.tensor_tensor_scan(xrc[:], dcy, q1[:], 0.0, ALU.mult, ALU.add),
                     ("q1", "dec"), ("xr0",))
                P.op("dve", lambda e, xic=xic, dcy=dcy: e.tensor_tensor_scan(xic[:], dcy, q3[:], 0.0, ALU.mult, ALU.add),
                     ("q3", "dec"), ("xi0",))
            else:
                P.op("dve", lambda e, xrc=xrc, dcy=dcy: e.tensor_tensor_scan(xrc[:], dcy, q1[:], xr[0][:, HT - 1:HT], ALU.mult, ALU.add),
                     ("q1", "dec", "xr0"), ("xr1",))
                P.op("dve", lambda e, xic=xic, dcy=dcy: e.tensor_tensor_scan(xic[:], dcy, q3[:], xi[0][:, HT - 1:HT], ALU.mult, ALU.add),
                     ("q3", "dec", "xi0"), ("xi1",))
                tt("dve", q1[:], xrc[:], csb[:], ALU.mult, ("xr1", kc), ("q1",))
                tt("dve", q3[:], xic[:], snb[:], ALU.mult, ("xi1", ks), ("q3",))
                tt("dve", q2[:], q1[:], q3[:], ALU.subtract, ("q1", "q3"), ("q2",))
                tt("dve", q1[:], xrc[:], snb[:], ALU.mult, ("xr1", ks), ("q1",))
                tt("dve", q3[:], xic[:], csb[:], ALU.mult, ("xi1", kc), ("q3",))
                tt("dve", dim[:], q1[:], q3[:], ALU.add, ("q1", "q3"), ("dim",))
                for qq in range(2):
                    yb, yk = ps[4 + qq], f"ps{4 + qq}"
                    csl = slice(qq * 512, (qq + 1) * 512)
                    mm(yb[:, :], cpad[:, 0, pi, :], q2[:, csl], j == 0, False, ("cpad", "q2"), (yk,))
                    mm(yb[:, :], cpad[:, 1, pi, :], dim[:, csl], False, j == 3, ("cpad", "dim"), (yk,))
                    if j == 3:
                        tok = slice(qq * 512, (qq + 1) * 512)
                        stt("dve", yt[:], uTa[:, kt, T + qq * 512:T + (qq + 1) * 512], dsk[:, kt:kt + 1], yb[:, :],
                            ALU.mult, ALU.add, ("uTa", "dsk", yk), ("yt",))
                        act(y2[:], yt[:], AF.Square, ("yt",), ("y2",))
                        ts("dve", y2[:], y2[:], 0.044715, 1.0, ALU.mult, ALU.add, ("y2",), ("y2",))
                        tt("dve", y2[:], y2[:], yt[:], ALU.mult, ("y2", "yt"), ("y2",))
                        act(y3[:], y2[:], AF.Sigmoid, ("y2",), ("y3",), scale=2.0 * math.sqrt(2.0 / math.pi))
                        tt("dve", ygT[:, kt, tok], yt[:], y3[:], ALU.mult, ("yt", "y3"), ("ygT",))
        P.barrier()
        wl = w_in[l]
        for g in range(2):
            s = load_w(wl[:, C_ZS + g * 512:C_ZS + (g + 1) * 512], 16, 512)
            for ct in range(4):
                kt = g * 4 + ct
                proj_fm(s, ct, 16, hT, "hT",
                        lambda b, th, kt=kt: act(szT[:, kt, th * 512:(th + 1) * 512], ps[b][:, :], AF.Silu,
                                                 (f"ps{b}",), ("szT",)))
        bg = SB(f"s_bg{l}", [128, 8], F32, at=W0 + 8192 + 256)
        load("sp", bg[:], b_glu[l].rearrange("(k p) -> p k", p=128), "pbg", (), ("bg",))
        gl = SB(f"s_gl{l}", [128, 512], BF16, at=W0 + 8192 + 512)
        for g in range(2):
            s = load_w(w_glu[l][:, g * 512:(g + 1) * 512], 8, 512)
            for ct in range(4):
                kt = g * 4 + ct
                def ev(b, th, kt=kt):
                    act(gl[:], ps[b][:, :], AF.Sigmoid, (f"ps{b}", "bg"), ("gl",), bias=bg[:, kt:kt + 1])
                    tt("dve", gl[:], gl[:], ygT[:, kt, th * 512:(th + 1) * 512], ALU.mult, ("gl", "ygT"), ("gl",))
                    tt("dve", szT[:, kt, th * 512:(th + 1) * 512], szT[:, kt, th * 512:(th + 1) * 512], gl[:], ALU.mult,
                       ("gl", "szT"), ("szT",))
                proj_fm(s, ct, 8, ygT, "ygT", ev)
        P.barrier()

    def phase_attn(l):
        wl = w_in[l]
        U = R_U
        kTh = [SB(f"a_kT{i}_{l}", [128, TT], BF16, at=U + i * 4096) for i in range(2)]
        vh = [SB(f"a_vh{i}_{l}", [128, 16, 128], BF16, at=U + 8192 + i * 4096) for i in range(2)]
        qf2 = [SB(f"a_qf{i}_{l}", [128, T], F32, at=U + 16384 + i * 4096) for i in range(2)]
        qb2 = [SB(f"a_qb{i}_{l}", [128, T], BF16, at=U + 24576 + i * 2048) for i in range(2)]
        sza2 = [SB(f"a_sza{i}_{l}", [128, T], BF16, at=U + 28672 + i * 2048) for i in range(2)]
        V = W0 + 8192
        pT = [SB(f"a_pT{i}_{l}", [128, 256], BF16, at=V + i * 512) for i in range(4)]
        mbT2 = [SB(f"a_mbT{i}_{l}", [8, T], BF16, at=V + 2048 + i * 2048) for i in range(2)]
        kmT = SB(f"a_kmT{l}", [128, 16, 8], F32, at=V + 6144)
        gsb = SB(f"a_g{l}", [128, 8, 8], F32, at=V + 6656)
        vb = SB(f"a_vb{l}", [128, 8, 8], F32, at=V + 6912)
        top8 = SB(f"a_top{l}", [128, 8, 8], F32, at=V + 7168)
        thr = SB(f"a_thr{l}", [128, 8], F32, at=V + 7424)
        mb = SB(f"a_mb{l}", [128, 8, 8], F32, at=V + 7488)
        rden = SB(f"a_rden{l}", [128, 256], F32, at=V + 7744)
        otmp = SB(f"a_otmp{l}", [128, 256], F32, at=V + 8768)
        cp("dve", vb[:], vbl[:, :].rearrange("p (i n) -> p i n", n=8), ("vbl",), ("vb",))
        ts("dve", vb[:, :, 0:4], vb[:, :, 0:4], flags[:, 2:3], None, ALU.add, None, ("vb", "flags"), ("vb",))
        scale = 1.0 / math.sqrt(128.0)
        pn = [0]
        nbanks[0] = 2
        wsl = {}

        def prologue(h):
            g, ct = h // 4, h % 4
            hb = h % 2
            qf, qb, sza, mbT = qf2[hb], qb2[hb], sza2[hb], mbT2[hb]
            kq, ksz, kmb = f"qf{hb}", f"sza{hb}", f"mbT{hb}"
            kt_, vt_ = kTh[hb], vh[hb]
            kk, vk = f"kTh{hb}", f"vh{hb}"

            def s0():
                if ct == 0:
                    wsl["q"] = load_w(wl[:, C_Q + g * 512:C_Q + (g + 1) * 512], 16, 512)
                    wsl["z"] = load_w(wl[:, C_ZA + g * 512:C_ZA + (g + 1) * 512], 16, 512)
                load("sp", kt_[:, 0:T], gk_rows(gout_c, h), kk + "d", ("gout_k",), (kk,))
                load("sp", kt_[:, T:TT], gk_rows(gin_c, h), kk + "d", ("gin_k",), (kk,))
                for c2 in range(2):
                    load("sp", vt_[:, c2 * 4:c2 * 4 + 4, :], gout_vc[c2][:, h * 128:(h + 1) * 128].rearrange("(i p) c -> p i c", p=128),
                         vk + "d", ("gout_v",), (vk,))
                    load("sp", vt_[:, 8 + c2 * 4:12 + c2 * 4, :], gin_vc[c2][:, h * 128:(h + 1) * 128].rearrange("(i p) c -> p i c", p=128),
                         vk + "d", ("gin_v",), (vk,))
                proj_fm(wsl["q"], ct, 16, hT, "hT",
                        lambda b, th: cp("act", qf[:, th * 512:(th + 1) * 512], ps[b][:, :], (f"ps{b}",), (kq + "f",)))

            def s1():
                rope(qf, qb, kq)

            def s2():
                proj_fm(wsl["z"], ct, 16, hT, "hT",
                        lambda b, th: act(sza[:, th * 512:(th + 1) * 512], ps[b][:, :], AF.Silu, (f"ps{b}",), (ksz,)))
                P.op("dve", lambda e: e.tensor_reduce(kmT[:, h, :], kt_[:, :].rearrange("p (n j) -> p n j", n=8),
                                                      AX.X, ALU.add), (kk,), ("kmT",))
                for i in range(8):
                    mm(ps[7][:, i * 8:(i + 1) * 8], qf[:, i * 128:(i + 1) * 128], kmT[:, h, :], True, True, (kq + "f", "kmT"), ("ps7",))

            def s3():
                tt("dve", gsb[:], ps[7][:, 0:64].rearrange("p (i n) -> p i n", n=8), vb[:], ALU.add, ("ps7", "vb"), ("gsb",))
                for i in range(8):
                    P.op("dve", lambda e, i=i: e.max(top8[:, i, :], gsb[:, i, :]), ("gsb",), ("top8",))
                ts("dve", thr[:], top8[:, :, 2], -1.0e29, None, ALU.max, None, ("top8",), ("thr",))
                tt("dve", mb[:], gsb[:], thr[:].unsqueeze(2).to_broadcast([128, 8, 8]), ALU.is_ge, ("gsb", "thr"), ("mb",))
                ts("dve", mb[:], mb[:], -1.0, BIG / scale, ALU.add, ALU.mult, ("mb",), ("mb",))

            def s4():
                for hf in range(2):
                    for ii in range(4):
                        i = hf * 4 + ii
                        P.op("pe", lambda e, i=i, ii=ii: e.transpose(ps[6][0:8, ii * 128:(ii + 1) * 128], mb[:, i, :], identf[:]),
                             ("mb", "identf"), ("ps6",))
                    cp("act", mbT[:, hf * 512:(hf + 1) * 512], ps[6][0:8, :], ("ps6",), (kmb,))
            return [s0, s1, s2, s3, s4]

        def attention(h, hooks):
            hb = h % 2
            qb, sza, mbT = qb2[hb], sza2[hb], mbT2[hb]
            kq, ksz, kmb = f"qf{hb}", f"sza{hb}", f"mbT{hb}"
            kt_, vt_ = kTh[hb], vh[hb]
            kk, vk = f"kTh{hb}", f"vh{hb}"
            tl = []
            for jb in range(4):
                tiles = [(n, a) for n in range(4 + jb + 1) for a in range(2)]
                for idx, (n, a) in enumerate(tiles):
                    tl.append((jb, n, a, idx == 0, idx == len(tiles) - 1))
            info = {}

            def emit_s(t):
                jb, n, a, first, last = tl[t]
                kt = n * 2 + a
                own = (n == 4 + jb)
                q0 = 128 if (own and a == 1) else 0
                nq = 256 - q0
                sp_ = ps[4 + pn[0] % 2][:, 0:nq]
                sk = f"ps{4 + pn[0] % 2}"
                pt = pT[pn[0] % 4]
                pk = f"pT{pn[0] % 4}"
                pn[0] += 1
                mm(sp_, kt_[:, kt * 128:(kt + 1) * 128], qb[:, jb * 256 + q0:(jb + 1) * 256], True, own, (kk, kq + "b"), (sk,))
                if not own:
                    mm(sp_, selm[0:8, n * 128:(n + 1) * 128], mbT[0:8, jb * 256 + q0:(jb + 1) * 256], False, True,
                       ("selm", kmb), (sk,))
                act(pt[:, 0:nq], sp_, AF.Exp, (sk,), (pk,), scale=scale)
                if own:
                    tt("dve", pt[:, 0:128], pt[:, 0:128], trib[:], ALU.mult, (pk, "trib"), (pk,))
                info[t] = (pt, pk, q0, nq, kt)

            def emit_pv(t):
                jb, n, a, first, last = tl[t]
                pt, pk, q0, nq, kt = info.pop(t)
                bank = ps[2 + jb % 2]
                bk = f"ps{2 + jb % 2}"
                oT = bank[:, 0:256]
                dn = bank[:, 256:512]
                mm(oT[:, q0:256], vt_[:, kt, :], pt[:, 0:nq], first, False, (vk, pk), (bk,))
                mm(dn[:, q0:256], onesb[:], pt[:, 0:nq], False, last, ("onesb", pk), (bk,))
                if last:
                    qs = slice(jb * 256, (jb + 1) * 256)
                    P.op("dve", lambda e, dn=dn: e.reciprocal(rden[:], dn), (bk,), ("rden",))
                    tt("dve", otmp[:], oT, rden[:], ALU.mult, (bk, "rden"), ("otmp",))
                    tt("dve", oaT[:, h, qs], otmp[:], sza[:, qs], ALU.mult, ("otmp", ksz), ("oaT",))
                    if hooks:
                        hooks.pop(0)()

            emit_s(0)
            for t in range(len(tl)):
                if t + 1 < len(tl):
                    emit_s(t + 1)
                emit_pv(t)
            while hooks:
                hooks.pop(0)()

        for st_ in prologue(0):
            st_()
        for h in range(NH):
            hooks = []
            if h + 1 < NH:
                stg = prologue(h + 1)
                stg[0]()
                hooks = stg[1:]
            attention(h, hooks)
        nbanks[0] = 4
        P.barrier()

    def phase_merge(l):
        wl = w_in[l]
        mgT = uTa[:, :, :].rearrange("p k (a t) -> p (k a) t", a=2)
        sg = [SB(f"m_sg{i}_{l}", [128, 4, T], BF16, at=W0 + i * 8192) for i in range(2)]
        m1 = SB(f"m_m1{l}", [128, 512], F32, at=W0 + 16384)
        for g in range(4):
            for which, c0 in ((0, C_GS), (1, C_GA)):
                s = load_w(wl[:, c0 + g * 512:c0 + (g + 1) * 512], 16, 512)
                for ct in range(4):
                    proj_fm(s, ct, 16, hT, "hT",
                            lambda b, th, ct=ct, which=which: act(sg[which][:, ct, th * 512:(th + 1) * 512], ps[b][:, :], AF.Sigmoid,
                                                                  (f"ps{b}",), (f"sg{which}",)))
            s = load_w(w_br_ssm[l][:, g * 512:(g + 1) * 512], 8, 512)
            for ct in range(4):
                proj_fm(s, ct, 8, szT, "szT",
                        lambda b, th, ct=ct: tt("dve", mgT[:, g * 4 + ct, th * 512:(th + 1) * 512], ps[b][:, :],
                                                sg[0][:, ct, th * 512:(th + 1) * 512], ALU.mult, (f"ps{b}", "sg0"), ("mgT",)))
            s = load_w(w_br_attn[l][:, g * 512:(g + 1) * 512], 16, 512)
            for ct in range(4):
                def ev(b, th, ct=ct):
                    dst = mgT[:, g * 4 + ct, th * 512:(th + 1) * 512]
                    tt("dve", m1[:], ps[b][:, :], sg[1][:, ct, th * 512:(th + 1) * 512], ALU.mult, (f"ps{b}", "sg1"), ("m1",))
                    tt("dve", dst, dst, m1[:], ALU.add, ("m1", "mgT"), ("mgT",))
                proj_fm(s, ct, 16, oaT, "oaT", ev)
        P.barrier()
        return mgT

    def phase_out(l, mgT, x_src, x_dst):
        wo = hoa[:, :, :, :].rearrange("p a k (n c) -> p (a k n) c", c=512)
        wsrc = w_out[l].rearrange("(k p) (n c) -> p k n c", p=128, c=512)
        for k in range(16):
            P.dma("pool", lambda e, k=k: e.dma_start(out=wo[:, k * 4:(k + 1) * 4, :], in_=wsrc[:, k, :, :]), "wo", (), ("wo",))
        gainb = SB(f"o_gain{l}", [128, D], F32, at=R_SZ)
        xt = SB(f"o_xt{l}", [128, D], F32, at=R_SZ + 8192)
        junk = SB(f"o_junk{l}", [128, 512], BF16, at=W0)
        st = SB(f"o_st{l}", [128, 8], F32, at=W0 + 1024)
        tmp = SB(f"o_tmp{l}", [128, 512], F32, at=W0 + 2048)
        load("sp", gainb[:], post_norm[l:l + 1, :].to_broadcast([128, D]), "ogain", (), ("gainb",))
        for i in range(8):
            load("sp", xt[:], x_src[i * 128:(i + 1) * 128, :], "oxt", (), ("xt",))
            for n4 in range(4):
                for k in range(16):
                    mm(ps[n4][:, :], mgT[:, k, i * 128:(i + 1) * 128], wo[:, k * 4 + n4, :], k == 0, k == 15, ("mgT", "wo"), (f"ps{n4}",))
                act(junk[:], ps[n4][:, :], AF.Square, (f"ps{n4}",), ("ojunk", "ost"), accum=st[:, n4:n4 + 1])
            P.op("dve", lambda e: e.tensor_reduce(st[:, 4:5], st[:, 0:4], AX.X, ALU.add), ("ost",), ("ost",))
            ts("dve", st[:, 5:6], st[:, 4:5], 1.0 / D, 1e-6, ALU.mult, ALU.add, ("ost",), ("ost",))
            act(st[:, 6:7], st[:, 5:6], AF.Sqrt, ("ost",), ("ost",))
            P.op("dve", lambda e: e.reciprocal(st[:, 7:8], st[:, 6:7]), ("ost",), ("ost",))
            for n4 in range(4):
                cs_ = slice(n4 * 512, (n4 + 1) * 512)
                stt("dve", tmp[:], ps[n4][:, :], st[:, 7:8], gainb[:, cs_], ALU.mult, ALU.mult, (f"ps{n4}", "ost", "gainb"), ("otmp2",))
                tt("dve", xt[:, cs_], xt[:, cs_], tmp[:], ALU.add, ("otmp2", "xt"), ("xt",))
            load("sp", x_dst[i * 128:(i + 1) * 128, :], xt[:], "oxo", ("xt",), ("xdst",))
        P.barrier()

    def dump(name, ap, shape, dt):
        o = nc.dram_tensor("d_" + name, list(shape), dt, kind="ExternalOutput").ap()
        load("sp", o, ap, "dump", (), ("dump_" + name,))

    def program():
        for l in range(DEPTH):
            x_src = x_in if l == 0 else x_mid
            x_dst = x_mid if l == 0 else y_out
            phase_norm(l, x_src)
            P.barrier()
            if upto == f"norm{l}":
                dump("hT", hT, [128, 16, T], BF16)
                dump("ropeT", ropeT[:], [32, 2, T], F32)
                return
            pstop = upto[3:] if (upto or "").startswith(f"p1{l}") and len(upto) > 3 else None
            phase_p1(l, pstop)
            P.barrier()
            if (upto or "").startswith(f"p1{l}"):
                dump("uTa", uTa[:, :, T:TT], [128, 8, T], BF16)
                if pstop != "a":
                    dump("gin_k", gin_c[1], [1024, T], BF16)
                if pstop not in ("a", "b"):
                    dump("gin_v", gin_vc[0], [512, D], BF16)
                if pstop is None:
                    dump("gout_k", gout_c[2], [2048, T], BF16)
                return
            st = phase_ssm(l)
            if upto == f"ssmprep{l}":
                dump("prm", st[3][:], [128, 16, 32], F32)
                dump("bbT", st[2][:], [128, 2, 32, 128], BF16)
                dump("cpad", st[1][:], [128, 2, 32, 128], BF16)
                dump("uTa", uTa[:], [128, 8, TT], BF16)
                return
            phase_ssm_main(l, st)
            if upto == f"ssm{l}":
                dump("ygT", st[0][:], [128, 8, T], BF16)
                dump("osT", szT[:], [128, 8, T], BF16)
                return
            phase_attn(l)
            if upto == f"attn{l}":
                dump("oaT", oaT, [128, 16, T], BF16)
                dump("osT", szT[:], [128, 8, T], BF16)
                return
            mgT = phase_merge(l)
            if upto == f"merge{l}":
                dump("mgT", mgT, [128, 16, T], BF16)
                return
            phase_out(l, mgT, x_src, x_dst)
            if upto == f"out{l}":
                dump("x_mid", x_mid, [T, D], F32)
                return
    program()
    P.barrier()

    import contextlib
    with contextlib.ExitStack() as es:
        sems = {}
        for k in list(Plan.ENG) + P.slots:
            sems[k] = es.enter_context(nc.semaphore("s_" + k))
        es.enter_context(nc.allow_non_contiguous_dma(reason="small strided parameter loads"))
        block = es.enter_context(nc.Block())

        refd = {k: set() for k in Plan.ENG}
        for e in Plan.ENG:
            for o in P.ops[e]:
                if o[0] == "wait" and o[1] in refd:
                    refd[o[1]].add(o[2])
        for k in Plan.ENG:
            if P.cnt.get(k, 0) > 0:
                refd[k].add(P.cnt[k])
        newc = {k: {c: i + 1 for i, c in enumerate(sorted(v))} for k, v in refd.items()}

        def emit(eng_name):
            def f(eng):
                n = 0
                for o in P.ops[eng_name]:
                    if o[0] == "wait":
                        v = newc[o[1]][o[2]] if o[1] in newc else o[2]
                        eng.wait_ge(sems[o[1]], v)
                    elif o[2] in newc:
                        n += 1
                        ins = o[1](eng)
                        if n in refd[o[2]]:
                            ins.then_inc(sems[o[2]], 1)
                    else:
                        o[1](eng).then_inc(sems[o[2]], o[3])
                if eng_name == "sp":
                    for k, c in P.cnt.items():
                        eng.wait_ge(sems[k], newc[k][c] if k in newc else c)
            return f
        block.tensor(emit("pe"))
        block.scalar(emit("act"))
        block.vector(emit("dve"))
        block.gpsimd(emit("pool"))
        block.sync(emit("sp"))
    return nc


_NC = {}


def run(inputs, n_cores=8, upto=None):
    bf = ml_dtypes.bfloat16
    x = np.ascontiguousarray(inputs["x"], dtype=np.float32)
    identf = np.eye(128, dtype=np.float32)
    perm = np.zeros((128, 128), np.float32)
    for m in range(16):
        perm[m + 16, m] = -1.0
        perm[m, m + 16] = 1.0
    tri = np.triu(np.ones((128, 128), np.float32))
    ones = np.ones((128, 128), np.float32)
    selm = np.zeros((8, 8, 128), np.float32)
    for n in range(8):
        selm[n, n, :] = 1.0
    tpos = np.tile(np.arange(TT, dtype=np.float32)[None, :], (128, 1))
    vbl = np.zeros((128, 8, 8), np.float32)
    for i in range(8):
        for n in range(4, 8):
            if (n - 4) >= i // 2:
                vbl[:, i, n] = NEG
    ropei = np.zeros((128, 1), np.float32)
    ropei[:32, 0] = np.arange(32) % 16
    consts = {"c_identf": identf, "c_perm": perm.astype(bf), "c_tri": tri.astype(bf), "c_ones": ones.astype(bf),
              "c_selm": selm.reshape(8, 1024).astype(bf), "c_tpos": tpos, "c_vbl": vbl.reshape(128, 64), "c_ropei": ropei}
    wnames = ["pre_norm", "post_norm", "w_in", "lam_re", "lam_im", "log_dt", "b_re", "b_im", "c_re", "c_im",
              "d_skip", "w_glu", "b_glu", "w_br_ssm", "w_br_attn", "w_out"]
    shared = {k: np.ascontiguousarray(inputs[k], dtype=np.float32) for k in wnames}
    shared.update(consts)
    in_maps = []
    for c in range(n_cores):
        b, s = c // 2, c % 2
        fl = np.zeros((128, 4), np.float32)
        fl[:, 0] = float(s)
        fl[:, 1] = float(s * T)
        fl[:, 2] = (float(s) - 1.0) * 1.0e30
        m = dict(shared)
        m["x"] = np.ascontiguousarray(x[b, s * T:(s + 1) * T, :])
        m["c_flags"] = fl
        in_maps.append(m)
    key = (n_cores, upto)
    if key not in _NC:
        _NC[key] = build_nc(n_cores, upto)
    res = run_bass_kernel_spmd(_NC[key], in_maps, core_ids=list(range(n_cores)))
    return res.results


def kernel(**inputs):
    results = run(inputs, 8, None)
    out = np.zeros((4, 2048, D), np.float32)
    for c in range(8):
        b, s = c // 2, c % 2
        out[b, s * T:(s + 1) * T, :] = results[c]["y"]
    return out
```

```python
import math
import numpy as np
import ml_dtypes
import concourse.bass as bass
import concourse.mybir as mybir
from concourse.bass_utils import run_bass_kernel_spmd

F32 = mybir.dt.float32
BF16 = mybir.dt.bfloat16
ALU = mybir.AluOpType
AF = mybir.ActivationFunctionType
AX = mybir.AxisListType

D = 2048
T = 1024
TT = 2048
NH = 16
DEPTH = 2
INW = 14336
C_U, C_ZS, C_Q, C_K, C_V, C_ZA, C_GS, C_GA = 0, 1024, 2048, 4096, 6144, 8192, 10240, 12288
MAGIC = 12582912.0
TWO_PI = 2.0 * math.pi
BIG = 30000.0
NEG = -1.0e30


class Plan:
    ENG = ("pe", "act", "dve", "pool", "sp")

    def __init__(self):
        self.ops = {e: [] for e in self.ENG}
        self.cnt = {}
        self.known = {e: {} for e in self.ENG}
        self.res = {}
        self.slots = []

    def _deps(self, reads, writes):
        deps = {}

        def add(d):
            if d is None:
                return
            k, c = d
            if deps.get(k, 0) < c:
                deps[k] = c
        for k in reads:
            r = self.res.get(k)
            if r:
                add(r[0])
        for k in writes:
            r = self.res.get(k)
            if r:
                add(r[0])
                for kk, cc in r[1].items():
                    add((kk, cc))
        return deps

    def _waits(self, eng, deps, skip_self=False):
        for k, c in deps.items():
            if skip_self and k == eng:
                continue
            if self.known[eng].get(k, 0) >= c:
                continue
            self.known[eng][k] = c
            self.ops[eng].append(("wait", k, c))

    def _update(self, reads, writes, prod):
        for k in reads:
            r = self.res.setdefault(k, [None, {}])
            if r[1].get(prod[0], 0) < prod[1]:
                r[1][prod[0]] = prod[1]
        for k in writes:
            self.res[k] = [prod, {}]

    def op(self, eng, fn, reads=(), writes=()):
        deps = self._deps(reads, writes)
        self._waits(eng, deps, skip_self=(eng == "pe"))
        c = self.cnt.get(eng, 0) + 1
        self.cnt[eng] = c
        self.ops[eng].append(("op", fn, eng, 1))
        self._update(reads, writes, (eng, c))

    def dma(self, q, fn, slot, reads=(), writes=(), inc=16):
        deps = self._deps(reads, writes)
        self._waits(q, deps)
        if slot not in self.slots:
            self.slots.append(slot)
        c = self.cnt.get(slot, 0) + inc
        self.cnt[slot] = c
        self.ops[q].append(("op", fn, slot, inc))
        self._update(reads, writes, (slot, c))

    def barrier(self):
        soft = getattr(self, "soft", set())
        allk = {k: c for k, c in self.cnt.items() if k not in soft}
        for e in self.ENG:
            self._waits(e, allk, skip_self=True)
        self.res = {k: [v[0], {}] for k, v in self.res.items() if v[0] is not None and v[0][0] in soft}


def build_nc(n_cores=8, upto=None):
    nc = bass.Bass("TRN2", target_bir_lowering=False)
    P = Plan()

    def din(name, shape, dt=F32):
        return nc.dram_tensor(name, list(shape), dt, kind="ExternalInput").ap()

    x_in = din("x", [T, D])
    pre_norm = din("pre_norm", [DEPTH, D])
    post_norm = din("post_norm", [DEPTH, D])
    w_in = din("w_in", [DEPTH, D, INW])
    lam_re = din("lam_re", [DEPTH, 64, 64])
    lam_im = din("lam_im", [DEPTH, 64, 64])
    log_dt = din("log_dt", [DEPTH, 64])
    b_re = din("b_re", [DEPTH, 64, 64, 16])
    b_im = din("b_im", [DEPTH, 64, 64, 16])
    c_re = din("c_re", [DEPTH, 64, 16, 64])
    c_im = din("c_im", [DEPTH, 64, 16, 64])
    d_skip = din("d_skip", [DEPTH, 1024])
    w_glu = din("w_glu", [DEPTH, 1024, 1024])
    b_glu = din("b_glu", [DEPTH, 1024])
    w_br_ssm = din("w_br_ssm", [DEPTH, 1024, D])
    w_br_attn = din("w_br_attn", [DEPTH, D, D])
    w_out = din("w_out", [DEPTH, D, D])
    c_identf = din("c_identf", [128, 128])
    c_perm = din("c_perm", [128, 128], BF16)
    c_tri = din("c_tri", [128, 128], BF16)
    c_ones = din("c_ones", [128, 128], BF16)
    c_selm = din("c_selm", [8, 1024], BF16)
    c_tpos = din("c_tpos", [128, TT])
    c_vbl = din("c_vbl", [128, 64])
    c_ropei = din("c_ropei", [128, 1])
    c_flags = din("c_flags", [128, 4])
    y_out = nc.dram_tensor("y", [T, D], F32, kind="ExternalOutput").ap()

    def dint(name, shape, dt):
        return nc.dram_tensor(name, list(shape), dt).ap()
    x_mid = dint("x_mid", [T, D], F32)
    gin_c = [dint(f"gin{i}", [1024, T], BF16) for i in range(5)]
    gout_c = [dint(f"gout{i}", [2048, T], BF16) for i in range(5)]
    gin_u = gin_c[0]
    gout_u = gout_c[0]
    gin_vc = [gin_c[3 + c].rearrange("(t two) c -> t (two c)", two=2) for c in range(2)]
    gout_vc = [gout_c[3 + c][0:1024, :].rearrange("(t two) c -> t (two c)", two=2) for c in range(2)]

    def gk_rows(gl, h):
        return gl[1 + h // 8][(h % 8) * 128:(h % 8 + 1) * 128, :]

    off = [0]

    arena = nc.alloc_sbuf_tensor_at("arena", [128, 103 * 1024], BF16, offset=16640)

    def SB(name, shape, dt, at=None):
        esz = 4 if dt == F32 else 2
        nbytes = int(np.prod(shape[1:])) * esz
        nb = (nbytes + 63) // 64 * 64
        if at is None:
            at = off[0]
            off[0] += nb
        assert at % 4 == 0 and at + nb <= 206 * 1024, (name, at, nb)
        ap = arena[0:shape[0], at // 2:(at + nbytes) // 2]
        if dt == F32:
            ap = ap.bitcast(F32)
        if len(shape) == 3:
            ap = ap.rearrange("p (a b) -> p a b", a=shape[1])
        elif len(shape) == 4:
            ap = ap.rearrange("p (a b c) -> p a b c", a=shape[1], b=shape[2])
        return ap

    hoa = SB("hoa", [128, 2, 16, T], BF16)
    hT = hoa[:, 0]
    oaT = hoa[:, 1]
    OA0 = 32768
    uTa = SB("uTa", [128, 8, TT], BF16)
    szT = SB("szT", [128, 8, T], BF16)
    wbuf = SB("wbuf", [128, 2, 16, 512], BF16)
    identf = SB("identf", [128, 128], F32)
    permb = SB("permb", [128, 128], BF16)
    trib = SB("trib", [128, 128], BF16)
    onesb = SB("onesb", [128, 128], BF16)
    selm = SB("selm", [8, 1024], BF16)
    vbl = SB("vbl", [128, 64], F32)
    flags = SB("flags", [128, 4], F32)
    ropei = SB("ropei", [128, 1], F32)
    ropeT = SB("ropeT", [32, 2, T], F32)
    small = SB("small", [128, 64], F32)
    tpos_sb = SB("tpos_sb", [128, TT], F32)
    W0 = off[0]
    R_HT, R_OA, R_U, R_SZ = 0, 32768, 65536, 98304

    ps = [nc.alloc_psum_tensor(f"ps{i}", [128, 512], F32) for i in range(8)]

    def load(q, out_ap, in_ap, slot, reads=(), writes=()):
        P.dma(q, lambda e: e.dma_start(out=out_ap, in_=in_ap), slot, reads, writes)

    wslot_n = [0]

    def load_w(src_ap, kch, ncols):
        s = wslot_n[0] % 2
        wslot_n[0] += 1
        src = src_ap.rearrange("(k p) c -> p k c", p=128)
        for k0 in range(0, kch, 4):
            k1 = min(kch, k0 + 4)
            P.dma("pool", lambda e, d=wbuf[:, s, k0:k1, 0:ncols], sr=src[:, k0:k1, :]: e.dma_start(out=d, in_=sr),
                  f"w{s}", reads=(), writes=(f"wbuf{s}",))
        return s

    def mm(out_ap, lhsT, rhs, start, stop, reads, writes, tp=None):
        if tp is None:
            P.op("pe", lambda e: e.matmul(out_ap, lhsT, rhs, start=start, stop=stop), reads, writes)
        else:
            P.op("pe", lambda e: e.matmul(out_ap, lhsT, rhs, start=start, stop=stop, tile_position=tp),
                 reads, writes)

    def act(out_ap, in_ap, func, reads, writes, scale=1.0, bias=0.0, accum=None):
        if accum is None:
            P.op("act", lambda e: e.activation(out_ap, in_ap, func, bias=bias, scale=scale), reads, writes)
        else:
            P.op("act", lambda e: e.activation(out_ap, in_ap, func, bias=bias, scale=scale, accum_out=accum),
                 reads, writes)

    def tt(eng, out_ap, a, b, op, reads, writes):
        P.op(eng, lambda e: e.tensor_tensor(out_ap, a, b, op), reads, writes)

    def ts(eng, out_ap, a, s1, s2, op0, op1, reads, writes):
        if op1 is None:
            P.op(eng, lambda e: e.tensor_scalar(out_ap, a, s1, None, op0), reads, writes)
        else:
            P.op(eng, lambda e: e.tensor_scalar(out_ap, a, s1, s2, op0, op1), reads, writes)

    def stt(eng, out_ap, a, s, b, op0, op1, reads, writes):
        P.op(eng, lambda e: e.scalar_tensor_tensor(out_ap, a, s, b, op0, op1), reads, writes)

    def cp(eng, out_ap, in_ap, reads, writes):
        if eng == "act":
            P.op("act", lambda e: e.copy(out_ap, in_ap), reads, writes)
        else:
            P.op(eng, lambda e: e.tensor_copy(out_ap, in_ap), reads, writes)

    def fracs(eng, turns, tmp, keys):
        ts(eng, tmp, turns, MAGIC, -MAGIC, ALU.add, ALU.add, keys, keys)
        tt(eng, turns, turns, tmp, ALU.subtract, keys, keys)

    bankn = [0]
    nbanks = [4]

    def nb4():
        b = bankn[0] % nbanks[0]
        bankn[0] += 1
        return b

    for dst, src, nm in ((identf, c_identf, "identf"), (permb, c_perm, "permb"),
                         (trib, c_tri, "trib"), (onesb, c_ones, "onesb"), (selm, c_selm, "selm"),
                         (vbl, c_vbl, "vbl"), (flags, c_flags, "flags"), (ropei, c_ropei, "ropei"),
                         (tpos_sb, c_tpos, "tpos")):
        load("sp", dst[:], src, "c_" + nm, (), (nm,))
    rt1 = SB("rt1", [32, T], F32, at=R_OA)
    rt2 = SB("rt2", [32, T], F32, at=R_OA + 4096)
    act(small[0:32, 0:1], ropei[0:32, :], AF.Exp, ("ropei",), ("small",),
        scale=-math.log(500000.0) / 16.0, bias=-math.log(TWO_PI))
    ts("dve", rt1[:], tpos_sb[0:32, 0:T], flags[0:32, 1:2], small[0:32, 0:1], ALU.add, ALU.mult,
       ("tpos", "flags", "small"), ("rt",))
    fracs("dve", rt1[:], rt2[:], ("rt",))
    act(ropeT[:, 1, :], rt1[:], AF.Sin, ("rt",), ("ropeT",), scale=TWO_PI)
    act(rt2[:], rt1[:], AF.Abs, ("rt",), ("rt",))
    act(ropeT[:, 0, :], rt2[:], AF.Sin, ("rt",), ("ropeT",), scale=-TWO_PI, bias=math.pi / 2)
    P.barrier()

    def phase_norm(l, x_src):
        xt = [SB(f"n_xt{i}_{l}", [128, D], F32, at=R_U + i * 8192) for i in range(2)]
        hb = SB(f"n_hb_{l}", [128, D], F32, at=R_U + 16384)
        gainb = SB(f"n_gain_{l}", [128, D], F32, at=R_U + 24576)
        junk = SB(f"n_junk_{l}", [128, D], BF16, at=R_OA)
        st = SB(f"n_st_{l}", [128, 8], F32, at=R_OA + 4096)
        load("sp", gainb[:], pre_norm[l:l + 1, :].to_broadcast([128, D]), "gain", (), ("gainb",))
        for i in range(8):
            xb = xt[i % 2]
            xk = f"xt{i % 2}"
            load("sp", xb[:], x_src[i * 128:(i + 1) * 128, :], xk, (), (xk,))
            act(junk[:], xb[:], AF.Square, (xk,), ("junk", "st"), accum=st[:, 0:1])
            ts("dve", st[:, 1:2], st[:, 0:1], 1.0 / D, 1e-6, ALU.mult, ALU.add, ("st",), ("st",))
            act(st[:, 2:3], st[:, 1:2], AF.Sqrt, ("st",), ("st",))
            P.op("dve", lambda e: e.reciprocal(st[:, 3:4], st[:, 2:3]), ("st",), ("st",))
            stt("dve", hb[:], xb[:], st[:, 3:4], gainb[:], ALU.mult, ALU.mult, (xk, "st", "gainb"), ("hb",))
            for g4 in range(4):
                b = nb4()
                for j in range(4):
                    k = g4 * 4 + j
                    P.op("pe", lambda e, o=ps[b][:, j * 128:(j + 1) * 128], k=k: e.transpose(o, hb[:, k * 128:(k + 1) * 128], identf[:]),
                         ("hb", "identf"), (f"ps{b}",))
                cp("act" if g4 % 2 == 0 else "dve", hT[:, g4 * 4:(g4 + 1) * 4, i * 128:(i + 1) * 128],
                   ps[b][:, :].rearrange("p (j t) -> p j t", j=4), (f"ps{b}",), ("hT",))

    def proj_fm(s, ct, kch, rhsT, rkey, evac):
        for th in range(2):
            b = nb4()
            for k in range(kch):
                mm(ps[b][:, :], wbuf[:, s, k, ct * 128:(ct + 1) * 128], rhsT[:, k, th * 512:(th + 1) * 512],
                   k == 0, k == kch - 1, (f"wbuf{s}", rkey), (f"ps{b}",))
            evac(b, th)

    def rope(qf, qb, key):
        r1 = SB("rp_r1" + key, [32, 512], F32, at=W0)
        r2 = SB("rp_r2" + key, [32, 512], F32, at=W0 + 2048)
        cp("act", qb[:], qf[:], (key + "f",), (key + "b",))
        for th in range(2):
            sl = slice(th * 512, (th + 1) * 512)
            mm(ps[7][:, :], permb[:], qb[:, sl], True, True, ("permb", key + "b"), ("ps7",))
            tt("dve", r1[:], qf[0:32, sl], ropeT[:, 0, sl], ALU.mult, (key + "f", "ropeT"), ("rp1",))
            tt("dve", r2[:], ps[7][0:32, :], ropeT[:, 1, sl], ALU.mult, ("ps7", "ropeT"), ("rp2",))
            tt("dve", qf[0:32, sl], r1[:], r2[:], ALU.add, ("rp1", "rp2"), (key + "f",))
        cp("act", qb[0:32, :], qf[0:32, :], (key + "f",), (key + "b",))

    def phase_p1(l, stop=None, hook=None):
        wl = w_in[l]
        kf = SB(f"p1_kf{l}", [128, T], F32, at=R_OA)
        kb = SB(f"p1_kb{l}", [128, T], BF16, at=R_OA + 4096)
        vst = [SB(f"p1_vst{i}_{l}", [128, 512], BF16, at=R_OA + 6144 + i * 1024) for i in range(2)]
        kml = SB(f"p1_kml{l}", [128, 64], F32, at=R_OA + 8192)
        for g in range(2):
            s = load_w(wl[:, C_U + g * 512:C_U + (g + 1) * 512], 16, 512)
            for ct in range(4):
                kt = g * 4 + ct
                proj_fm(s, ct, 16, hT, "hT",
                        lambda b, th, kt=kt: cp("act", uTa[:, kt, T + th * 512:T + (th + 1) * 512], ps[b][:, :],
                                                (f"ps{b}",), ("uTa",)))
        load("sp", gin_u.rearrange("(k p) t -> p k t", p=128), uTa[:, :, T:TT], "gu", ("uTa",), ("gin_u",))
        if stop == "a":
            return

        def gather(ci, key):
            P.dma("pool", lambda e: e.collective_compute(
                "AllGather", ALU.bypass, replica_groups=[[2 * i, 2 * i + 1] for i in range(n_cores // 2)],
                ins=[gin_c[ci].opt()], outs=[gout_c[ci].opt()]), f"cc{ci}", reads=("gin_" + key,),
                writes=("gout_" + key,), inc=1)
        gather(0, "u")
        if hook is not None:
            hook()
        for g in range(4):
            s = load_w(wl[:, C_K + g * 512:C_K + (g + 1) * 512], 16, 512)
            for ct in range(4):
                h = g * 4 + ct
                proj_fm(s, ct, 16, hT, "hT",
                        lambda b, th: cp("act", kf[:, th * 512:(th + 1) * 512], ps[b][:, :], (f"ps{b}",), ("kff",)))
                rope(kf, kb, "kf")
                load("sp", gk_rows(gin_c, h), kb[:], "gk", ("kfb",), (f"gin_k{h // 8}",))
        if stop == "b":
            return
        for g in range(4):
            s = load_w(wl[:, C_V + g * 512:C_V + (g + 1) * 512], 16, 512)
            for i in range(8):
                b = nb4()
                for k in range(16):
                    mm(ps[b][:, :], hT[:, k, i * 128:(i + 1) * 128], wbuf[:, s, k, 0:512], k == 0, k == 15,
                       (f"wbuf{s}", "hT"), (f"ps{b}",))
                vs = vst[i % 2]
                cp("act", vs[:], ps[b][:, :], (f"ps{b}",), (f"vst{i % 2}",))
                load("sp", gin_vc[i // 4][(i % 4) * 128:(i % 4 + 1) * 128, g * 512:(g + 1) * 512], vs[:], f"gv{i % 2}",
                     (f"vst{i % 2}",), (f"gin_v{i // 4}",))
        if stop == "c":
            return
        for ci, key in ((1, "k0"), (2, "k1"), (3, "v0"), (4, "v1")):
            gather(ci, key)
        P.soft = {"cc1", "cc2", "cc3", "cc4"}

    def phase_ssm(l):
        A = R_OA
        ygT = SB(f"s_yg{l}", [128, 8, T], BF16, at=A)
        cpad = SB(f"s_cpad{l}", [128, 2, 32, 128], BF16, at=A + 16384)
        B = W0
        bbT = SB(f"s_bbT{l}", [128, 2, 32, 128], BF16, at=B + 16384)
        prm = SB(f"s_prm{l}", [128, 16, 32], F32, at=B + 4096)
        P.op("pool", lambda e: e.memset(bbT[:], 0.0), (), ("bbT",))
        LR, LI, DT, MAG, FR, AR, AI, DEN, NRM, FRE, FIM, T1, T2, T3, T4, T5 = range(16)
        pv = lambda i: prm[:, i, :]
        for two in range(2):
            load("sp", prm[two * 64:(two + 1) * 64, LR, :], lam_re[l].rearrange("(pi two) p -> two p pi", two=2)[two], "pl", (), ("prm",))
            load("sp", prm[two * 64:(two + 1) * 64, LI, :], lam_im[l].rearrange("(pi two) p -> two p pi", two=2)[two], "pl", (), ("prm",))
            load("sp", prm[two * 64:(two + 1) * 64, DT, :],
                 log_dt[l:l + 1, :].rearrange("o (pi two) -> o two pi", two=2)[:, two, :].to_broadcast([64, 32]),
                 "pl", (), ("prm",))
        K = ("prm",)
        act(pv(DT), pv(DT), AF.Exp, K, K)
        tt("dve", pv(T1), pv(LR), pv(DT), ALU.mult, K, K)
        act(pv(MAG), pv(T1), AF.Exp, K, K)
        tt("dve", pv(T2), pv(LI), pv(DT), ALU.mult, K, K)
        ts("dve", pv(FR), pv(T2), 1.0 / TWO_PI, None, ALU.mult, None, K, K)
        fracs("dve", pv(FR), pv(T3), K)
        act(pv(T4), pv(FR), AF.Sin, K, K, scale=TWO_PI)
        act(pv(T3), pv(FR), AF.Abs, K, K)
        act(pv(T5), pv(T3), AF.Sin, K, K, scale=-TWO_PI, bias=math.pi / 2)
        tt("dve", pv(AR), pv(MAG), pv(T5), ALU.mult, K, K)
        tt("dve", pv(AI), pv(MAG), pv(T4), ALU.mult, K, K)
        tt("dve", pv(T1), pv(LR), pv(LR), ALU.mult, K, K)
        tt("dve", pv(T2), pv(LI), pv(LI), ALU.mult, K, K)
        tt("dve", pv(DEN), pv(T1), pv(T2), ALU.add, K, K)
        P.op("dve", lambda e: e.reciprocal(pv(DEN), pv(DEN)), K, K)
        ts("dve", pv(NRM), pv(AR), -1.0, None, ALU.add, None, K, K)
        tt("dve", pv(T1), pv(NRM), pv(LR), ALU.mult, K, K)
        tt("dve", pv(T2), pv(AI), pv(LI), ALU.mult, K, K)
        tt("dve", pv(T1), pv(T1), pv(T2), ALU.add, K, K)
        tt("dve", pv(FRE), pv(T1), pv(DEN), ALU.mult, K, K)
        tt("dve", pv(T1), pv(AI), pv(LR), ALU.mult, K, K)
        tt("dve", pv(T2), pv(NRM), pv(LI), ALU.mult, K, K)
        tt("dve", pv(T1), pv(T1), pv(T2), ALU.subtract, K, K)
        tt("dve", pv(FIM), pv(T1), pv(DEN), ALU.mult, K, K)
        bS = [SB(f"s_bS0_{l}", [128, 32, 32], F32, at=R_OA + 8192), SB(f"s_bS1_{l}", [128, 32, 32], F32, at=R_OA + 12288),
              SB(f"s_bS2_{l}", [128, 32, 32], F32, at=W0 + 32768), SB(f"s_bS3_{l}", [128, 32, 32], F32, at=W0 + 36864)]
        for i in range(2):
            P.op("dve", lambda e, t=bS[i]: e.memset(t[:], 0.0), (), (f"bS{i}",))
            src = (b_re, b_im)[i][l].rearrange("(pi two) p c -> two p pi c", two=2)
            for two in range(2):
                load("sp", bS[i][two * 64:(two + 1) * 64, :, two * 16:(two + 1) * 16], src[two], "pb", (), (f"bS{i}",))
        fre_b = pv(FRE).unsqueeze(2).to_broadcast([128, 32, 32])
        fim_b = pv(FIM).unsqueeze(2).to_broadcast([128, 32, 32])
        KB = ("bS0", "bS1", "bS2", "bS3", "prm")
        bbS = SB(f"s_bbS{l}", [128, 2, 32, 32], F32, at=W0 + 8192)
        tt("dve", bS[2][:], bS[0][:], fre_b, ALU.mult, KB, KB)
        tt("dve", bS[3][:], bS[1][:], fim_b, ALU.mult, KB, KB)
        tt("dve", bbS[:, 0], bS[2][:], bS[3][:], ALU.subtract, KB, ("bbS",))
        tt("dve", bS[2][:], bS[1][:], fre_b, ALU.mult, KB, KB)
        tt("dve", bS[3][:], bS[0][:], fim_b, ALU.mult, KB, KB)
        tt("dve", bbS[:, 1], bS[2][:], bS[3][:], ALU.add, KB + ("bbS",), ("bbS",))
        for ri in range(2):
            for kt in range(8):
                P.op("pe", lambda e, ri=ri, kt=kt: e.transpose(ps[4][:, 0:128], bbS[:, ri, kt * 4:(kt + 1) * 4, :].rearrange("p a b -> p (a b)"), identf[:]),
                     ("bbS", "identf"), ("ps4",))
                for a4 in range(4):
                    cp("act", bbT[32 * a4:32 * a4 + 32, ri, kt * 4 + a4, :], ps[4][32 * a4:32 * a4 + 32, 0:128], ("ps4",), ("bbT",))
        P.op("pool", lambda e: e.memset(cpad[:], 0.0), (), ("cpad",))
        cS = [SB(f"s_cS{i}_{l}", [32, 32, 128], F32, at=R_SZ) for i in range(1)]
        for ri in range(2):
            P.op("dve", lambda e: e.memset(cS[0][:], 0.0), ("ps5",), ("cS",))
            src = (c_re, c_im)[ri][l].rearrange("(pi two) c p -> two c pi p", two=2)
            for two in range(2):
                load("sp", cS[0][two * 16:(two + 1) * 16, :, two * 64:(two + 1) * 64], src[two], "pc", (), ("cS",))
            for pi in range(32):
                P.op("pe", lambda e, pi=pi: e.transpose(ps[5][:, 0:32], cS[0][:, pi, :], identf[0:32, 0:32]),
                     ("cS", "identf"), ("ps5",))
                j = pi % 4
                act(cpad[:, ri, pi, j * 32:(j + 1) * 32], ps[5][:, 0:32], AF.Copy, ("ps5",), ("cpad",),
                    scale=(1.0 if ri == 0 else -1.0))
        dsk = SB(f"s_dsk{l}", [128, 8], F32, at=B + 6144)
        load("sp", dsk[:], d_skip[l].rearrange("(k p) -> p k", p=128), "pd", (), ("dsk",))
        return ygT, cpad, bbT, prm, dsk, None, (FR, MAG)

    def phase_ssm_main(l, st):
        ygT, cpad, bbT, prm, dsk, tl, (FR, MAG) = st
        Mb = R_SZ
        HT = 1024

        def F(name, dt, o):
            return SB(f"s_{name}{l}", [128, HT], dt, at=Mb + o)
        trn = [F("trn0", F32, 0), F("trn1", F32, 4096)]
        ttmp = F("ttmp", F32, 8192)
        af = F("af", F32, 12288)
        sn = [F("sn0", BF16, 16384), F("sn1", BF16, 18432)]
        cs = [F("cs0", BF16, 20480), F("cs1", BF16, 22528)]
        bre = [F("bre0", BF16, 24576), F("bre1", BF16, 26624)]
        bim = [F("bim0", BF16, 28672), F("bim1", BF16, 30720)]
        q1 = F("q1", BF16, 32768); q2 = F("q2", BF16, 34816); q3 = F("q3", BF16, 36864)
        xr = [F("xr0", BF16, 38912), F("xr1", BF16, 40960)]
        xi = [F("xi0", BF16, 43008), F("xi1", BF16, 45056)]
        dim = F("dim", BF16, 47104)
        yt = SB(f"s_yt{l}", [128, 512], F32, at=W0 + 32768)
        y2 = SB(f"s_y2{l}", [128, 512], F32, at=W0 + 32768 + 2048)
        y3 = SB(f"s_y3{l}", [128, 512], F32, at=W0 + 32768 + 4096)
        dec = SB(f"s_dec{l}", [128, 32], F32, at=W0 + 8192 + 128)
        cp("dve", dec[:], prm[:, MAG, :], ("prm",), ("dec",))
        its = [(pi, Hh) for pi in range(32) for Hh in range(2)]
        qn = [0]

        def frac_of(n):
            pi, Hh = its[n]
            tr, tk = trn[n % 2], f"trn{n % 2}"
            ts("dve", tr[:], tpos_sb[:, Hh * HT:(Hh + 1) * HT], prm[:, FR, pi:pi + 1], None, ALU.mult, None, ("tpos", "prm"), (tk,))
            ts("dve", ttmp[:], tr[:], MAGIC, -MAGIC, ALU.add, ALU.add, (tk,), ("ttmp",))
            tt("dve", tr[:], tr[:], ttmp[:], ALU.subtract, (tk, "ttmp"), (tk,))

        frac_of(0)
        for n, (pi, Hh) in enumerate(its):
            kt, j = pi // 4, pi % 4
            b2 = n % 2
            tr, tk = trn[b2], f"trn{b2}"
            snb, csb, breb, bimb = sn[b2], cs[b2], bre[b2], bim[b2]
            ks, kc, kbr, kbi = f"sn{b2}", f"cs{b2}", f"bre{b2}", f"bim{b2}"
            for qq in range(2):
                sl = slice(Hh * HT + qq * 512, Hh * HT + (qq + 1) * 512)
                pb = (qn[0] % 2) * 2
                qn[0] += 1
                mm(ps[pb][:, :], bbT[:, 0, pi, :], uTa[:, kt, sl], True, True, ("bbT", "uTa"), (f"ps{pb}",))
                mm(ps[pb + 1][:, :], bbT[:, 1, pi, :], uTa[:, kt, sl], True, True, ("bbT", "uTa"), (f"ps{pb + 1}",))
                cp("act", breb[:, qq * 512:(qq + 1) * 512], ps[pb][:, :], (f"ps{pb}",), (kbr,))
                cp("act", bimb[:, qq * 512:(qq + 1) * 512], ps[pb + 1][:, :], (f"ps{pb + 1}",), (kbi,))
            act(snb[:], tr[:], AF.Sin, (tk,), (ks,), scale=TWO_PI)
            act(af[:], tr[:], AF.Abs, (tk,), ("af",))
            act(csb[:], af[:], AF.Sin, ("af",), (kc,), scale=-TWO_PI, bias=math.pi / 2)
            if n + 1 < len(its):
                frac_of(n + 1)
            tt("dve", q1[:], breb[:], csb[:], ALU.mult, (kbr, kc), ("q1",))
            tt("dve", q2[:], bimb[:], snb[:], ALU.mult, (kbi, ks), ("q2",))
            tt("dve", q1[:], q1[:], q2[:], ALU.add, ("q1", "q2"), ("q1",))
            tt("dve", q3[:], bimb[:], csb[:], ALU.mult, (kbi, kc), ("q3",))
            tt("dve", q2[:], breb[:], snb[:], ALU.mult, (kbr, ks), ("q2",))
            tt("dve", q3[:], q3[:], q2[:], ALU.subtract, ("q3", "q2"), ("q3",))
            dcy = dec[:, pi:pi + 1].to_broadcast([128, HT])
            xrc, xic = xr[Hh], xi[Hh]
            if Hh == 0:
                P.op("dve", lambda e, xrc=xrc, dcy=dcy: e.tensor_tensor_scan(xrc[:], dcy, q1[:], 0.0, ALU.mult, ALU.add),
                     ("q1", "dec"), ("xr0",))
                P.op("dve", lambda e, xic=xic, dcy=dcy: e.tensor_tensor_scan(xic[:], dcy, q3[:], 0.0, ALU.mult, ALU.add),
                     ("q3", "dec"), ("xi0",))
            else:
                P.op("dve", lambda e, xrc=xrc, dcy=dcy: e.tensor_tensor_scan(xrc[:], dcy, q1[:], xr[0][:, HT - 1:HT], ALU.mult, ALU.add),
                     ("q1", "dec", "xr0"), ("xr1",))
                P.op("dve", lambda e, xic=xic, dcy=dcy: e.tensor_tensor_scan(xic[:], dcy, q3[:], xi[0][:, HT - 1:HT], ALU.mult, ALU.add),
                     ("q3", "dec", "xi0"), ("xi1",))
                tt("dve", q1[:], xrc[:], csb[:], ALU.mult, ("xr1", kc), ("q1",))
                tt("dve", q3[:], xic[:], snb[:], ALU.mult, ("xi1", ks), ("q3",))
                tt("dve", q2[:], q1[:], q3[:], ALU.subtract, ("q1", "q3"), ("q2",))
                tt("dve", q1[:], xrc[:], snb[:], ALU.mult, ("xr1", ks), ("q1",))
                tt("dve", q3[:], xic[:], csb[:], ALU.mult, ("xi1", kc), ("q3",))
                tt("dve", dim[:], q1[:], q3[:], ALU.add, ("q1", "q3"), ("dim",))
                for qq in range(2):
                    yb, yk = ps[4 + qq], f"ps{4 + qq}"
                    csl = slice(qq * 512, (qq + 1) * 512)
                    mm(yb[:, :], cpad[:, 0, pi, :], q2[:, csl], j == 0, False, ("cpad", "q2"), (yk,))
                    mm(yb[:, :], cpad[:, 1, pi, :], dim[:, csl], False, j == 3, ("cpad", "dim"), (yk,))
                    if j == 3:
                        tok = slice(qq * 512, (qq + 1) * 512)
                        stt("dve", yt[:], uTa[:, kt, T + qq * 512:T + (qq + 1) * 512], dsk[:, kt:kt + 1], yb[:, :],
                            ALU.mult, ALU.add, ("uTa", "dsk", yk), ("yt",))
                        act(y2[:], yt[:], AF.Square, ("yt",), ("y2",))
                        ts("dve", y2[:], y2[:], 0.044715, 1.0, ALU.mult, ALU.add, ("y2",), ("y2",))
                        tt("dve", y2[:], y2[:], yt[:], ALU.mult, ("y2", "yt"), ("y2",))
                        act(y3[:], y2[:], AF.Sigmoid, ("y2",), ("y3",), scale=2.0 * math.sqrt(2.0 / math.pi))
                        tt("dve", ygT[:, kt, tok], yt[:], y3[:], ALU.mult, ("yt", "y3"), ("ygT",))
        P.barrier()
        wl = w_in[l]
        for g in range(2):
            s = load_w(wl[:, C_ZS + g * 512:C_ZS + (g + 1) * 512], 16, 512)
            for ct in range(4):
                kt = g * 4 + ct
                proj_fm(s, ct, 16, hT, "hT",
                        lambda b, th, kt=kt: act(szT[:, kt, th * 512:(th + 1) * 512], ps[b][:, :], AF.Silu,
                                                 (f"ps{b}",), ("szT",)))
        bg = SB(f"s_bg{l}", [128, 8], F32, at=W0 + 8192 + 256)
        load("sp", bg[:], b_glu[l].rearrange("(k p) -> p k", p=128), "pbg", (), ("bg",))
        gl = SB(f"s_gl{l}", [128, 512], BF16, at=W0 + 8192 + 512)
        for g in range(2):
            s = load_w(w_glu[l][:, g * 512:(g + 1) * 512], 8, 512)
            for ct in range(4):
                kt = g * 4 + ct
                def ev(b, th, kt=kt):
                    act(gl[:], ps[b][:, :], AF.Sigmoid, (f"ps{b}", "bg"), ("gl",), bias=bg[:, kt:kt + 1])
                    tt("dve", gl[:], gl[:], ygT[:, kt, th * 512:(th + 1) * 512], ALU.mult, ("gl", "ygT"), ("gl",))
                    tt("dve", szT[:, kt, th * 512:(th + 1) * 512], szT[:, kt, th * 512:(th + 1) * 512], gl[:], ALU.mult,
                       ("gl", "szT"), ("szT",))
                proj_fm(s, ct, 8, ygT, "ygT", ev)
        P.barrier()

    def phase_attn(l):
        wl = w_in[l]
        U = R_U
        kTh = [SB(f"a_kT{i}_{l}", [128, TT], BF16, at=U + i * 4096) for i in range(2)]
        vh = [SB(f"a_vh{i}_{l}", [128, 16, 128], BF16, at=U + 8192 + i * 4096) for i in range(2)]
        qf2 = [SB(f"a_qf{i}_{l}", [128, T], F32, at=U + 16384 + i * 4096) for i in range(2)]
        qb2 = [SB(f"a_qb{i}_{l}", [128, T], BF16, at=U + 24576 + i * 2048) for i in range(2)]
        sza2 = [SB(f"a_sza{i}_{l}", [128, T], BF16, at=U + 28672 + i * 2048) for i in range(2)]
        V = W0 + 8192
        pT = [SB(f"a_pT{i}_{l}", [128, 256], BF16, at=V + i * 512) for i in range(4)]
        mbT2 = [SB(f"a_mbT{i}_{l}", [8, T], BF16, at=V + 2048 + i * 2048) for i in range(2)]
        kmT = SB(f"a_kmT{l}", [128, 16, 8], F32, at=V + 6144)
        gsb = SB(f"a_g{l}", [128, 8, 8], F32, at=V + 6656)
        vb = SB(f"a_vb{l}", [128, 8, 8], F32, at=V + 6912)
        top8 = SB(f"a_top{l}", [128, 8, 8], F32, at=V + 7168)
        thr = SB(f"a_thr{l}", [128, 8], F32, at=V + 7424)
        mb = SB(f"a_mb{l}", [128, 8, 8], F32, at=V + 7488)
        rden = SB(f"a_rden{l}", [128, 256], F32, at=V + 7744)
        otmp = SB(f"a_otmp{l}", [128, 256], F32, at=V + 8768)
        cp("dve", vb[:], vbl[:, :].rearrange("p (i n) -> p i n", n=8), ("vbl",), ("vb",))
        ts("dve", vb[:, :, 0:4], vb[:, :, 0:4], flags[:, 2:3], None, ALU.add, None, ("vb", "flags"), ("vb",))
        scale = 1.0 / math.sqrt(128.0)
        pn = [0]
        nbanks[0] = 2
        wsl = {}

        def prologue(h):
            g, ct = h // 4, h % 4
            hb = h % 2
            qf, qb, sza, mbT = qf2[hb], qb2[hb], sza2[hb], mbT2[hb]
            kq, ksz, kmb = f"qf{hb}", f"sza{hb}", f"mbT{hb}"
            kt_, vt_ = kTh[hb], vh[hb]
            kk, vk = f"kTh{hb}", f"vh{hb}"

            def s0():
                if ct == 0:
                    wsl["q"] = load_w(wl[:, C_Q + g * 512:C_Q + (g + 1) * 512], 16, 512)
                    wsl["z"] = load_w(wl[:, C_ZA + g * 512:C_ZA + (g + 1) * 512], 16, 512)
                load("sp", kt_[:, 0:T], gk_rows(gout_c, h), kk + "d", (f"gout_k{h // 8}",), (kk,))
                load("sp", kt_[:, T:TT], gk_rows(gin_c, h), kk + "d", (f"gin_k{h // 8}",), (kk,))
                for c2 in range(2):
                    load("sp", vt_[:, c2 * 4:c2 * 4 + 4, :], gout_vc[c2][:, h * 128:(h + 1) * 128].rearrange("(i p) c -> p i c", p=128),
                         vk + "d", (f"gout_v{c2}",), (vk,))
                    load("sp", vt_[:, 8 + c2 * 4:12 + c2 * 4, :], gin_vc[c2][:, h * 128:(h + 1) * 128].rearrange("(i p) c -> p i c", p=128),
                         vk + "d", (f"gin_v{c2}",), (vk,))
                proj_fm(wsl["q"], ct, 16, hT, "hT",
                        lambda b, th: cp("act", qf[:, th * 512:(th + 1) * 512], ps[b][:, :], (f"ps{b}",), (kq + "f",)))

            def s1():
                rope(qf, qb, kq)

            def s2():
                proj_fm(wsl["z"], ct, 16, hT, "hT",
                        lambda b, th: act(sza[:, th * 512:(th + 1) * 512], ps[b][:, :], AF.Silu, (f"ps{b}",), (ksz,)))
                P.op("dve", lambda e: e.tensor_reduce(kmT[:, h, :], kt_[:, :].rearrange("p (n j) -> p n j", n=8),
                                                      AX.X, ALU.add), (kk,), ("kmT",))
                for i in range(8):
                    mm(ps[7][:, i * 8:(i + 1) * 8], qf[:, i * 128:(i + 1) * 128], kmT[:, h, :], True, True, (kq + "f", "kmT"), ("ps7",))

            def s3():
                tt("dve", gsb[:], ps[7][:, 0:64].rearrange("p (i n) -> p i n", n=8), vb[:], ALU.add, ("ps7", "vb"), ("gsb",))
                for i in range(8):
                    P.op("dve", lambda e, i=i: e.max(top8[:, i, :], gsb[:, i, :]), ("gsb",), ("top8",))
                ts("dve", thr[:], top8[:, :, 2], -1.0e29, None, ALU.max, None, ("top8",), ("thr",))
                tt("dve", mb[:], gsb[:], thr[:].unsqueeze(2).to_broadcast([128, 8, 8]), ALU.is_ge, ("gsb", "thr"), ("mb",))
                ts("dve", mb[:], mb[:], -1.0, BIG / scale, ALU.add, ALU.mult, ("mb",), ("mb",))

            def s4():
                for hf in range(2):
                    for ii in range(4):
                        i = hf * 4 + ii
                        P.op("pe", lambda e, i=i, ii=ii: e.transpose(ps[6][0:8, ii * 128:(ii + 1) * 128], mb[:, i, :], identf[:]),
                             ("mb", "identf"), ("ps6",))
                    cp("act", mbT[:, hf * 512:(hf + 1) * 512], ps[6][0:8, :], ("ps6",), (kmb,))
            return [s0, s1, s2, s3, s4]

        def attention(h, hooks):
            hb = h % 2
            qb, sza, mbT = qb2[hb], sza2[hb], mbT2[hb]
            kq, ksz, kmb = f"qf{hb}", f"sza{hb}", f"mbT{hb}"
            kt_, vt_ = kTh[hb], vh[hb]
            kk, vk = f"kTh{hb}", f"vh{hb}"
            tl = []
            for jb in range(4):
                tiles = [(n, a) for n in range(4 + jb + 1) for a in range(2)]
                for idx, (n, a) in enumerate(tiles):
                    tl.append((jb, n, a, idx == 0, idx == len(tiles) - 1))
            info = {}

            def emit_s(t):
                jb, n, a, first, last = tl[t]
                kt = n * 2 + a
                own = (n == 4 + jb)
                q0 = 128 if (own and a == 1) else 0
                nq = 256 - q0
                sp_ = ps[4 + pn[0] % 2][:, 0:nq]
                sk = f"ps{4 + pn[0] % 2}"
                pt = pT[pn[0] % 4]
                pk = f"pT{pn[0] % 4}"
                pn[0] += 1
                mm(sp_, kt_[:, kt * 128:(kt + 1) * 128], qb[:, jb * 256 + q0:(jb + 1) * 256], True, own, (kk, kq + "b"), (sk,))
                if not own:
                    mm(sp_, selm[0:8, n * 128:(n + 1) * 128], mbT[0:8, jb * 256 + q0:(jb + 1) * 256], False, True,
                       ("selm", kmb), (sk,))
                act(pt[:, 0:nq], sp_, AF.Exp, (sk,), (pk,), scale=scale)
                if own:
                    tt("dve", pt[:, 0:128], pt[:, 0:128], trib[:], ALU.mult, (pk, "trib"), (pk,))
                info[t] = (pt, pk, q0, nq, kt)

            def emit_pv(t):
                jb, n, a, first, last = tl[t]
                pt, pk, q0, nq, kt = info.pop(t)
                bank = ps[2 + jb % 2]
                bk = f"ps{2 + jb % 2}"
                oT = bank[:, 0:256]
                dn = bank[:, 256:512]
                mm(oT[:, q0:256], vt_[:, kt, :], pt[:, 0:nq], first, False, (vk, pk), (bk,))
                mm(dn[:, q0:256], onesb[:], pt[:, 0:nq], False, last, ("onesb", pk), (bk,))
                if last:
                    qs = slice(jb * 256, (jb + 1) * 256)
                    P.op("dve", lambda e, dn=dn: e.reciprocal(rden[:], dn), (bk,), ("rden",))
                    tt("dve", otmp[:], oT, rden[:], ALU.mult, (bk, "rden"), ("otmp",))
                    tt("dve", oaT[:, h, qs], otmp[:], sza[:, qs], ALU.mult, ("otmp", ksz), ("oaT",))
                    if hooks:
                        hooks.pop(0)()

            emit_s(0)
            for t in range(len(tl)):
                if t + 1 < len(tl):
                    emit_s(t + 1)
                emit_pv(t)
            while hooks:
                hooks.pop(0)()

        for st_ in prologue(0):
            st_()
        for h in range(NH):
            hooks = []
            if h + 1 < NH:
                stg = prologue(h + 1)
                stg[0]()
                hooks = stg[1:]
            attention(h, hooks)
        nbanks[0] = 4
        P.soft = set()
        P.barrier()

    def phase_merge(l):
        wl = w_in[l]
        mgT = uTa[:, :, :].rearrange("p k (a t) -> p (k a) t", a=2)
        sg = [SB(f"m_sg{i}_{l}", [128, 4, T], BF16, at=W0 + i * 8192) for i in range(2)]
        m1 = SB(f"m_m1{l}", [128, 512], F32, at=W0 + 16384)
        for g in range(4):
            for which, c0 in ((0, C_GS), (1, C_GA)):
                s = load_w(wl[:, c0 + g * 512:c0 + (g + 1) * 512], 16, 512)
                for ct in range(4):
                    proj_fm(s, ct, 16, hT, "hT",
                            lambda b, th, ct=ct, which=which: act(sg[which][:, ct, th * 512:(th + 1) * 512], ps[b][:, :], AF.Sigmoid,
                                                                  (f"ps{b}",), (f"sg{which}",)))
            s = load_w(w_br_ssm[l][:, g * 512:(g + 1) * 512], 8, 512)
            for ct in range(4):
                proj_fm(s, ct, 8, szT, "szT",
                        lambda b, th, ct=ct: tt("dve", mgT[:, g * 4 + ct, th * 512:(th + 1) * 512], ps[b][:, :],
                                                sg[0][:, ct, th * 512:(th + 1) * 512], ALU.mult, (f"ps{b}", "sg0"), ("mgT",)))
            s = load_w(w_br_attn[l][:, g * 512:(g + 1) * 512], 16, 512)
            for ct in range(4):
                def ev(b, th, ct=ct):
                    dst = mgT[:, g * 4 + ct, th * 512:(th + 1) * 512]
                    tt("dve", m1[:], ps[b][:, :], sg[1][:, ct, th * 512:(th + 1) * 512], ALU.mult, (f"ps{b}", "sg1"), ("m1",))
                    tt("dve", dst, dst, m1[:], ALU.add, ("m1", "mgT"), ("mgT",))
                proj_fm(s, ct, 16, oaT, "oaT", ev)
        P.barrier()
        return mgT

    def phase_out(l, mgT, x_src, x_dst):
        wo = hoa[:, :, :, :].rearrange("p a k (n c) -> p (a k n) c", c=512)
        wsrc = w_out[l].rearrange("(k p) (n c) -> p k n c", p=128, c=512)
        wo4 = wo.rearrange("p (k n) c -> p k n c", n=4)
        for n4 in range(4):
            for kh in range(4):
                P.dma("pool", lambda e, n4=n4, kh=kh: e.dma_start(out=wo4[:, kh * 4:(kh + 1) * 4, n4, :], in_=wsrc[:, kh * 4:(kh + 1) * 4, n4, :]),
                      f"wo{n4}", (), (f"wo{n4}",))
        gainb = SB(f"o_gain{l}", [128, D], F32, at=R_SZ)
        xt = SB(f"o_xt{l}", [128, D], F32, at=R_SZ + 8192)
        junk = SB(f"o_junk{l}", [128, 512], BF16, at=W0)
        st = SB(f"o_st{l}", [128, 8], F32, at=W0 + 1024)
        tmp = SB(f"o_tmp{l}", [128, 512], F32, at=W0 + 2048)
        load("sp", gainb[:], post_norm[l:l + 1, :].to_broadcast([128, D]), "ogain", (), ("gainb",))
        for i in range(8):
            load("sp", xt[:], x_src[i * 128:(i + 1) * 128, :], "oxt", (), ("xt",))
            for n4 in range(4):
                for k in range(16):
                    mm(ps[n4][:, :], mgT[:, k, i * 128:(i + 1) * 128], wo[:, k * 4 + n4, :], k == 0, k == 15, ("mgT", f"wo{n4}"), (f"ps{n4}",))
                act(junk[:], ps[n4][:, :], AF.Square, (f"ps{n4}",), ("ojunk", "ost"), accum=st[:, n4:n4 + 1])
            P.op("dve", lambda e: e.tensor_reduce(st[:, 4:5], st[:, 0:4], AX.X, ALU.add), ("ost",), ("ost",))
            ts("dve", st[:, 5:6], st[:, 4:5], 1.0 / D, 1e-6, ALU.mult, ALU.add, ("ost",), ("ost",))
            act(st[:, 6:7], st[:, 5:6], AF.Sqrt, ("ost",), ("ost",))
            P.op("dve", lambda e: e.reciprocal(st[:, 7:8], st[:, 6:7]), ("ost",), ("ost",))
            for n4 in range(4):
                cs_ = slice(n4 * 512, (n4 + 1) * 512)
                stt("dve", tmp[:], ps[n4][:, :], st[:, 7:8], gainb[:, cs_], ALU.mult, ALU.mult, (f"ps{n4}", "ost", "gainb"), ("otmp2",))
                tt("dve", xt[:, cs_], xt[:, cs_], tmp[:], ALU.add, ("otmp2", "xt"), ("xt",))
            load("sp", x_dst[i * 128:(i + 1) * 128, :], xt[:], "oxo", ("xt",), ("xdst",))
        P.barrier()

    def dump(name, ap, shape, dt):
        o = nc.dram_tensor("d_" + name, list(shape), dt, kind="ExternalOutput").ap()
        load("sp", o, ap, "dump", (), ("dump_" + name,))

    def program():
        for l in range(DEPTH):
            x_src = x_in if l == 0 else x_mid
            x_dst = x_mid if l == 0 else y_out
            phase_norm(l, x_src)
            P.barrier()
            if upto == f"norm{l}":
                dump("hT", hT, [128, 16, T], BF16)
                dump("ropeT", ropeT[:], [32, 2, T], F32)
                return
            pstop = upto[3:] if (upto or "").startswith(f"p1{l}") and len(upto) > 3 else None
            stbox = []
            phase_p1(l, pstop, hook=(lambda l=l: stbox.append(phase_ssm(l))) if pstop is None else None)
            P.barrier()
            if (upto or "").startswith(f"p1{l}"):
                dump("uTa", uTa[:, :, T:TT], [128, 8, T], BF16)
                if pstop != "a":
                    dump("gin_k", gin_c[1], [1024, T], BF16)
                if pstop not in ("a", "b"):
                    dump("gin_v", gin_vc[0], [512, D], BF16)
                if pstop is None:
                    dump("gout_k", gout_c[2], [2048, T], BF16)
                return
            st = stbox[0]
            load("sp", uTa[:, :, 0:T], gout_u[0:1024, :].rearrange("(k p) t -> p k t", p=128), "gul", ("gout_u",), ("uTa",))
            for kt8 in range(8):
                ts("dve", uTa[:, kt8, 0:T], uTa[:, kt8, 0:T], flags[:, 0:1], None, ALU.mult, None, ("uTa", "flags"), ("uTa",))
            P.barrier()
            if upto == f"ssmprep{l}":
                dump("prm", st[3][:], [128, 16, 32], F32)
                dump("bbT", st[2][:], [128, 2, 32, 128], BF16)
                dump("cpad", st[1][:], [128, 2, 32, 128], BF16)
                dump("uTa", uTa[:], [128, 8, TT], BF16)
                return
            phase_ssm_main(l, st)
            if upto == f"ssm{l}":
                dump("ygT", st[0][:], [128, 8, T], BF16)
                dump("osT", szT[:], [128, 8, T], BF16)
                return
            phase_attn(l)
            if upto == f"attn{l}":
                dump("oaT", oaT, [128, 16, T], BF16)
                dump("osT", szT[:], [128, 8, T], BF16)
                return
            mgT = phase_merge(l)
            if upto == f"merge{l}":
                dump("mgT", mgT, [128, 16, T], BF16)
                return
            phase_out(l, mgT, x_src, x_dst)
            if upto == f"out{l}":
                dump("x_mid", x_mid, [T, D], F32)
                return
    program()
    P.barrier()

    import contextlib
    with contextlib.ExitStack() as es:
        sems = {}
        for k in list(Plan.ENG) + P.slots:
            sems[k] = es.enter_context(nc.semaphore("s_" + k))
        es.enter_context(nc.allow_non_contiguous_dma(reason="small strided parameter loads"))
        block = es.enter_context(nc.Block())

        refd = {k: set() for k in Plan.ENG}
        for e in Plan.ENG:
            for o in P.ops[e]:
                if o[0] == "wait" and o[1] in refd:
                    refd[o[1]].add(o[2])
        for k in Plan.ENG:
            if P.cnt.get(k, 0) > 0:
                refd[k].add(P.cnt[k])
        newc = {k: {c: i + 1 for i, c in enumerate(sorted(v))} for k, v in refd.items()}

        def emit(eng_name):
            def f(eng):
                n = 0
                for o in P.ops[eng_name]:
                    if o[0] == "wait":
                        v = newc[o[1]][o[2]] if o[1] in newc else o[2]
                        eng.wait_ge(sems[o[1]], v)
                    elif o[2] in newc:
                        n += 1
                        ins = o[1](eng)
                        if n in refd[o[2]]:
                            ins.then_inc(sems[o[2]], 1)
                    else:
                        o[1](eng).then_inc(sems[o[2]], o[3])
                if eng_name == "sp":
                    for k, c in P.cnt.items():
                        eng.wait_ge(sems[k], newc[k][c] if k in newc else c)
            return f
        block.tensor(emit("pe"))
        block.scalar(emit("act"))
        block.vector(emit("dve"))
        block.gpsimd(emit("pool"))
        block.sync(emit("sp"))
    return nc


_NC = {}


def run(inputs, n_cores=8, upto=None):
    bf = ml_dtypes.bfloat16
    x = np.ascontiguousarray(inputs["x"], dtype=np.float32)
    identf = np.eye(128, dtype=np.float32)
    perm = np.zeros((128, 128), np.float32)
    for m in range(16):
        perm[m + 16, m] = -1.0
        perm[m, m + 16] = 1.0
    tri = np.triu(np.ones((128, 128), np.float32))
    ones = np.ones((128, 128), np.float32)
    selm = np.zeros((8, 8, 128), np.float32)
    for n in range(8):
        selm[n, n, :] = 1.0
    tpos = np.tile(np.arange(TT, dtype=np.float32)[None, :], (128, 1))
    vbl = np.zeros((128, 8, 8), np.float32)
    for i in range(8):
        for n in range(4, 8):
            if (n - 4) >= i // 2:
                vbl[:, i, n] = NEG
    ropei = np.zeros((128, 1), np.float32)
    ropei[:32, 0] = np.arange(32) % 16
    consts = {"c_identf": identf, "c_perm": perm.astype(bf), "c_tri": tri.astype(bf), "c_ones": ones.astype(bf),
              "c_selm": selm.reshape(8, 1024).astype(bf), "c_tpos": tpos, "c_vbl": vbl.reshape(128, 64), "c_ropei": ropei}
    wnames = ["pre_norm", "post_norm", "w_in", "lam_re", "lam_im", "log_dt", "b_re", "b_im", "c_re", "c_im",
              "d_skip", "w_glu", "b_glu", "w_br_ssm", "w_br_attn", "w_out"]
    shared = {k: np.ascontiguousarray(inputs[k], dtype=np.float32) for k in wnames}
    shared.update(consts)
    in_maps = []
    for c in range(n_cores):
        b, s = c // 2, c % 2
        fl = np.zeros((128, 4), np.float32)
        fl[:, 0] = float(s)
        fl[:, 1] = float(s * T)
        fl[:, 2] = (float(s) - 1.0) * 1.0e30
        m = dict(shared)
        m["x"] = np.ascontiguousarray(x[b, s * T:(s + 1) * T, :])
        m["c_flags"] = fl
        in_maps.append(m)
    key = (n_cores, upto)
    if key not in _NC:
        _NC[key] = build_nc(n_cores, upto)
    res = run_bass_kernel_spmd(_NC[key], in_maps, core_ids=list(range(n_cores)))
    return res.results


def kernel(**inputs):
    results = run(inputs, 8, None)
    out = np.zeros((4, 2048, D), np.float32)
    for c in range(8):
        b, s = c // 2, c % 2
        out[b, s * T:(s + 1) * T, :] = results[c]["y"]
    return out
```

```python
import math
import numpy as np
import ml_dtypes
import concourse.bass as bass
import concourse.mybir as mybir
from concourse.bass_utils import run_bass_kernel_spmd

F32 = mybir.dt.float32
BF16 = mybir.dt.bfloat16
ALU = mybir.AluOpType
AF = mybir.ActivationFunctionType
AX = mybir.AxisListType

D = 2048
T = 1024
TT = 2048
NH = 16
DEPTH = 2
INW = 14336
C_U, C_ZS, C_Q, C_K, C_V, C_ZA, C_GS, C_GA = 0, 1024, 2048, 4096, 6144, 8192, 10240, 12288
MAGIC = 12582912.0
TWO_PI = 2.0 * math.pi
BIG = 30000.0
NEG = -1.0e30


class Plan:
    ENG = ("pe", "act", "dve", "pool", "sp")

    def __init__(self):
        self.ops = {e: [] for e in self.ENG}
        self.cnt = {}
        self.known = {e: {} for e in self.ENG}
        self.res = {}
        self.slots = []

    def _deps(self, reads, writes):
        deps = {}

        def add(d):
            if d is None:
                return
            k, c = d
            if deps.get(k, 0) < c:
                deps[k] = c
        for k in reads:
            r = self.res.get(k)
            if r:
                add(r[0])
        for k in writes:
            r = self.res.get(k)
            if r:
                add(r[0])
                for kk, cc in r[1].items():
                    add((kk, cc))
        return deps

    def _waits(self, eng, deps, skip_self=False):
        for k, c in deps.items():
            if skip_self and k == eng:
                continue
            if self.known[eng].get(k, 0) >= c:
                continue
            self.known[eng][k] = c
            self.ops[eng].append(("wait", k, c))

    def _update(self, reads, writes, prod):
        for k in reads:
            r = self.res.setdefault(k, [None, {}])
            if r[1].get(prod[0], 0) < prod[1]:
                r[1][prod[0]] = prod[1]
        for k in writes:
            self.res[k] = [prod, {}]

    def op(self, eng, fn, reads=(), writes=()):
        deps = self._deps(reads, writes)
        self._waits(eng, deps, skip_self=(eng == "pe"))
        c = self.cnt.get(eng, 0) + 1
        self.cnt[eng] = c
        self.ops[eng].append(("op", fn, eng, 1))
        self._update(reads, writes, (eng, c))

    def dma(self, q, fn, slot, reads=(), writes=(), inc=16):
        deps = self._deps(reads, writes)
        self._waits(q, deps)
        if slot not in self.slots:
            self.slots.append(slot)
        c = self.cnt.get(slot, 0) + inc
        self.cnt[slot] = c
        self.ops[q].append(("op", fn, slot, inc))
        self._update(reads, writes, (slot, c))

    def barrier(self):
        soft = getattr(self, "soft", set())
        allk = {k: c for k, c in self.cnt.items() if k not in soft}
        for e in self.ENG:
            self._waits(e, allk, skip_self=True)
        self.res = {k: [v[0], {}] for k, v in self.res.items() if v[0] is not None and v[0][0] in soft}


def build_nc(n_cores=8, upto=None):
    nc = bass.Bass("TRN2", target_bir_lowering=False)
    P = Plan()

    def din(name, shape, dt=F32):
        return nc.dram_tensor(name, list(shape), dt, kind="ExternalInput").ap()

    x_in = din("x", [T, D])
    pre_norm = din("pre_norm", [DEPTH, D])
    post_norm = din("post_norm", [DEPTH, D])
    w_in = din("w_in", [DEPTH, D, INW])
    lam_re = din("lam_re", [DEPTH, 64, 64])
    lam_im = din("lam_im", [DEPTH, 64, 64])
    log_dt = din("log_dt", [DEPTH, 64])
    b_re = din("b_re", [DEPTH, 64, 64, 16])
    b_im = din("b_im", [DEPTH, 64, 64, 16])
    c_re = din("c_re", [DEPTH, 64, 16, 64])
    c_im = din("c_im", [DEPTH, 64, 16, 64])
    d_skip = din("d_skip", [DEPTH, 1024])
    w_glu = din("w_glu", [DEPTH, 1024, 1024])
    b_glu = din("b_glu", [DEPTH, 1024])
    w_br_ssm = din("w_br_ssm", [DEPTH, 1024, D])
    w_br_attn = din("w_br_attn", [DEPTH, D, D])
    w_out = din("w_out", [DEPTH, D, D])
    c_identf = din("c_identf", [128, 128])
    c_perm = din("c_perm", [128, 128], BF16)
    c_tri = din("c_tri", [128, 128], BF16)
    c_ones = din("c_ones", [128, 128], BF16)
    c_selm = din("c_selm", [8, 1024], BF16)
    c_tpos = din("c_tpos", [128, TT])
    c_vbl = din("c_vbl", [128, 64])
    c_ropei = din("c_ropei", [128, 1])
    c_flags = din("c_flags", [128, 4])
    y_out = nc.dram_tensor("y", [T, D], F32, kind="ExternalOutput").ap()

    def dint(name, shape, dt):
        return nc.dram_tensor(name, list(shape), dt).ap()
    x_mid = dint("x_mid", [T, D], F32)
    gin_c = [dint(f"gin{i}", [1024, T], BF16) for i in range(5)]
    gout_c = [dint(f"gout{i}", [2048, T], BF16) for i in range(5)]
    gin_u = gin_c[0]
    gout_u = gout_c[0]
    gin_vc = [gin_c[3 + c].rearrange("(t two) c -> t (two c)", two=2) for c in range(2)]
    gout_vc = [gout_c[3 + c][0:1024, :].rearrange("(t two) c -> t (two c)", two=2) for c in range(2)]

    def gk_rows(gl, h):
        return gl[1 + h // 8][(h % 8) * 128:(h % 8 + 1) * 128, :]

    off = [0]

    arena = nc.alloc_sbuf_tensor_at("arena", [128, 103 * 1024], BF16, offset=16640)

    def SB(name, shape, dt, at=None):
        esz = 4 if dt == F32 else 2
        nbytes = int(np.prod(shape[1:])) * esz
        nb = (nbytes + 63) // 64 * 64
        if at is None:
            at = off[0]
            off[0] += nb
        assert at % 4 == 0 and at + nb <= 206 * 1024, (name, at, nb)
        ap = arena[0:shape[0], at // 2:(at + nbytes) // 2]
        if dt == F32:
            ap = ap.bitcast(F32)
        if len(shape) == 3:
            ap = ap.rearrange("p (a b) -> p a b", a=shape[1])
        elif len(shape) == 4:
            ap = ap.rearrange("p (a b c) -> p a b c", a=shape[1], b=shape[2])
        return ap

    hoa = SB("hoa", [128, 2, 16, T], BF16)
    hT = hoa[:, 0]
    oaT = hoa[:, 1]
    OA0 = 32768
    uTa = SB("uTa", [128, 8, TT], BF16)
    szT = SB("szT", [128, 8, T], BF16)
    wbuf = SB("wbuf", [128, 2, 16, 512], BF16)
    identf = SB("identf", [128, 128], F32)
    permb = SB("permb", [128, 128], BF16)
    trib = SB("trib", [128, 128], BF16)
    onesb = SB("onesb", [128, 128], BF16)
    selm = SB("selm", [8, 1024], BF16)
    vbl = SB("vbl", [128, 64], F32)
    flags = SB("flags", [128, 4], F32)
    ropei = SB("ropei", [128, 1], F32)
    ropeT = SB("ropeT", [32, 2, T], F32)
    small = SB("small", [128, 64], F32)
    tpos_sb = SB("tpos_sb", [128, TT], F32)
    W0 = off[0]
    R_HT, R_OA, R_U, R_SZ = 0, 32768, 65536, 98304

    ps = [nc.alloc_psum_tensor(f"ps{i}", [128, 512], F32) for i in range(8)]

    def load(q, out_ap, in_ap, slot, reads=(), writes=()):
        P.dma(q, lambda e: e.dma_start(out=out_ap, in_=in_ap), slot, reads, writes)

    wslot_n = [0]

    def load_w(src_ap, kch, ncols):
        s = wslot_n[0] % 2
        wslot_n[0] += 1
        src = src_ap.rearrange("(k p) c -> p k c", p=128)
        for k0 in range(0, kch, 4):
            k1 = min(kch, k0 + 4)
            P.dma("pool", lambda e, d=wbuf[:, s, k0:k1, 0:ncols], sr=src[:, k0:k1, :]: e.dma_start(out=d, in_=sr),
                  f"w{s}", reads=(), writes=(f"wbuf{s}",))
        return s

    def mm(out_ap, lhsT, rhs, start, stop, reads, writes, tp=None):
        if tp is None:
            P.op("pe", lambda e: e.matmul(out_ap, lhsT, rhs, start=start, stop=stop), reads, writes)
        else:
            P.op("pe", lambda e: e.matmul(out_ap, lhsT, rhs, start=start, stop=stop, tile_position=tp),
                 reads, writes)

    def act(out_ap, in_ap, func, reads, writes, scale=1.0, bias=0.0, accum=None):
        if accum is None:
            P.op("act", lambda e: e.activation(out_ap, in_ap, func, bias=bias, scale=scale), reads, writes)
        else:
            P.op("act", lambda e: e.activation(out_ap, in_ap, func, bias=bias, scale=scale, accum_out=accum),
                 reads, writes)

    def tt(eng, out_ap, a, b, op, reads, writes):
        P.op(eng, lambda e: e.tensor_tensor(out_ap, a, b, op), reads, writes)

    def ts(eng, out_ap, a, s1, s2, op0, op1, reads, writes):
        if op1 is None:
            P.op(eng, lambda e: e.tensor_scalar(out_ap, a, s1, None, op0), reads, writes)
        else:
            P.op(eng, lambda e: e.tensor_scalar(out_ap, a, s1, s2, op0, op1), reads, writes)

    def stt(eng, out_ap, a, s, b, op0, op1, reads, writes):
        P.op(eng, lambda e: e.scalar_tensor_tensor(out_ap, a, s, b, op0, op1), reads, writes)

    def cp(eng, out_ap, in_ap, reads, writes):
        if eng == "act":
            P.op("act", lambda e: e.copy(out_ap, in_ap), reads, writes)
        else:
            P.op(eng, lambda e: e.tensor_copy(out_ap, in_ap), reads, writes)

    def fracs(eng, turns, tmp, keys):
        ts(eng, tmp, turns, MAGIC, -MAGIC, ALU.add, ALU.add, keys, keys)
        tt(eng, turns, turns, tmp, ALU.subtract, keys, keys)

    bankn = [0]
    nbanks = [4]

    def nb4():
        b = bankn[0] % nbanks[0]
        bankn[0] += 1
        return b

    for dst, src, nm in ((identf, c_identf, "identf"), (permb, c_perm, "permb"),
                         (trib, c_tri, "trib"), (onesb, c_ones, "onesb"), (selm, c_selm, "selm"),
                         (vbl, c_vbl, "vbl"), (flags, c_flags, "flags"), (ropei, c_ropei, "ropei"),
                         (tpos_sb, c_tpos, "tpos")):
        load("sp", dst[:], src, "c_" + nm, (), (nm,))
    rt1 = SB("rt1", [32, T], F32, at=R_OA)
    rt2 = SB("rt2", [32, T], F32, at=R_OA + 4096)
    act(small[0:32, 0:1], ropei[0:32, :], AF.Exp, ("ropei",), ("small",),
        scale=-math.log(500000.0) / 16.0, bias=-math.log(TWO_PI))
    ts("dve", rt1[:], tpos_sb[0:32, 0:T], flags[0:32, 1:2], small[0:32, 0:1], ALU.add, ALU.mult,
       ("tpos", "flags", "small"), ("rt",))
    fracs("dve", rt1[:], rt2[:], ("rt",))
    act(ropeT[:, 1, :], rt1[:], AF.Sin, ("rt",), ("ropeT",), scale=TWO_PI)
    act(rt2[:], rt1[:], AF.Abs, ("rt",), ("rt",))
    act(ropeT[:, 0, :], rt2[:], AF.Sin, ("rt",), ("ropeT",), scale=-TWO_PI, bias=math.pi / 2)
    P.barrier()

    def phase_norm(l, x_src):
        xt = [SB(f"n_xt{i}_{l}", [128, D], F32, at=R_U + i * 8192) for i in range(2)]
        hb = SB(f"n_hb_{l}", [128, D], F32, at=R_U + 16384)
        gainb = SB(f"n_gain_{l}", [128, D], F32, at=R_U + 24576)
        junk = SB(f"n_junk_{l}", [128, D], BF16, at=R_OA)
        st = SB(f"n_st_{l}", [128, 8], F32, at=R_OA + 4096)
        load("sp", gainb[:], pre_norm[l:l + 1, :].to_broadcast([128, D]), "gain", (), ("gainb",))
        for i in range(8):
            xb = xt[i % 2]
            xk = f"xt{i % 2}"
            load("sp", xb[:], x_src[i * 128:(i + 1) * 128, :], xk, (), (xk,))
            act(junk[:], xb[:], AF.Square, (xk,), ("junk", "st"), accum=st[:, 0:1])
            ts("dve", st[:, 1:2], st[:, 0:1], 1.0 / D, 1e-6, ALU.mult, ALU.add, ("st",), ("st",))
            act(st[:, 2:3], st[:, 1:2], AF.Sqrt, ("st",), ("st",))
            P.op("dve", lambda e: e.reciprocal(st[:, 3:4], st[:, 2:3]), ("st",), ("st",))
            stt("dve", hb[:], xb[:], st[:, 3:4], gainb[:], ALU.mult, ALU.mult, (xk, "st", "gainb"), ("hb",))
            for g4 in range(4):
                b = nb4()
                for j in range(4):
                    k = g4 * 4 + j
                    P.op("pe", lambda e, o=ps[b][:, j * 128:(j + 1) * 128], k=k: e.transpose(o, hb[:, k * 128:(k + 1) * 128], identf[:]),
                         ("hb", "identf"), (f"ps{b}",))
                cp("act" if g4 % 2 == 0 else "dve", hT[:, g4 * 4:(g4 + 1) * 4, i * 128:(i + 1) * 128],
                   ps[b][:, :].rearrange("p (j t) -> p j t", j=4), (f"ps{b}",), ("hT",))

    def proj_fm(s, ct, kch, rhsT, rkey, evac):
        for th in range(2):
            b = nb4()
            for k in range(kch):
                mm(ps[b][:, :], wbuf[:, s, k, ct * 128:(ct + 1) * 128], rhsT[:, k, th * 512:(th + 1) * 512],
                   k == 0, k == kch - 1, (f"wbuf{s}", rkey), (f"ps{b}",))
            evac(b, th)

    def rope(qf, qb, key):
        r1 = SB("rp_r1" + key, [32, 512], F32, at=W0)
        r2 = SB("rp_r2" + key, [32, 512], F32, at=W0 + 2048)
        cp("act", qb[:], qf[:], (key + "f",), (key + "b",))
        for th in range(2):
            sl = slice(th * 512, (th + 1) * 512)
            mm(ps[7][:, :], permb[:], qb[:, sl], True, True, ("permb", key + "b"), ("ps7",))
            tt("dve", r1[:], qf[0:32, sl], ropeT[:, 0, sl], ALU.mult, (key + "f", "ropeT"), ("rp1",))
            tt("dve", r2[:], ps[7][0:32, :], ropeT[:, 1, sl], ALU.mult, ("ps7", "ropeT"), ("rp2",))
            tt("dve", qf[0:32, sl], r1[:], r2[:], ALU.add, ("rp1", "rp2"), (key + "f",))
        cp("act", qb[0:32, :], qf[0:32, :], (key + "f",), (key + "b",))

    def phase_p1(l, stop=None, hook=None, hook2=None):
        wl = w_in[l]
        kf = SB(f"p1_kf{l}", [128, T], F32, at=R_OA)
        kb = SB(f"p1_kb{l}", [128, T], BF16, at=R_OA + 4096)
        vst = [SB(f"p1_vst{i}_{l}", [128, 512], BF16, at=R_OA + 6144 + i * 1024) for i in range(2)]
        kml = SB(f"p1_kml{l}", [128, 64], F32, at=R_OA + 8192)
        for g in range(2):
            s = load_w(wl[:, C_U + g * 512:C_U + (g + 1) * 512], 16, 512)
            for ct in range(4):
                kt = g * 4 + ct
                proj_fm(s, ct, 16, hT, "hT",
                        lambda b, th, kt=kt: cp("act", uTa[:, kt, T + th * 512:T + (th + 1) * 512], ps[b][:, :],
                                                (f"ps{b}",), ("uTa",)))
        load("sp", gin_u.rearrange("(k p) t -> p k t", p=128), uTa[:, :, T:TT], "gu", ("uTa",), ("gin_u",))
        if stop == "a":
            return

        def gather(ci, key):
            P.dma("pool", lambda e: e.collective_compute(
                "AllGather", ALU.bypass, replica_groups=[[2 * i, 2 * i + 1] for i in range(n_cores // 2)],
                ins=[gin_c[ci].opt()], outs=[gout_c[ci].opt()]), f"cc{ci}", reads=("gin_" + key,),
                writes=("gout_" + key,), inc=1)
        gather(0, "u")
        if hook is not None:
            hook()
        for g in range(4):
            s = load_w(wl[:, C_K + g * 512:C_K + (g + 1) * 512], 16, 512)
            for ct in range(4):
                h = g * 4 + ct
                proj_fm(s, ct, 16, hT, "hT",
                        lambda b, th: cp("act", kf[:, th * 512:(th + 1) * 512], ps[b][:, :], (f"ps{b}",), ("kff",)))
                rope(kf, kb, "kf")
                load("sp", gk_rows(gin_c, h), kb[:], "gk", ("kfb",), (f"gin_k{h // 8}",))
        if stop == "b":
            return
        if hook2 is not None:
            hook2()
        for g in range(4):
            s = load_w(wl[:, C_V + g * 512:C_V + (g + 1) * 512], 16, 512)
            for i in range(8):
                b = nb4()
                for k in range(16):
                    mm(ps[b][:, :], hT[:, k, i * 128:(i + 1) * 128], wbuf[:, s, k, 0:512], k == 0, k == 15,
                       (f"wbuf{s}", "hT"), (f"ps{b}",))
                vs = vst[i % 2]
                cp("act", vs[:], ps[b][:, :], (f"ps{b}",), (f"vst{i % 2}",))
                load("sp", gin_vc[i // 4][(i % 4) * 128:(i % 4 + 1) * 128, g * 512:(g + 1) * 512], vs[:], f"gv{i % 2}",
                     (f"vst{i % 2}",), (f"gin_v{i // 4}",))
        if stop == "c":
            return
        for ci, key in ((1, "k0"), (2, "k1"), (3, "v0"), (4, "v1")):
            gather(ci, key)
        P.soft = {"cc1", "cc2", "cc3", "cc4"}

    def phase_ssm(l):
        A = R_OA
        ygT = SB(f"s_yg{l}", [128, 8, T], BF16, at=A)
        cpad = SB(f"s_cpad{l}", [128, 2, 32, 128], BF16, at=A + 16384)
        B = W0
        bbT = SB(f"s_bbT{l}", [128, 2, 32, 128], BF16, at=B + 16384)
        prm = SB(f"s_prm{l}", [128, 16, 32], F32, at=B + 4096)
        P.op("pool", lambda e: e.memset(bbT[:], 0.0), (), ("bbT",))
        LR, LI, DT, MAG, FR, AR, AI, DEN, NRM, FRE, FIM, T1, T2, T3, T4, T5 = range(16)
        pv = lambda i: prm[:, i, :]
        for two in range(2):
            load("sp", prm[two * 64:(two + 1) * 64, LR, :], lam_re[l].rearrange("(pi two) p -> two p pi", two=2)[two], "pl", (), ("prm",))
            load("sp", prm[two * 64:(two + 1) * 64, LI, :], lam_im[l].rearrange("(pi two) p -> two p pi", two=2)[two], "pl", (), ("prm",))
            load("sp", prm[two * 64:(two + 1) * 64, DT, :],
                 log_dt[l:l + 1, :].rearrange("o (pi two) -> o two pi", two=2)[:, two, :].to_broadcast([64, 32]),
                 "pl", (), ("prm",))
        K = ("prm",)
        act(pv(DT), pv(DT), AF.Exp, K, K)
        tt("dve", pv(T1), pv(LR), pv(DT), ALU.mult, K, K)
        act(pv(MAG), pv(T1), AF.Exp, K, K)
        tt("dve", pv(T2), pv(LI), pv(DT), ALU.mult, K, K)
        ts("dve", pv(FR), pv(T2), 1.0 / TWO_PI, None, ALU.mult, None, K, K)
        fracs("dve", pv(FR), pv(T3), K)
        act(pv(T4), pv(FR), AF.Sin, K, K, scale=TWO_PI)
        act(pv(T3), pv(FR), AF.Abs, K, K)
        act(pv(T5), pv(T3), AF.Sin, K, K, scale=-TWO_PI, bias=math.pi / 2)
        tt("dve", pv(AR), pv(MAG), pv(T5), ALU.mult, K, K)
        tt("dve", pv(AI), pv(MAG), pv(T4), ALU.mult, K, K)
        tt("dve", pv(T1), pv(LR), pv(LR), ALU.mult, K, K)
        tt("dve", pv(T2), pv(LI), pv(LI), ALU.mult, K, K)
        tt("dve", pv(DEN), pv(T1), pv(T2), ALU.add, K, K)
        P.op("dve", lambda e: e.reciprocal(pv(DEN), pv(DEN)), K, K)
        ts("dve", pv(NRM), pv(AR), -1.0, None, ALU.add, None, K, K)
        tt("dve", pv(T1), pv(NRM), pv(LR), ALU.mult, K, K)
        tt("dve", pv(T2), pv(AI), pv(LI), ALU.mult, K, K)
        tt("dve", pv(T1), pv(T1), pv(T2), ALU.add, K, K)
        tt("dve", pv(FRE), pv(T1), pv(DEN), ALU.mult, K, K)
        tt("dve", pv(T1), pv(AI), pv(LR), ALU.mult, K, K)
        tt("dve", pv(T2), pv(NRM), pv(LI), ALU.mult, K, K)
        tt("dve", pv(T1), pv(T1), pv(T2), ALU.subtract, K, K)
        tt("dve", pv(FIM), pv(T1), pv(DEN), ALU.mult, K, K)
        bS = [SB(f"s_bS0_{l}", [128, 32, 32], F32, at=R_OA + 8192), SB(f"s_bS1_{l}", [128, 32, 32], F32, at=R_OA + 12288),
              SB(f"s_bS2_{l}", [128, 32, 32], F32, at=W0 + 32768), SB(f"s_bS3_{l}", [128, 32, 32], F32, at=W0 + 36864)]
        for i in range(2):
            P.op("dve", lambda e, t=bS[i]: e.memset(t[:], 0.0), (), (f"bS{i}",))
            src = (b_re, b_im)[i][l].rearrange("(pi two) p c -> two p pi c", two=2)
            for two in range(2):
                load("sp", bS[i][two * 64:(two + 1) * 64, :, two * 16:(two + 1) * 16], src[two], "pb", (), (f"bS{i}",))
        fre_b = pv(FRE).unsqueeze(2).to_broadcast([128, 32, 32])
        fim_b = pv(FIM).unsqueeze(2).to_broadcast([128, 32, 32])
        KB = ("bS0", "bS1", "bS2", "bS3", "prm")
        bbS = SB(f"s_bbS{l}", [128, 2, 32, 32], F32, at=W0 + 8192)
        tt("dve", bS[2][:], bS[0][:], fre_b, ALU.mult, KB, KB)
        tt("dve", bS[3][:], bS[1][:], fim_b, ALU.mult, KB, KB)
        tt("dve", bbS[:, 0], bS[2][:], bS[3][:], ALU.subtract, KB, ("bbS",))
        tt("dve", bS[2][:], bS[1][:], fre_b, ALU.mult, KB, KB)
        tt("dve", bS[3][:], bS[0][:], fim_b, ALU.mult, KB, KB)
        tt("dve", bbS[:, 1], bS[2][:], bS[3][:], ALU.add, KB + ("bbS",), ("bbS",))
        P.op("pool", lambda e: e.memset(cpad[:], 0.0), (), ("cpad",))
        cS = SB(f"s_cS_{l}", [32, 32, 128], F32, at=R_SZ)

        def load_cS(ri):
            P.op("dve", lambda e: e.memset(cS[:], 0.0), (), ("cS",))
            src = (c_re, c_im)[ri][l].rearrange("(pi two) c p -> two c pi p", two=2)
            for two in range(2):
                load("sp", cS[two * 16:(two + 1) * 16, :, two * 64:(two + 1) * 64], src[two], "pc", (), ("cS",))
        load_cS(0)

        def part_b():
            n = 0
            for ri in range(2):
                for kt in range(8):
                    bk = 4 + n % 4
                    key = f"ps{bk}"
                    n += 1
                    P.op("pe", lambda e, ri=ri, kt=kt, bk=bk: e.transpose(ps[bk][:, 0:128], bbS[:, ri, kt * 4:(kt + 1) * 4, :].rearrange("p a b -> p (a b)"), identf[:]),
                         ("bbS", "identf"), (key,))
                    for a4 in range(4):
                        cp("act", bbT[32 * a4:32 * a4 + 32, ri, kt * 4 + a4, :], ps[bk][32 * a4:32 * a4 + 32, 0:128], (key,), ("bbT",))
            for ri in range(2):
                if ri == 1:
                    load_cS(1)
                for pi in range(32):
                    bk = 4 + pi % 4
                    col = 0
                    key = f"ps{bk}"
                    P.op("pe", lambda e, pi=pi, bk=bk, col=col: e.transpose(ps[bk][:, col:col + 32], cS[:, pi, :], identf[0:32, 0:32]),
                         ("cS", "identf"), (key,))
                    j = pi % 4
                    act(cpad[:, ri, pi, j * 32:(j + 1) * 32], ps[bk][:, col:col + 32], AF.Copy, (key,), ("cpad",),
                        scale=(1.0 if ri == 0 else -1.0))
        dsk = SB(f"s_dsk{l}", [128, 8], F32, at=B + 6144)
        load("sp", dsk[:], d_skip[l].rearrange("(k p) -> p k", p=128), "pd", (), ("dsk",))
        return (ygT, cpad, bbT, prm, dsk, None, (FR, MAG)), part_b

    def phase_ssm_main(l, st):
        ygT, cpad, bbT, prm, dsk, tl, (FR, MAG) = st
        Mb = R_SZ
        HT = 1024

        def F(name, dt, o):
            return SB(f"s_{name}{l}", [128, HT], dt, at=Mb + o)
        trn = [F("trn0", F32, 0), F("trn1", F32, 4096)]
        ttmp = F("ttmp", F32, 8192)
        af = F("af", F32, 12288)
        sn = [F("sn0", BF16, 16384), F("sn1", BF16, 18432)]
        cs = [F("cs0", BF16, 20480), F("cs1", BF16, 22528)]
        bre = [F("bre0", BF16, 24576), F("bre1", BF16, 26624)]
        bim = [F("bim0", BF16, 28672), F("bim1", BF16, 30720)]
        q1 = F("q1", BF16, 32768); q2 = F("q2", BF16, 34816); q3 = F("q3", BF16, 36864)
        xr = [F("xr0", BF16, 38912), F("xr1", BF16, 40960)]
        xi = [F("xi0", BF16, 43008), F("xi1", BF16, 45056)]
        dim = F("dim", BF16, 47104)
        yt = SB(f"s_yt{l}", [128, 512], F32, at=W0 + 32768)
        y2 = SB(f"s_y2{l}", [128, 512], F32, at=W0 + 32768 + 2048)
        y3 = SB(f"s_y3{l}", [128, 512], F32, at=W0 + 32768 + 4096)
        dec = SB(f"s_dec{l}", [128, 32], F32, at=W0 + 8192 + 128)
        cp("dve", dec[:], prm[:, MAG, :], ("prm",), ("dec",))
        its = [(pi, Hh) for pi in range(32) for Hh in range(2)]
        qn = [0]

        def frac_of(n):
            pi, Hh = its[n]
            tr, tk = trn[n % 2], f"trn{n % 2}"
            ts("dve", tr[:], tpos_sb[:, Hh * HT:(Hh + 1) * HT], prm[:, FR, pi:pi + 1], None, ALU.mult, None, ("tpos", "prm"), (tk,))
            ts("dve", ttmp[:], tr[:], MAGIC, -MAGIC, ALU.add, ALU.add, (tk,), ("ttmp",))
            tt("dve", tr[:], tr[:], ttmp[:], ALU.subtract, (tk, "ttmp"), (tk,))

        frac_of(0)
        for n, (pi, Hh) in enumerate(its):
            kt, j = pi // 4, pi % 4
            b2 = n % 2
            tr, tk = trn[b2], f"trn{b2}"
            snb, csb, breb, bimb = sn[b2], cs[b2], bre[b2], bim[b2]
            ks, kc, kbr, kbi = f"sn{b2}", f"cs{b2}", f"bre{b2}", f"bim{b2}"
            for qq in range(2):
                sl = slice(Hh * HT + qq * 512, Hh * HT + (qq + 1) * 512)
                pb = (qn[0] % 2) * 2
                qn[0] += 1
                mm(ps[pb][:, :], bbT[:, 0, pi, :], uTa[:, kt, sl], True, True, ("bbT", "uTa"), (f"ps{pb}",))
                mm(ps[pb + 1][:, :], bbT[:, 1, pi, :], uTa[:, kt, sl], True, True, ("bbT", "uTa"), (f"ps{pb + 1}",))
                cp("act", breb[:, qq * 512:(qq + 1) * 512], ps[pb][:, :], (f"ps{pb}",), (kbr,))
                cp("act", bimb[:, qq * 512:(qq + 1) * 512], ps[pb + 1][:, :], (f"ps{pb + 1}",), (kbi,))
            act(snb[:], tr[:], AF.Sin, (tk,), (ks,), scale=TWO_PI)
            act(af[:], tr[:], AF.Abs, (tk,), ("af",))
            act(csb[:], af[:], AF.Sin, ("af",), (kc,), scale=-TWO_PI, bias=math.pi / 2)
            if n + 1 < len(its):
                frac_of(n + 1)
            tt("dve", q1[:], breb[:], csb[:], ALU.mult, (kbr, kc), ("q1",))
            tt("dve", q2[:], bimb[:], snb[:], ALU.mult, (kbi, ks), ("q2",))
            tt("dve", q1[:], q1[:], q2[:], ALU.add, ("q1", "q2"), ("q1",))
            tt("dve", q3[:], bimb[:], csb[:], ALU.mult, (kbi, kc), ("q3",))
            tt("dve", q2[:], breb[:], snb[:], ALU.mult, (kbr, ks), ("q2",))
            tt("dve", q3[:], q3[:], q2[:], ALU.subtract, ("q3", "q2"), ("q3",))
            dcy = dec[:, pi:pi + 1].to_broadcast([128, HT])
            xrc, xic = xr[Hh], xi[Hh]
            if Hh == 0:
                P.op("dve", lambda e, xrc=xrc, dcy=dcy: e.tensor_tensor_scan(xrc[:], dcy, q1[:], 0.0, ALU.mult, ALU.add),
                     ("q1", "dec"), ("xr0",))
                P.op("dve", lambda e, xic=xic, dcy=dcy: e.tensor_tensor_scan(xic[:], dcy, q3[:], 0.0, ALU.mult, ALU.add),
                     ("q3", "dec"), ("xi0",))
            else:
                P.op("dve", lambda e, xrc=xrc, dcy=dcy: e.tensor_tensor_scan(xrc[:], dcy, q1[:], xr[0][:, HT - 1:HT], ALU.mult, ALU.add),
                     ("q1", "dec", "xr0"), ("xr1",))
                P.op("dve", lambda e, xic=xic, dcy=dcy: e.tensor_tensor_scan(xic[:], dcy, q3[:], xi[0][:, HT - 1:HT], ALU.mult, ALU.add),
                     ("q3", "dec", "xi0"), ("xi1",))
                tt("dve", q1[:], xrc[:], csb[:], ALU.mult, ("xr1", kc), ("q1",))
                tt("dve", q3[:], xic[:], snb[:], ALU.mult, ("xi1", ks), ("q3",))
                tt("dve", q2[:], q1[:], q3[:], ALU.subtract, ("q1", "q3"), ("q2",))
                tt("dve", q1[:], xrc[:], snb[:], ALU.mult, ("xr1", ks), ("q1",))
                tt("dve", q3[:], xic[:], csb[:], ALU.mult, ("xi1", kc), ("q3",))
                tt("dve", dim[:], q1[:], q3[:], ALU.add, ("q1", "q3"), ("dim",))
                for qq in range(2):
                    yb, yk = ps[4 + qq], f"ps{4 + qq}"
                    csl = slice(qq * 512, (qq + 1) * 512)
                    mm(yb[:, :], cpad[:, 0, pi, :], q2[:, csl], j == 0, False, ("cpad", "q2"), (yk,))
                    mm(yb[:, :], cpad[:, 1, pi, :], dim[:, csl], False, j == 3, ("cpad", "dim"), (yk,))
                    if j == 3:
                        tok = slice(qq * 512, (qq + 1) * 512)
                        stt("dve", yt[:], uTa[:, kt, T + qq * 512:T + (qq + 1) * 512], dsk[:, kt:kt + 1], yb[:, :],
                            ALU.mult, ALU.add, ("uTa", "dsk", yk), ("yt",))
                        act(y2[:], yt[:], AF.Square, ("yt",), ("y2",))
                        ts("dve", y2[:], y2[:], 0.044715, 1.0, ALU.mult, ALU.add, ("y2",), ("y2",))
                        tt("dve", y2[:], y2[:], yt[:], ALU.mult, ("y2", "yt"), ("y2",))
                        act(y3[:], y2[:], AF.Sigmoid, ("y2",), ("y3",), scale=2.0 * math.sqrt(2.0 / math.pi))
                        tt("dve", ygT[:, kt, tok], yt[:], y3[:], ALU.mult, ("yt", "y3"), ("ygT",))
        P.barrier()
        wl = w_in[l]
        for g in range(2):
            s = load_w(wl[:, C_ZS + g * 512:C_ZS + (g + 1) * 512], 16, 512)
            for ct in range(4):
                kt = g * 4 + ct
                proj_fm(s, ct, 16, hT, "hT",
                        lambda b, th, kt=kt: act(szT[:, kt, th * 512:(th + 1) * 512], ps[b][:, :], AF.Silu,
                                                 (f"ps{b}",), ("szT",)))
        bg = SB(f"s_bg{l}", [128, 8], F32, at=W0 + 8192 + 256)
        load("sp", bg[:], b_glu[l].rearrange("(k p) -> p k", p=128), "pbg", (), ("bg",))
        gl = SB(f"s_gl{l}", [128, 512], BF16, at=W0 + 8192 + 512)
        for g in range(2):
            s = load_w(w_glu[l][:, g * 512:(g + 1) * 512], 8, 512)
            for ct in range(4):
                kt = g * 4 + ct
                def ev(b, th, kt=kt):
                    act(gl[:], ps[b][:, :], AF.Sigmoid, (f"ps{b}", "bg"), ("gl",), bias=bg[:, kt:kt + 1])
                    tt("dve", gl[:], gl[:], ygT[:, kt, th * 512:(th + 1) * 512], ALU.mult, ("gl", "ygT"), ("gl",))
                    tt("dve", szT[:, kt, th * 512:(th + 1) * 512], szT[:, kt, th * 512:(th + 1) * 512], gl[:], ALU.mult,
                       ("gl", "szT"), ("szT",))
                proj_fm(s, ct, 8, ygT, "ygT", ev)
        P.barrier()

    def phase_attn(l):
        wl = w_in[l]
        U = R_U
        kTh = [SB(f"a_kT{i}_{l}", [128, TT], BF16, at=U + i * 4096) for i in range(2)]
        vh = [SB(f"a_vh{i}_{l}", [128, 16, 128], BF16, at=U + 8192 + i * 4096) for i in range(2)]
        qf2 = [SB(f"a_qf{i}_{l}", [128, T], F32, at=U + 16384 + i * 4096) for i in range(2)]
        qb2 = [SB(f"a_qb{i}_{l}", [128, T], BF16, at=U + 24576 + i * 2048) for i in range(2)]
        sza2 = [SB(f"a_sza{i}_{l}", [128, T], BF16, at=U + 28672 + i * 2048) for i in range(2)]
        V = W0 + 8192
        pT = [SB(f"a_pT{i}_{l}", [128, 256], BF16, at=V + i * 512) for i in range(4)]
        mbT2 = [SB(f"a_mbT{i}_{l}", [8, T], BF16, at=V + 2048 + i * 2048) for i in range(2)]
        kmT = SB(f"a_kmT{l}", [128, 16, 8], F32, at=V + 6144)
        gsb = SB(f"a_g{l}", [128, 8, 8], F32, at=V + 6656)
        vb = SB(f"a_vb{l}", [128, 8, 8], F32, at=V + 6912)
        top8 = SB(f"a_top{l}", [128, 8, 8], F32, at=V + 7168)
        thr = SB(f"a_thr{l}", [128, 8], F32, at=V + 7424)
        mb = SB(f"a_mb{l}", [128, 8, 8], F32, at=V + 7488)
        rden = SB(f"a_rden{l}", [128, 256], F32, at=V + 7744)
        otmp = SB(f"a_otmp{l}", [128, 256], F32, at=V + 8768)
        acc2 = [SB(f"a_acc{i}_{l}", [128, 256], F32, at=V + 9792 + i * 1024) for i in range(2)]
        onesf = SB(f"a_onesf{l}", [128, 128], F32, at=V + 11840)
        P.op("dve", lambda e: e.memset(onesf[:], 1.0), (), ("onesf",))
        cp("dve", vb[:], vbl[:, :].rearrange("p (i n) -> p i n", n=8), ("vbl",), ("vb",))
        ts("dve", vb[:, :, 0:4], vb[:, :, 0:4], flags[:, 2:3], None, ALU.add, None, ("vb", "flags"), ("vb",))
        scale = 1.0 / math.sqrt(128.0)
        pn = [0]
        nbanks[0] = 2
        wsl = {}

        def prologue(h):
            g, ct = h // 4, h % 4
            hb = h % 2
            qf, qb, sza, mbT = qf2[hb], qb2[hb], sza2[hb], mbT2[hb]
            kq, ksz, kmb = f"qf{hb}", f"sza{hb}", f"mbT{hb}"
            kt_, vt_ = kTh[hb], vh[hb]
            kk, vk = f"kTh{hb}", f"vh{hb}"

            def s0():
                if ct == 0:
                    wsl["q"] = load_w(wl[:, C_Q + g * 512:C_Q + (g + 1) * 512], 16, 512)
                    wsl["z"] = load_w(wl[:, C_ZA + g * 512:C_ZA + (g + 1) * 512], 16, 512)
                load("sp", kt_[:, 0:T], gk_rows(gout_c, h), kk + "d", (f"gout_k{h // 8}",), (kk,))
                load("sp", kt_[:, T:TT], gk_rows(gin_c, h), kk + "d", (f"gin_k{h // 8}",), (kk,))
                for c2 in range(2):
                    load("sp", vt_[:, c2 * 4:c2 * 4 + 4, :], gout_vc[c2][:, h * 128:(h + 1) * 128].rearrange("(i p) c -> p i c", p=128),
                         vk + "d", (f"gout_v{c2}",), (vk,))
                    load("sp", vt_[:, 8 + c2 * 4:12 + c2 * 4, :], gin_vc[c2][:, h * 128:(h + 1) * 128].rearrange("(i p) c -> p i c", p=128),
                         vk + "d", (f"gin_v{c2}",), (vk,))
                proj_fm(wsl["q"], ct, 16, hT, "hT",
                        lambda b, th: cp("act", qf[:, th * 512:(th + 1) * 512], ps[b][:, :], (f"ps{b}",), (kq + "f",)))

            def s1():
                rope(qf, qb, kq)

            def s2():
                proj_fm(wsl["z"], ct, 16, hT, "hT",
                        lambda b, th: act(sza[:, th * 512:(th + 1) * 512], ps[b][:, :], AF.Silu, (f"ps{b}",), (ksz,)))
                P.op("dve", lambda e: e.tensor_reduce(kmT[:, h, :], kt_[:, :].rearrange("p (n j) -> p n j", n=8),
                                                      AX.X, ALU.add), (kk,), ("kmT",))
                for i in range(8):
                    mm(ps[7][:, i * 8:(i + 1) * 8], qf[:, i * 128:(i + 1) * 128], kmT[:, h, :], True, True, (kq + "f", "kmT"), ("ps7",))

            def s3():
                tt("dve", gsb[:], ps[7][:, 0:64].rearrange("p (i n) -> p i n", n=8), vb[:], ALU.add, ("ps7", "vb"), ("gsb",))
                for i in range(8):
                    P.op("dve", lambda e, i=i: e.max(top8[:, i, :], gsb[:, i, :]), ("gsb",), ("top8",))
                ts("dve", thr[:], top8[:, :, 2], -1.0e29, None, ALU.max, None, ("top8",), ("thr",))
                tt("dve", mb[:], gsb[:], thr[:].unsqueeze(2).to_broadcast([128, 8, 8]), ALU.is_ge, ("gsb", "thr"), ("mb",))
                ts("dve", mb[:], mb[:], -1.0, BIG / scale, ALU.add, ALU.mult, ("mb",), ("mb",))

            def s4():
                for hf in range(2):
                    for ii in range(4):
                        i = hf * 4 + ii
                        P.op("pe", lambda e, i=i, ii=ii: e.transpose(ps[6][0:8, ii * 128:(ii + 1) * 128], mb[:, i, :], identf[:]),
                             ("mb", "identf"), ("ps6",))
                    cp("act", mbT[:, hf * 512:(hf + 1) * 512], ps[6][0:8, :], ("ps6",), (kmb,))
            return [s0, s1, s2, s3, s4]

        def attention(h, hooks):
            hb = h % 2
            qb, sza, mbT = qb2[hb], sza2[hb], mbT2[hb]
            kq, ksz, kmb = f"qf{hb}", f"sza{hb}", f"mbT{hb}"
            kt_, vt_ = kTh[hb], vh[hb]
            kk, vk = f"kTh{hb}", f"vh{hb}"
            tl = []
            for jb in range(4):
                tiles = [(n, a) for n in range(4 + jb + 1) for a in range(2)]
                for idx, (n, a) in enumerate(tiles):
                    tl.append((jb, n, a, idx == 0, idx == len(tiles) - 1))
            info = {}

            def emit_s(t):
                jb, n, a, first, last = tl[t]
                kt = n * 2 + a
                own = (n == 4 + jb)
                q0 = 128 if (own and a == 1) else 0
                nq = 256 - q0
                sp_ = ps[4 + pn[0] % 2][:, 0:nq]
                sk = f"ps{4 + pn[0] % 2}"
                pt = pT[pn[0] % 4]
                pk = f"pT{pn[0] % 4}"
                pn[0] += 1
                mm(sp_, kt_[:, kt * 128:(kt + 1) * 128], qb[:, jb * 256 + q0:(jb + 1) * 256], True, own, (kk, kq + "b"), (sk,))
                if not own:
                    mm(sp_, selm[0:8, n * 128:(n + 1) * 128], mbT[0:8, jb * 256 + q0:(jb + 1) * 256], False, True,
                       ("selm", kmb), (sk,))
                act(pt[:, 0:nq], sp_, AF.Exp, (sk,), (pk,), scale=scale)
                if own:
                    tt("dve", pt[:, 0:128], pt[:, 0:128], trib[:], ALU.mult, (pk, "trib"), (pk,))
                ac, ak = acc2[jb % 2], f"acc{jb % 2}"
                if first:
                    cp("dve", ac[:, :], pt[:, 0:256], (pk,), (ak,))
                else:
                    tt("dve", ac[:, q0:256], ac[:, q0:256], pt[:, 0:nq], ALU.add, (ak, pk), (ak,))
                info[t] = (pt, pk, q0, nq, kt)

            def emit_pv(t):
                jb, n, a, first, last = tl[t]
                pt, pk, q0, nq, kt = info.pop(t)
                bank = ps[2 + jb % 2]
                bk = f"ps{2 + jb % 2}"
                oT = bank[:, 0:256]
                dn = bank[:, 256:512]
                mm(oT[:, q0:256], vt_[:, kt, :], pt[:, 0:nq], first, False, (vk, pk), (bk,))
                if last:
                    mm(dn, onesf[:], acc2[jb % 2][:, :], False, True, ("onesf", f"acc{jb % 2}"), (bk,))
                    qs = slice(jb * 256, (jb + 1) * 256)
                    P.op("dve", lambda e, dn=dn: e.reciprocal(rden[:], dn), (bk,), ("rden",))
                    tt("dve", otmp[:], oT, rden[:], ALU.mult, (bk, "rden"), ("otmp",))
                    tt("dve", oaT[:, h, qs], otmp[:], sza[:, qs], ALU.mult, ("otmp", ksz), ("oaT",))
                    if hooks:
                        hooks.pop(0)()

            emit_s(0)
            for t in range(len(tl)):
                if t + 1 < len(tl):
                    emit_s(t + 1)
                emit_pv(t)
            while hooks:
                hooks.pop(0)()

        for st_ in prologue(0):
            st_()
        for h in range(NH):
            hooks = []
            if h + 1 < NH:
                stg = prologue(h + 1)
                stg[0]()
                hooks = stg[1:]
            attention(h, hooks)
        nbanks[0] = 4
        P.soft = set()
        P.barrier()

    def phase_merge(l):
        wl = w_in[l]
        mgT = uTa[:, :, :].rearrange("p k (a t) -> p (k a) t", a=2)
        sg = [SB(f"m_sg{i}_{l}", [128, 4, T], BF16, at=W0 + i * 8192) for i in range(2)]
        m1 = SB(f"m_m1{l}", [128, 512], F32, at=W0 + 16384)
        for g in range(4):
            for which, c0 in ((0, C_GS), (1, C_GA)):
                s = load_w(wl[:, c0 + g * 512:c0 + (g + 1) * 512], 16, 512)
                for ct in range(4):
                    proj_fm(s, ct, 16, hT, "hT",
                            lambda b, th, ct=ct, which=which: act(sg[which][:, ct, th * 512:(th + 1) * 512], ps[b][:, :], AF.Sigmoid,
                                                                  (f"ps{b}",), (f"sg{which}",)))
            s = load_w(w_br_ssm[l][:, g * 512:(g + 1) * 512], 8, 512)
            for ct in range(4):
                proj_fm(s, ct, 8, szT, "szT",
                        lambda b, th, ct=ct: tt("dve", mgT[:, g * 4 + ct, th * 512:(th + 1) * 512], ps[b][:, :],
                                                sg[0][:, ct, th * 512:(th + 1) * 512], ALU.mult, (f"ps{b}", "sg0"), ("mgT",)))
            s = load_w(w_br_attn[l][:, g * 512:(g + 1) * 512], 16, 512)
            for ct in range(4):
                def ev(b, th, ct=ct):
                    dst = mgT[:, g * 4 + ct, th * 512:(th + 1) * 512]
                    tt("dve", m1[:], ps[b][:, :], sg[1][:, ct, th * 512:(th + 1) * 512], ALU.mult, (f"ps{b}", "sg1"), ("m1",))
                    tt("dve", dst, dst, m1[:], ALU.add, ("m1", "mgT"), ("mgT",))
                proj_fm(s, ct, 16, oaT, "oaT", ev)
        P.barrier()
        return mgT

    def phase_out(l, mgT, x_src, x_dst):
        wo = hoa[:, :, :, :].rearrange("p a k (n c) -> p (a k n) c", c=512)
        wsrc = w_out[l].rearrange("(k p) (n c) -> p k n c", p=128, c=512)
        wo4 = wo.rearrange("p (k n) c -> p k n c", n=4)
        for n4 in range(4):
            for kh in range(4):
                P.dma("pool", lambda e, n4=n4, kh=kh: e.dma_start(out=wo4[:, kh * 4:(kh + 1) * 4, n4, :], in_=wsrc[:, kh * 4:(kh + 1) * 4, n4, :]),
                      f"wo{n4}", (), (f"wo{n4}",))
        gainb = SB(f"o_gain{l}", [128, D], F32, at=R_SZ)
        xt = SB(f"o_xt{l}", [128, D], F32, at=R_SZ + 8192)
        junk = SB(f"o_junk{l}", [128, 512], BF16, at=W0)
        st = SB(f"o_st{l}", [128, 8], F32, at=W0 + 1024)
        tmp = SB(f"o_tmp{l}", [128, 512], F32, at=W0 + 2048)
        load("sp", gainb[:], post_norm[l:l + 1, :].to_broadcast([128, D]), "ogain", (), ("gainb",))
        for i in range(8):
            load("sp", xt[:], x_src[i * 128:(i + 1) * 128, :], "oxt", (), ("xt",))
            for n4 in range(4):
                for k in range(16):
                    mm(ps[n4][:, :], mgT[:, k, i * 128:(i + 1) * 128], wo[:, k * 4 + n4, :], k == 0, k == 15, ("mgT", f"wo{n4}"), (f"ps{n4}",))
                act(junk[:], ps[n4][:, :], AF.Square, (f"ps{n4}",), ("ojunk", "ost"), accum=st[:, n4:n4 + 1])
            P.op("dve", lambda e: e.tensor_reduce(st[:, 4:5], st[:, 0:4], AX.X, ALU.add), ("ost",), ("ost",))
            ts("dve", st[:, 5:6], st[:, 4:5], 1.0 / D, 1e-6, ALU.mult, ALU.add, ("ost",), ("ost",))
            act(st[:, 6:7], st[:, 5:6], AF.Sqrt, ("ost",), ("ost",))
            P.op("dve", lambda e: e.reciprocal(st[:, 7:8], st[:, 6:7]), ("ost",), ("ost",))
            for n4 in range(4):
                cs_ = slice(n4 * 512, (n4 + 1) * 512)
                stt("dve", tmp[:], ps[n4][:, :], st[:, 7:8], gainb[:, cs_], ALU.mult, ALU.mult, (f"ps{n4}", "ost", "gainb"), ("otmp2",))
                tt("dve", xt[:, cs_], xt[:, cs_], tmp[:], ALU.add, ("otmp2", "xt"), ("xt",))
            load("sp", x_dst[i * 128:(i + 1) * 128, :], xt[:], "oxo", ("xt",), ("xdst",))
        P.barrier()

    def dump(name, ap, shape, dt):
        o = nc.dram_tensor("d_" + name, list(shape), dt, kind="ExternalOutput").ap()
        load("sp", o, ap, "dump", (), ("dump_" + name,))

    def program():
        for l in range(DEPTH):
            x_src = x_in if l == 0 else x_mid
            x_dst = x_mid if l == 0 else y_out
            phase_norm(l, x_src)
            P.barrier()
            if upto == f"norm{l}":
                dump("hT", hT, [128, 16, T], BF16)
                dump("ropeT", ropeT[:], [32, 2, T], F32)
                return
            pstop = upto[3:] if (upto or "").startswith(f"p1{l}") and len(upto) > 3 else None
            stbox = []
            phase_p1(l, pstop, hook=(lambda l=l: stbox.append(phase_ssm(l))) if pstop is None else None,
                     hook2=(lambda: stbox[0][1]()) if pstop is None else None)
            P.barrier()
            if (upto or "").startswith(f"p1{l}"):
                dump("uTa", uTa[:, :, T:TT], [128, 8, T], BF16)
                if pstop != "a":
                    dump("gin_k", gin_c[1], [1024, T], BF16)
                if pstop not in ("a", "b"):
                    dump("gin_v", gin_vc[0], [512, D], BF16)
                if pstop is None:
                    dump("gout_k", gout_c[2], [2048, T], BF16)
                return
            st = stbox[0][0]
            load("sp", uTa[:, :, 0:T], gout_u[0:1024, :].rearrange("(k p) t -> p k t", p=128), "gul", ("gout_u",), ("uTa",))
            for kt8 in range(8):
                ts("dve", uTa[:, kt8, 0:T], uTa[:, kt8, 0:T], flags[:, 0:1], None, ALU.mult, None, ("uTa", "flags"), ("uTa",))
            P.barrier()
            if upto == f"ssmprep{l}":
                dump("prm", st[3][:], [128, 16, 32], F32)
                dump("bbT", st[2][:], [128, 2, 32, 128], BF16)
                dump("cpad", st[1][:], [128, 2, 32, 128], BF16)
                dump("uTa", uTa[:], [128, 8, TT], BF16)
                return
            phase_ssm_main(l, st)
            if upto == f"ssm{l}":
                dump("ygT", st[0][:], [128, 8, T], BF16)
                dump("osT", szT[:], [128, 8, T], BF16)
                return
            phase_attn(l)
            if upto == f"attn{l}":
                dump("oaT", oaT, [128, 16, T], BF16)
                dump("osT", szT[:], [128, 8, T], BF16)
                return
            mgT = phase_merge(l)
            if upto == f"merge{l}":
                dump("mgT", mgT, [128, 16, T], BF16)
                return
            phase_out(l, mgT, x_src, x_dst)
            if upto == f"out{l}":
                dump("x_mid", x_mid, [T, D], F32)
                return
    program()
    P.barrier()

    import contextlib
    with contextlib.ExitStack() as es:
        sems = {}
        for k in list(Plan.ENG) + P.slots:
            sems[k] = es.enter_context(nc.semaphore("s_" + k))
        es.enter_context(nc.allow_non_contiguous_dma(reason="small strided parameter loads"))
        block = es.enter_context(nc.Block())

        refd = {k: set() for k in Plan.ENG}
        for e in Plan.ENG:
            for o in P.ops[e]:
                if o[0] == "wait" and o[1] in refd:
                    refd[o[1]].add(o[2])
        for k in Plan.ENG:
            if P.cnt.get(k, 0) > 0:
                refd[k].add(P.cnt[k])
        newc = {k: {c: i + 1 for i, c in enumerate(sorted(v))} for k, v in refd.items()}

        def emit(eng_name):
            def f(eng):
                n = 0
                for o in P.ops[eng_name]:
                    if o[0] == "wait":
                        v = newc[o[1]][o[2]] if o[1] in newc else o[2]
                        eng.wait_ge(sems[o[1]], v)
                    elif o[2] in newc:
                        n += 1
                        ins = o[1](eng)
                        if n in refd[o[2]]:
                            ins.then_inc(sems[o[2]], 1)
                    else:
                        o[1](eng).then_inc(sems[o[2]], o[3])
                if eng_name == "sp":
                    for k, c in P.cnt.items():
                        eng.wait_ge(sems[k], newc[k][c] if k in newc else c)
            return f
        block.tensor(emit("pe"))
        block.scalar(emit("act"))
        block.vector(emit("dve"))
        block.gpsimd(emit("pool"))
        block.sync(emit("sp"))
    return nc


_NC = {}


def run(inputs, n_cores=8, upto=None):
    bf = ml_dtypes.bfloat16
    x = np.ascontiguousarray(inputs["x"], dtype=np.float32)
    identf = np.eye(128, dtype=np.float32)
    perm = np.zeros((128, 128), np.float32)
    for m in range(16):
        perm[m + 16, m] = -1.0
        perm[m, m + 16] = 1.0
    tri = np.triu(np.ones((128, 128), np.float32))
    ones = np.ones((128, 128), np.float32)
    selm = np.zeros((8, 8, 128), np.float32)
    for n in range(8):
        selm[n, n, :] = 1.0
    tpos = np.tile(np.arange(TT, dtype=np.float32)[None, :], (128, 1))
    vbl = np.zeros((128, 8, 8), np.float32)
    for i in range(8):
        for n in range(4, 8):
            if (n - 4) >= i // 2:
                vbl[:, i, n] = NEG
    ropei = np.zeros((128, 1), np.float32)
    ropei[:32, 0] = np.arange(32) % 16
    consts = {"c_identf": identf, "c_perm": perm.astype(bf), "c_tri": tri.astype(bf), "c_ones": ones.astype(bf),
              "c_selm": selm.reshape(8, 1024).astype(bf), "c_tpos": tpos, "c_vbl": vbl.reshape(128, 64), "c_ropei": ropei}
    wnames = ["pre_norm", "post_norm", "w_in", "lam_re", "lam_im", "log_dt", "b_re", "b_im", "c_re", "c_im",
              "d_skip", "w_glu", "b_glu", "w_br_ssm", "w_br_attn", "w_out"]
    shared = {k: np.ascontiguousarray(inputs[k], dtype=np.float32) for k in wnames}
    shared.update(consts)
    in_maps = []
    for c in range(n_cores):
        b, s = c // 2, c % 2
        fl = np.zeros((128, 4), np.float32)
        fl[:, 0] = float(s)
        fl[:, 1] = float(s * T)
        fl[:, 2] = (float(s) - 1.0) * 1.0e30
        m = dict(shared)
        m["x"] = np.ascontiguousarray(x[b, s * T:(s + 1) * T, :])
        m["c_flags"] = fl
        in_maps.append(m)
    key = (n_cores, upto)
    if key not in _NC:
        _NC[key] = build_nc(n_cores, upto)
    res = run_bass_kernel_spmd(_NC[key], in_maps, core_ids=list(range(n_cores)))
    return res.results


def kernel(**inputs):
    results = run(inputs, 8, None)
    out = np.zeros((4, 2048, D), np.float32)
    for c in range(8):
        b, s = c // 2, c % 2
        out[b, s * T:(s + 1) * T, :] = results[c]["y"]
    return out
```
